# Optimizing a Trainium2 kernel written in Bass

```python
import math
import jax, jax.numpy as jnp
from jax import lax
import numpy as np

D_MODEL = 1024
BATCH = 4
SEQ = 8192
DEPTH = 2

MEM_LEN = 256
HEAD_DIM = 64
NSA_HEADS = 8
NSA_KV_HEADS = 2
NSA_HPG = NSA_HEADS // NSA_KV_HEADS
NSA_WIDTH = NSA_HEADS * HEAD_DIM
KV_WIDTH = NSA_KV_HEADS * HEAD_DIM
CMP_BLOCK = 64
SEL_TOPN = 16
WINDOW = 512
Q_BLOCK = 128
FORCE_SCORE = 1e4
LRU_WIDTH = D_MODEL // 2
LRU_BLOCKS = 8
LRU_BLOCK_DIM = LRU_WIDTH // LRU_BLOCKS
CONV_WIDTH = 4
LRU_C = 8.0
MIX_WIDTH = NSA_WIDTH + LRU_WIDTH
IN_COLS = NSA_WIDTH + 6 * KV_WIDTH + 3 * NSA_HEADS + 2 * LRU_WIDTH
X_HEADS = 4
X_HEAD_DIM = D_MODEL // X_HEADS
D_FF = 4 * D_MODEL
N_BUCKETS = 32
MAX_DISTANCE = 128
EPS = 1e-6
NEG = -1e30

kernel_name = "hymba_nsa_rglru_sandwich_trunk"


def rmsnorm(x, g):
    xf = x.astype(jnp.float32)
    r = xf * lax.rsqrt(jnp.mean(xf * xf, axis=-1, keepdims=True) + EPS)
    return (r * g.astype(jnp.float32)).astype(x.dtype)


def masked_softmax(logits, mask):
    l = jnp.where(mask, logits.astype(jnp.float32), NEG)
    m = jnp.max(l, axis=-1, keepdims=True)
    e = jnp.where(mask, jnp.exp(l - m), 0.0)
    return e / jnp.maximum(jnp.sum(e, axis=-1, keepdims=True), 1e-30)


def t5_bucket(dist):
    max_exact = N_BUCKETS // 2
    d = jnp.maximum(dist, 0)
    df = jnp.maximum(d, 1).astype(jnp.float32)
    large = max_exact + (jnp.log(df / max_exact) / math.log(MAX_DISTANCE / max_exact)
                         * (N_BUCKETS - max_exact)).astype(jnp.int32)
    large = jnp.minimum(large, N_BUCKETS - 1)
    return jnp.where(d < max_exact, d, large)


def nsa_attention(q, kc_raw, vc_raw, ks, vs, kw, vw, gates, pe_k, pe_v, w_ck, w_cv, rel_bias):
    B, S = q.shape[0], q.shape[1]
    G, HPG, Dh = NSA_KV_HEADS, NSA_HPG, HEAD_DIM
    nb = S // CMP_BLOCK
    n_sel = min(SEL_TOPN, nb)
    scale = Dh ** -0.5

    def compress(raw, pe, w):
        blocks = raw.reshape(B, nb, CMP_BLOCK, G, Dh) + pe[None, None, :, None, :]
        return jnp.einsum('bnrgd,rde->bnge', blocks, w)

    kc = compress(kc_raw, pe_k, w_ck)
    vc = compress(vc_raw, pe_v, w_cv)
    ks_blk = ks.reshape(B, nb, CMP_BLOCK, G, Dh).transpose(0, 3, 1, 2, 4)
    vs_blk = vs.reshape(B, nb, CMP_BLOCK, G, Dh).transpose(0, 3, 1, 2, 4)
    kw_pad = jnp.pad(kw, ((0, 0), (WINDOW, 0), (0, 0), (0, 0)))
    vw_pad = jnp.pad(vw, ((0, 0), (WINDOW, 0), (0, 0), (0, 0)))
    bias_g = rel_bias.reshape(N_BUCKETS, G, HPG)
    blk_idx = jnp.arange(nb)
    blk_end = blk_idx * CMP_BLOCK + CMP_BLOCK - 1
    b_ix = jnp.arange(B)[:, None, None, None]
    g_ix = jnp.arange(G)[None, :, None, None]

    def one_block(s0):
        t = s0 + jnp.arange(Q_BLOCK)
        qb = lax.dynamic_slice_in_dim(q, s0, Q_BLOCK, axis=1)
        gb = lax.dynamic_slice_in_dim(gates, s0, Q_BLOCK, axis=1)

        dist_c = t[:, None] - blk_end[None, :]
        mask_c = dist_c >= 0
        bias_c = bias_g[t5_bucket(dist_c)].transpose(2, 3, 0, 1)
        s_c = jnp.einsum('bqghd,bngd->bghqn', qb, kc) * scale + bias_c
        p_c = masked_softmax(s_c, mask_c)
        o_c = jnp.einsum('bghqn,bngd->bqghd', p_c.astype(vc.dtype), vc)

        importance = jnp.sum(p_c, axis=2)
        cur = (t // CMP_BLOCK)[:, None]
        j = blk_idx[None, :]
        forced = (j == 0) | (j == cur) | (j == cur - 1)
        sel_score = jnp.where(j <= cur, jnp.where(forced, FORCE_SCORE, importance), NEG)
        top_val, top_idx = lax.top_k(sel_score, n_sel)
        blk_valid = top_val > NEG / 2
        k_sel = ks_blk[b_ix, g_ix, top_idx]
        v_sel = vs_blk[b_ix, g_ix, top_idx]
        pos = top_idx[..., None] * CMP_BLOCK + jnp.arange(CMP_BLOCK)
        dist_s = t[None, None, :, None, None] - pos
        mask_s = blk_valid[..., None] & (dist_s >= 0)
        bias_s = jnp.moveaxis(bias_g[t5_bucket(dist_s), g_ix[..., None]], -1, 2)
        s_s = jnp.einsum('bqghd,bgqnrd->bghqnr', qb, k_sel) * scale + bias_s
        flat = (B, G, HPG, Q_BLOCK, n_sel * CMP_BLOCK)
        mask_s_full = jnp.broadcast_to(mask_s[:, :, None], s_s.shape).reshape(flat)
        p_s = masked_softmax(s_s.reshape(flat), mask_s_full).reshape(s_s.shape)
        o_s = jnp.einsum('bghqnr,bgqnrd->bqghd', p_s.astype(v_sel.dtype), v_sel)

        kwb = lax.dynamic_slice_in_dim(kw_pad, s0, WINDOW + Q_BLOCK, axis=1)
        vwb = lax.dynamic_slice_in_dim(vw_pad, s0, WINDOW + Q_BLOCK, axis=1)
        kpos = s0 - WINDOW + jnp.arange(WINDOW + Q_BLOCK)
        dist_w = t[:, None] - kpos[None, :]
        mask_w = (kpos[None, :] >= 0) & (dist_w >= 0) & (dist_w < WINDOW)
        bias_w = bias_g[t5_bucket(dist_w)].transpose(2, 3, 0, 1)
        s_w = jnp.einsum('bqghd,bkgd->bghqk', qb, kwb) * scale + bias_w
        p_w = masked_softmax(s_w, mask_w)
        o_w = jnp.einsum('bghqk,bkgd->bqghd', p_w.astype(vwb.dtype), vwb)

        g = jax.nn.sigmoid(gb.astype(jnp.float32))
        out = g[..., 0:1] * o_c + g[..., 1:2] * o_s + g[..., 2:3] * o_w
        return out.reshape(B, Q_BLOCK, NSA_WIDTH).astype(q.dtype)

    starts = jnp.arange(S // Q_BLOCK) * Q_BLOCK
    outs = lax.map(one_block, starts)
    return outs.transpose(1, 0, 2, 3).reshape(B, S, NSA_WIDTH)


def rg_lru_branch(xg, xr, conv_w, conv_b, w_a, b_a, w_x, b_x, lam):
    B, S, _ = xr.shape
    y = jax.nn.gelu(xg)
    xp = jnp.pad(xr, ((0, 0), (CONV_WIDTH - 1, 0), (0, 0)))
    xc = conv_b + sum(xp[:, k:k + S] * conv_w[k] for k in range(CONV_WIDTH))
    xb = xc.reshape(B, S, LRU_BLOCKS, LRU_BLOCK_DIM)
    r = jax.nn.sigmoid(jnp.einsum('bsnd,nde->bsne', xb, w_a).reshape(B, S, LRU_WIDTH) + b_a)
    i = jax.nn.sigmoid(jnp.einsum('bsnd,nde->bsne', xb, w_x).reshape(B, S, LRU_WIDTH) + b_x)
    log_a = -LRU_C * jax.nn.softplus(-lam.astype(jnp.float32)) * r.astype(jnp.float32)
    a = jnp.exp(log_a)
    b = jnp.sqrt(-jnp.expm1(2.0 * log_a)) * (i * xc).astype(jnp.float32)

    def combine(left, right):
        a1, b1 = left
        a2, b2 = right
        return a1 * a2, a2 * b1 + b2

    _, h = lax.associative_scan(combine, (a, b), axis=1)
    return h.astype(xr.dtype) * y


def setup_inputs(seed: int = 0) -> dict:
    key = jax.random.key(seed)
    ks = jax.random.split(key, 32)
    f32 = jnp.float32

    def nrm(k, shape, scale):
        return jax.random.normal(k, shape, f32) * scale

    def gain(k, shape):
        return 1.0 + 0.05 * jax.random.normal(k, shape, f32)

    L, D = DEPTH, D_MODEL
    a8 = jax.random.uniform(ks[20], (L, LRU_WIDTH), f32, minval=0.9, maxval=0.999)
    a = a8 ** (1.0 / LRU_C)
    lam = jnp.log(a) - jnp.log1p(-a)
    return {
        "x": nrm(ks[0], (BATCH, SEQ, D), 1.0),
        "mem": nrm(ks[1], (BATCH, MEM_LEN, D), 1.0),
        "rel_bias": nrm(ks[2], (N_BUCKETS, NSA_HEADS), 0.5),
        "ln_mix_pre": gain(ks[3], (L, D)),
        "ln_mix_post": gain(ks[4], (L, D)),
        "w_in": nrm(ks[5], (L, D, IN_COLS), D ** -0.5),
        "cmp_pe_k": nrm(ks[6], (L, CMP_BLOCK, HEAD_DIM), 0.1),
        "cmp_pe_v": nrm(ks[7], (L, CMP_BLOCK, HEAD_DIM), 0.1),
        "cmp_w_k": nrm(ks[8], (L, CMP_BLOCK, HEAD_DIM, HEAD_DIM), (CMP_BLOCK * HEAD_DIM) ** -0.5),
        "cmp_w_v": nrm(ks[9], (L, CMP_BLOCK, HEAD_DIM, HEAD_DIM), (CMP_BLOCK * HEAD_DIM) ** -0.5),
        "conv_w": nrm(ks[10], (L, CONV_WIDTH, LRU_WIDTH), CONV_WIDTH ** -0.5),
        "conv_b": nrm(ks[11], (L, LRU_WIDTH), 0.02),
        "lru_wa": nrm(ks[12], (L, LRU_BLOCKS, LRU_BLOCK_DIM, LRU_BLOCK_DIM), LRU_BLOCK_DIM ** -0.5),
        "lru_ba": nrm(ks[13], (L, LRU_WIDTH), 0.02),
        "lru_wx": nrm(ks[14], (L, LRU_BLOCKS, LRU_BLOCK_DIM, LRU_BLOCK_DIM), LRU_BLOCK_DIM ** -0.5),
        "lru_bx": nrm(ks[15], (L, LRU_WIDTH), 0.02),
        "lru_lambda": lam,
        "gn_attn": gain(ks[16], (L, NSA_WIDTH)),
        "gn_lru": gain(ks[17], (L, LRU_WIDTH)),
        "w_out": nrm(ks[18], (L, MIX_WIDTH, D), MIX_WIDTH ** -0.5),
        "ln_x_pre": gain(ks[19], (L, D)),
        "ln_x_post": gain(ks[21], (L, D)),
        "ln_mem": gain(ks[22], (L, D)),
        "xq": nrm(ks[23], (L, D, D), D ** -0.5),
        "xkv": nrm(ks[24], (L, D, 2 * D), D ** -0.5),
        "xo": nrm(ks[25], (L, D, D), D ** -0.5),
        "ln_mlp_pre": gain(ks[26], (L, D)),
        "ln_mlp_post": gain(ks[27], (L, D)),
        "mlp_w1": nrm(ks[28], (L, D, D_FF), D ** -0.5),
        "mlp_w2": nrm(ks[29], (L, D_FF, D), D_FF ** -0.5),
    }


def reference(x, mem, rel_bias, ln_mix_pre, ln_mix_post, w_in, cmp_pe_k, cmp_pe_v, cmp_w_k, cmp_w_v,
              conv_w, conv_b, lru_wa, lru_ba, lru_wx, lru_bx, lru_lambda, gn_attn, gn_lru, w_out,
              ln_x_pre, ln_x_post, ln_mem, xq, xkv, xo, ln_mlp_pre, ln_mlp_post, mlp_w1, mlp_w2):
    B, S, D = x.shape
    M = mem.shape[1]
    sizes = [NSA_WIDTH] + [KV_WIDTH] * 6 + [3 * NSA_HEADS, LRU_WIDTH, LRU_WIDTH]
    offsets = [int(o) for o in np.cumsum(sizes)[:-1]]
    for l in range(DEPTH):
        h = rmsnorm(x, ln_mix_pre[l])
        z = h @ w_in[l]
        q, kc, vc, ksl, vsl, kwn, vwn, gt, xg, xr = jnp.split(z, offsets, axis=-1)
        kvshape = (B, S, NSA_KV_HEADS, HEAD_DIM)
        att = nsa_attention(
            q.reshape(B, S, NSA_KV_HEADS, NSA_HPG, HEAD_DIM),
            kc.reshape(kvshape), vc.reshape(kvshape), ksl.reshape(kvshape), vsl.reshape(kvshape),
            kwn.reshape(kvshape), vwn.reshape(kvshape),
            gt.reshape(B, S, NSA_KV_HEADS, NSA_HPG, 3),
            cmp_pe_k[l], cmp_pe_v[l], cmp_w_k[l], cmp_w_v[l], rel_bias)
        lru = rg_lru_branch(xg, xr, conv_w[l], conv_b[l], lru_wa[l], lru_ba[l],
                            lru_wx[l], lru_bx[l], lru_lambda[l])
        mixed = jnp.concatenate([rmsnorm(att, gn_attn[l]), rmsnorm(lru, gn_lru[l])], axis=-1) @ w_out[l]
        x = x + rmsnorm(mixed, ln_mix_post[l])

        h = rmsnorm(x, ln_x_pre[l])
        mn = rmsnorm(mem, ln_mem[l])
        cq = (h @ xq[l]).reshape(B, S, X_HEADS, X_HEAD_DIM)
        ck, cv = jnp.split((mn @ xkv[l]).reshape(B, M, 2, X_HEADS, X_HEAD_DIM), 2, axis=2)
        ck, cv = ck[:, :, 0], cv[:, :, 0]
        s = jnp.einsum('bshd,bmhd->bhsm', cq, ck).astype(jnp.float32) * (X_HEAD_DIM ** -0.5)
        p = jax.nn.softmax(s, axis=-1).astype(cv.dtype)
        co = jnp.einsum('bhsm,bmhd->bshd', p, cv).reshape(B, S, D) @ xo[l]
        x = x + rmsnorm(co, ln_x_post[l])

        h = rmsnorm(x, ln_mlp_pre[l])
        u = jnp.square(jax.nn.relu(h @ mlp_w1[l]))
        x = x + rmsnorm(u @ mlp_w2[l], ln_mlp_post[l])
    return x
```

```python
import numpy as np
from contextlib import ExitStack
import concourse.bass as bass
import concourse.mybir as mybir
from concourse.bass_utils import run_bass_kernel_spmd

F32 = mybir.dt.float32
BF16 = mybir.dt.bfloat16
AF = mybir.ActivationFunctionType
ALU = mybir.AluOpType
AX = mybir.AxisListType


class TL:
    def __init__(self, sem, step):
        self.sem = sem
        self.step = step
        self.val = 0


class Buf:
    def __init__(self, t, name, space):
        self.t = t
        self.name = name
        self.space = space
        self.last_w = None
        self.readers = {}
        self.dtl = None

    def __getitem__(self, idx):
        return View(self, self.t[idx])

    @property
    def v(self):
        return View(self, self.t[:])


class View:
    def __init__(self, buf, ap):
        self.buf = buf
        self.ap = ap

    def __getitem__(self, idx):
        return View(self.buf, self.ap[idx])

    def rr(self, pat, **kw):
        return View(self.buf, self.ap.rearrange(pat, **kw))

    def bc(self, shape):
        return View(self.buf, self.ap.broadcast_to(shape))


def _bufs(views):
    out = []
    for v in views:
        if v is None:
            continue
        if isinstance(v, Buf):
            out.append(v)
        elif isinstance(v, View):
            out.append(v.buf)
    return out


def A(v):
    if isinstance(v, View):
        return v.ap
    if isinstance(v, Buf):
        return v.t[:]
    return v


ENGS = ['pe', 'act', 'dve', 'pool', 'sp']


class KB:
    def __init__(self, nc, es):
        self.nc = nc
        self.es = es
        self.tl = {}
        for e in ENGS:
            self.tl[e] = TL(es.enter_context(nc.semaphore("s_" + e)), 1)
        self.prog = {e: [] for e in ENGS}
        self.seen = {e: {} for e in ENGS}
        self.dtls = []
        self.free_dtls = []
        self.phase_dtls = []
        self.root_es = es
        self.nbuf = 0
        self.cur_phase = 0

    def sb(self, shape, dtype, name=None):
        self.nbuf += 1
        name = (name or "sb") + "_%d" % self.nbuf
        t = self.es.enter_context(self.nc.sbuf_tensor(name, list(shape), dtype))
        b = Buf(t, name, 'sb')
        b.phase = self.cur_phase
        return b

    def ps(self, shape, dtype, name=None):
        self.nbuf += 1
        name = (name or "ps") + "_%d" % self.nbuf
        t = self.es.enter_context(self.nc.psum_tensor(name, list(shape), dtype))
        b = Buf(t, name, 'ps')
        b.phase = self.cur_phase
        return b

    def dram(self, name, shape, dtype, kind="Internal"):
        t = self.nc.dram_tensor(name, list(shape), dtype, kind=kind)
        return Buf(t, name, 'dram')

    def sub(self, buf, idx, name=None):
        b = Buf(None, name or (buf.name + "_sub"), buf.space)
        b.t = _Sub(buf.t[idx])
        return b

    def _waits(self, e, reads, writes, own_tl=None):
        need = {}

        def add(tl, val):
            if need.get(tl, 0) < val:
                need[tl] = val
        mytl = self.tl[e]
        for b in reads:
            if b.last_w:
                add(*b.last_w)
        for b in writes:
            if b.last_w:
                tl, val = b.last_w
                if (tl is mytl and e == 'pe') or (own_tl is not None and tl is own_tl):
                    pass
                else:
                    add(tl, val)
            for tl, val in b.readers.items():
                if tl is mytl and e == 'pe':
                    continue
                add(tl, val)
        waits = []
        for tl, val in need.items():
            if tl.step == 16:
                val = tl.val
            if self.seen[e].get(tl, 0) >= val:
                continue
            self.seen[e][tl] = val
            waits.append((tl, val))
        return waits

    def op(self, e, fn, reads, writes):
        reads = _bufs(reads)
        writes = _bufs(writes)
        waits = self._waits(e, reads, writes)
        tl = self.tl[e]
        tl.val += 1
        v = tl.val
        self.prog[e].append((waits, fn, tl.sem, 1))
        for b in reads:
            b.readers[tl] = v
        for b in writes:
            b.last_w = (tl, v)
            b.readers = {}

    def dma(self, e, out, in_, **kw):
        ob = out.buf if isinstance(out, View) else out
        ib = in_.buf if isinstance(in_, View) else in_
        side = ob if ob.space != 'dram' else ib
        if side.dtl is None:
            if self.free_dtls:
                side.dtl = self.free_dtls.pop()
            else:
                side.dtl = TL(self.root_es.enter_context(self.nc.semaphore("d%d" % len(self.dtls))), 16)
                self.dtls.append(side.dtl)
            if getattr(side, 'phase', 0) == self.cur_phase and self.cur_phase > 0:
                self.phase_dtls.append(side.dtl)
        dtl = side.dtl
        waits = self._waits(e, [ib], [ob], own_tl=dtl)
        dtl.val += 16
        v = dtl.val
        oa, ia = A(out), A(in_)
        self.prog[e].append((waits, lambda eng: eng.dma_start(out=oa, in_=ia, **kw), dtl.sem, 16))
        ib.readers[dtl] = v
        ob.last_w = (dtl, v)
        ob.readers = {}

    def barrier(self):
        for e in ENGS:
            waits = []
            for tl in list(self.tl.values()) + self.dtls:
                if tl.val > 0 and self.seen[e].get(tl, 0) < tl.val and tl is not None:
                    self.seen[e][tl] = tl.val
                    waits.append((tl, tl.val))
            self.prog[e].append((waits, None, None, 0))

    def emit_block(self):
        nc = self.nc
        prog = self.prog
        self.prog = {e: [] for e in ENGS}
        self.free_dtls.extend(self.phase_dtls)
        self.phase_dtls = []
        with nc.Block() as block:
            def run(eng, lst):
                for waits, fn, sem, inc in lst:
                    for tl, val in waits:
                        eng.wait_ge(tl.sem, val)
                    if fn is not None:
                        ins = fn(eng)
                        ins.then_inc(sem, inc)

            @block.tensor
            def _(eng):
                run(eng, prog['pe'])

            @block.scalar
            def _(eng):
                run(eng, prog['act'])

            @block.vector
            def _(eng):
                run(eng, prog['dve'])

            @block.gpsimd
            def _(eng):
                run(eng, prog['pool'])

            @block.sync
            def _(eng):
                run(eng, prog['sp'])

    def mm(self, out, lhsT, rhs, start, stop=True, sgc=False, **kw):
        oa, la, ra = A(out), A(lhsT), A(rhs)
        if sgc:
            kw['skip_group_check'] = True
        self.op('pe', lambda eng: eng.matmul(oa, la, ra, start=start, stop=stop, **kw),
                [lhsT, rhs], [out])

    def tr(self, out, in_, ident):
        oa, ia, da = A(out), A(in_), A(ident)
        self.op('pe', lambda eng: eng.transpose(oa, ia, da), [in_, ident], [out])

    def act(self, out, in_, func, bias=None, scale=None, accum_out=None, e='act'):
        oa, ia = A(out), A(in_)
        kw = {}
        rd = [in_]
        if bias is not None:
            kw['bias'] = A(bias)
            rd.append(bias)
        if scale is not None:
            kw['scale'] = A(scale)
            rd.append(scale)
        wr = [out]
        if accum_out is not None:
            kw['accum_out'] = A(accum_out)
            wr.append(accum_out)
        self.op(e, lambda eng: eng.activation(oa, ia, func, **kw), rd, wr)

    def tt(self, out, in0, in1, op, e='dve'):
        oa, a0, a1 = A(out), A(in0), A(in1)
        self.op(e, lambda eng: eng.tensor_tensor(oa, a0, a1, op), [in0, in1], [out])

    def ts(self, out, in0, s1, s2=None, op0=None, op1=None, e='dve', accum_out=None):
        oa, a0 = A(out), A(in0)
        rd = [in0, s1, s2]
        kw = {}
        wr = [out]
        if accum_out is not None:
            kw['accum_out'] = A(accum_out)
            wr.append(accum_out)
        a1, a2 = A(s1), A(s2)
        if op1 is None:
            self.op(e, lambda eng: eng.tensor_scalar(oa, a0, a1, None, op0, **kw), rd, wr)
        else:
            self.op(e, lambda eng: eng.tensor_scalar(oa, a0, a1, a2, op0, op1, **kw), rd, wr)

    def stt(self, out, in0, scalar, in1, op0, op1, e='dve'):
        oa, a0, sc, a1 = A(out), A(in0), A(scalar), A(in1)
        self.op(e, lambda eng: eng.scalar_tensor_tensor(oa, a0, sc, a1, op0, op1), [in0, scalar, in1], [out])

    def copy(self, out, in_, e='dve'):
        oa, ia = A(out), A(in_)
        if e == 'act':
            self.op(e, lambda eng: eng.copy(oa, ia), [in_], [out])
        else:
            self.op(e, lambda eng: eng.tensor_copy(oa, ia), [in_], [out])

    def memset(self, out, val, e='pool'):
        oa = A(out)
        self.op(e, lambda eng: eng.memset(oa, val), [], [out])

    def recip(self, out, in_, e='dve'):
        oa, ia = A(out), A(in_)
        self.op(e, lambda eng: eng.reciprocal(oa, ia), [in_], [out])


class _Sub:
    def __init__(self, ap):
        self.ap = ap

    def __getitem__(self, idx):
        return self.ap[idx]

import math

EPS = 1e-6
NEGM = -30000.0
D = 1024
NV = 72
O_Q, O_KC, O_VC, O_KS, O_VS, O_KW, O_VW, O_GT, O_XG, O_XR = 0, 512, 640, 768, 896, 1024, 1152, 1280, 1304, 1816


class RPool:
    def __init__(self, kb, n, shape, dtype, name, ps=False):
        mk = kb.ps if ps else kb.sb
        self.bufs = [mk(shape, dtype, "%s%d" % (name, i)) for i in range(n)]
        self.i = 0

    def get(self):
        b = self.bufs[self.i % len(self.bufs)]
        self.i += 1
        return b


def phase_begin(kb):
    kb.pes = ExitStack()
    kb.pes.__enter__()
    kb.outer_es = kb.es
    kb.es = kb.pes
    kb.phase_dtls = []


def phase_end(kb):
    kb.barrier()
    kb.emit_block()
    kb.es = kb.outer_es
    kb.pes.__exit__(None, None, None)


class Ctx:
    pass


def wload(kb, c, dst, src, gcol=None, mul=None):
    n = src.ap.shape[-1]
    st = c.wstage.get()
    q = c.dq[c.dqi % 2]
    c.dqi += 1
    kb.dma(q, st[:, :n], src)
    e = ['dve', 'pool'][c.cei % 2]
    c.cei += 1
    if gcol is not None and mul is not None:
        kb.ts(dst, st[:, :n], gcol, float(mul), op0=ALU.mult, op1=ALU.mult, e=e)
    elif gcol is not None:
        kb.ts(dst, st[:, :n], gcol, None, op0=ALU.mult, e=e)
    elif mul is not None:
        kb.ts(dst, st[:, :n], float(mul), None, op0=ALU.mult, e=e)
    else:
        kb.copy(dst, st[:, :n], e=e)


def rstd_from_ss(kb, ss, n):
    kb.ts(ss, ss, 1.0 / n, EPS, op0=ALU.mult, op1=ALU.add)
    kb.act(ss, ss, AF.Sqrt)
    kb.recip(ss, ss)


def norm_T(kb, c, xv, hT_dst, ident):
    st = c.stat.get()
    junk = c.junk.get()
    kb.act(junk.v, xv, AF.Square, accum_out=st[:, 0:1])
    rstd_from_ss(kb, st[:, 0:1], D)
    hb = c.hb.get()
    kb.ts(hb.v, xv, st[:, 0:1], None, op0=ALU.mult)
    pT = c.pT.get()
    for k in range(8):
        kb.tr(pT[:, k * 128:(k + 1) * 128], hb[:, k * 128:(k + 1) * 128], ident.v)
    kb.copy(hT_dst, pT.v.rr("p (c t) -> p c t", c=8), e='act')


def phase_M1(kb, g, l, xsrc):
    T, NT = g.T, g.NT
    phase_begin(kb)
    c = Ctx()
    c.wstage = RPool(kb, 3, [128, 2048], F32, "wst")
    c.dq = ['sp', 'act']
    c.dqi = 0
    c.cei = 0
    gc = kb.sb([128, NV], F32, "gc")
    kb.dma('sp', gc.v, g.gcols[l])
    WF = kb.sb([128, 8, 2048], BF16, "WF")
    WT = kb.sb([128, 8, 280], BF16, "WT")
    w = g.w_in
    for k in range(8):
        rows = slice(k * 128, (k + 1) * 128)
        gk = gc[:, k:k + 1]
        wload(kb, c, WF[:, k, 0:512], w[l, rows, O_Q:O_Q + 512], gk, 0.125)
        wload(kb, c, WF[:, k, 512:768], w[l, rows, O_KC:O_KC + 256], gk)
        wload(kb, c, WF[:, k, 768:896], w[l, rows, O_KS:O_KS + 128], gk)
        wload(kb, c, WF[:, k, 896:1024], w[l, rows, O_KW:O_KW + 128], gk)
        wload(kb, c, WF[:, k, 1024:2048], w[l, rows, O_XG:O_XG + 1024], gk)
        wload(kb, c, WT[:, k, 0:128], w[l, rows, O_VS:O_VS + 128], gk)
        wload(kb, c, WT[:, k, 128:256], w[l, rows, O_VW:O_VW + 128], gk)
        wload(kb, c, WT[:, k, 256:280], w[l, rows, O_GT:O_GT + 24], gk)
    ident = kb.sb([128, 128], BF16, "identb")
    idf = kb.sb([128, 128], F32, "identf")
    kb.dma('sp', idf.v, g.ident.v)
    kb.copy(ident.v, idf.v)
    c.stat = RPool(kb, 4, [128, 4], F32, "stat")
    c.junk = RPool(kb, 2, [128, 1024], F32, "junk")
    c.hb = RPool(kb, 2, [128, 1024], BF16, "hb")
    c.pT = RPool(kb, 1, [128, 1024], BF16, "pT", ps=True)
    xp = RPool(kb, 3, [128, 1024], F32, "xin")
    hTp = RPool(kb, 2, [128, 8, 512], BF16, "hT")
    ptm = RPool(kb, 1, [128, 512], F32, "ptm", ps=True)
    pfm = RPool(kb, 3, [128, 512], F32, "pfm", ps=True)
    vst = RPool(kb, 2, [128, 280], F32, "vst")
    fb = RPool(kb, 3, [128, 512], BF16, "fb")
    ff = RPool(kb, 3, [128, 512], F32, "ff")
    MT = min(512, T)
    nsub = MT // 128
    ei = 0
    for m in range(T // MT):
        hT = hTp.get()
        for s in range(nsub):
            t0 = m * MT + s * 128
            xs = xp.get()
            kb.dma('sp', xs.v, xsrc[t0:t0 + 128, :])
            norm_T(kb, c, xs.v, hT[:, :, s * 128:(s + 1) * 128], ident)
            pt = ptm.get()
            for k in range(8):
                kb.mm(pt[:, 0:280], hT[:, k, s * 128:(s + 1) * 128], WT[:, k, :], start=(k == 0), stop=(k == 7))
            vs = vst.get()
            kb.copy(vs.v, pt[:, 0:280], e='dve')
            kb.dma('sp', g.zV[t0:t0 + 128, :], vs.v)
        for fc in range(16):
            pf = pfm.get()
            for k in range(8):
                kb.mm(pf[:, :MT], WF[:, k, fc * 128:(fc + 1) * 128], hT[:, k, :MT], start=(k == 0), stop=(k == 7))
            e = ['act', 'dve'][ei % 2]
            ei += 1
            if fc < 8:
                o = fb.get()
                kb.copy(o[:, :MT], pf[:, :MT], e=e)
                kb.dma('act' if fc % 2 else 'sp', g.zTb[fc * 128:(fc + 1) * 128, m * MT:(m + 1) * MT], o[:, :MT])
            else:
                o = ff.get()
                kb.copy(o[:, :MT], pf[:, :MT], e=e)
                kb.dma('act' if fc % 2 else 'sp', g.zTf[(fc - 8) * 128:(fc - 7) * 128, m * MT:(m + 1) * MT], o[:, :MT])
    phase_end(kb)


def load_const_bf16(kb, c, dram_view, shape, name):
    st = kb.sb(shape, F32, name + "_f")
    kb.dma('sp', st.v, dram_view)
    b = kb.sb(shape, BF16, name)
    kb.copy(b.v, st.v, e='pool')
    return b


def phase_ATT(kb, g, l):
    T, NT, NB = g.T, g.NT, g.NB
    phase_begin(kb)
    c = Ctx()
    c.wstage = RPool(kb, 1, [128, 2048], F32, "wst")
    c.dq = ['sp', 'act']
    c.dqi = 0
    c.cei = 0
    ident = load_const_bf16(kb, c, g.ident.v, [128, 128], "ident")
    onesb = load_const_bf16(kb, c, g.ones_tab.v, [128, 128], "onesb")
    TCt = kb.sb([128, 8, 256], F32, "TCt")
    kb.dma('sp', TCt.v.rr("p h j -> p (h j)"), g.tc_tab.v)
    C31 = kb.sb([128, 8, 128], F32, "C31")
    kb.dma('act', C31.v.rr("p h j -> p (h j)"), g.c31_tab.v)
    DGs = kb.sb([128, 8, 128], F32, "DGs")
    kb.dma('sp', DGs.v.rr("p h j -> p (h j)"), g.dg_tab.v)
    kb.tt(DGs.v, DGs.v, C31.v, ALU.subtract, e='pool')
    SDGs = kb.sb([128, 8, 128], F32, "SDGs")
    kb.dma('act', SDGs.v.rr("p h j -> p (h j)"), g.sdg_tab.v)
    kb.tt(SDGs.v, SDGs.v, C31.v, ALU.subtract, e='pool')
    WEt = kb.sb([128, 4, 128], F32, "WEt")
    kb.dma('sp', WEt.v.rr("p h j -> p (h j)"), g.we_tab.v)
    ADDT = kb.sb([128, 256], F32, "ADDT")
    kb.dma('sp', ADDT.v, g.addt_tab.v)
    RV0 = kb.sb([128, 1], F32, "RV0")
    kb.dma('sp', RV0.v, g.rv0_tab.v)
    EX = kb.sb([128, T], BF16, "EX")
    KC2 = kb.sb([128, T], BF16, "KC2")
    VC2 = kb.sb([128, T], BF16, "VC2")
    EXa, EXb = KC2, VC2
    kb.memset(EXa.v, 1.0, e='pool')
    a0, a1, a2 = A(EXa.v), A(EXb.v), A(EX.v)
    kb.op('pool', lambda eng: eng.affine_select(a1, a0, [[1, T]], ALU.is_ge, 0.0, base=0, channel_multiplier=-64),
          [EXa], [EXb])
    kb.op('pool', lambda eng: eng.affine_select(a2, a1, [[-1, T]], ALU.is_ge, 0.0, base=63, channel_multiplier=64),
          [EXb], [EX])
    ssa = kb.sb([128, 2 * NT], F32, "ssa")
    LV = 9
    if LV <= 1:
        phase_end(kb)
        return

    KsT = kb.sb([64, T], BF16, "KsT")
    KwT = kb.sb([64, T], BF16, "KwT")
    VsA = kb.sb([128, NT, 65], BF16, "VsA")
    VwA = kb.sb([128, NT, 65], BF16, "VwA")
    Wck = kb.sb([128, 32, 128], BF16, "Wck")
    Wcv = kb.sb([128, 32, 64], BF16, "Wcv")
    pek = kb.sb([128, 32], BF16, "pek")
    pev = kb.sb([128, 32], BF16, "pev")
    pef = kb.sb([128, 64], F32, "pef")
    ckc = kb.sb([128, 1], F32, "ckc")
    cvr = kb.sb([1, 64], BF16, "cvr")
    KcT = kb.sb([128, NB], BF16, "KcT")
    Vc = kb.sb([128, 64], BF16, "Vc")
    vstage = RPool(kb, 2, [128, 16, 64], F32, "vstg")
    bankA = kb.ps([128, 512], F32, "bankA")
    bankT = kb.ps([128, 1024], BF16, "bankT")
    bankOc = kb.ps([128, 512], F32, "bankOc")
    stp = RPool(kb, 3, [128, 512], F32, "ST", ps=True)
    bOs = kb.ps([128, 512], F32, "bOs")
    bOw = kb.ps([128, 512], F32, "bOw")
    qap = RPool(kb, 3, [64, 4, 128], BF16, "Q4")
    gtp = RPool(kb, 2, [128, 12], F32, "GT")
    gsp = RPool(kb, 2, [128, 12], F32, "GS")
    scp = RPool(kb, 2, [128, 4, NB], F32, "sC")
    ecp = RPool(kb, 2, [128, 4, NB], F32, "eC")
    ecbp = RPool(kb, 2, [128, 4, NB], BF16, "eCb")
    ectp = RPool(kb, 2, [128, 4, 128], BF16, "eCT")
    stat = RPool(kb, 3, [128, 48], F32, "st")
    impp = RPool(kb, 2, [128, NB], F32, "imp")
    selp = RPool(kb, 2, [128, NB], F32, "sel")
    sel2p = RPool(kb, 2, [128, NB], F32, "sel2")
    selmp = RPool(kb, 2, [128, NB], BF16, "selm")
    negp = RPool(kb, 2, [128, 4, 128], BF16, "negm")
    tmpp = RPool(kb, 2, [128, 4, 128], F32, "tmpl")
    pp = RPool(kb, 4, [128, 4, 128], BF16, "P")
    attp = RPool(kb, 2, [128, 4, 64], F32, "att")
    attbp = RPool(kb, 2, [128, 256], BF16, "attb")
    attTp = RPool(kb, 2, [128, 2, 128], BF16, "attT")
    junk = kb.sb([128, 256], F32, "junk")

    kb.dma('sp', pef[:, 0:32], g.pe2k[l])
    kb.dma('sp', pef[:, 32:64], g.pe2v[l])
    kb.copy(pek.v, pef[:, 0:32], e='pool')
    kb.copy(pev.v, pef[:, 32:64], e='pool')
    st = c.wstage.get()
    kb.dma('sp', st.v.rr("p (c e) -> p c e", e=64), g.cmp_w_k[l].rr("(c p) e -> p c e", p=128))
    kb.copy(Wck[:, :, 0:64], st.v.rr("p (c e) -> p c e", e=64), e='dve')
    kb.copy(Wck[:, :, 64:128], st.v.rr("p (c e) -> p c e", e=64), e='pool')
    st = c.wstage.get()
    kb.dma('act', st.v.rr("p (c e) -> p c e", e=64), g.cmp_w_v[l].rr("(c p) e -> p c e", p=128))
    kb.copy(Wcv.v, st.v.rr("p (c e) -> p c e", e=64), e='dve')

    for g_ in range(2):
        kb.dma('sp', KsT[0:64, :], g.zTb[768 + g_ * 64:768 + g_ * 64 + 64, :])
        kb.dma('act', KwT[0:64, :], g.zTb[896 + g_ * 64:896 + g_ * 64 + 64, :])
        kb.dma('sp', KC2[0:64, :], g.zTb[512 + g_ * 64:512 + g_ * 64 + 64, :])
        kb.dma('sp', KC2[64:128, 0:T - 1], g.zTb[512 + g_ * 64:512 + g_ * 64 + 64, 1:T])
        kb.dma('act', VC2[0:64, :], g.zTb[640 + g_ * 64:640 + g_ * 64 + 64, :])
        kb.dma('act', VC2[64:128, 0:T - 1], g.zTb[640 + g_ * 64:640 + g_ * 64 + 64, 1:T])
        for (VA, coff) in ((VsA, g_ * 64), (VwA, 128 + g_ * 64)):
            kb.memset(VA[:, :, 64:65], 1.0, e='pool')
            for j0 in range(0, NT, 16):
                nj = min(16, NT - j0)
                vs = vstage.get()
                kb.dma('sp', vs[:, :nj, :], g.zV[j0 * 128:(j0 + nj) * 128, coff:coff + 64].rr("(j p) d -> p j d", p=128))
                kb.copy(VA[:, j0:j0 + nj, 0:64], vs[:, :nj, :], e='dve')
        if LV <= 2:
            continue
        KC2v = KC2.v.rr("p (n r) -> p n r", r=64)
        VC2v = VC2.v.rr("p (n r) -> p n r", r=64)
        for cc in range(32):
            kb.mm(bankA[:, 0:1], Wck[:, cc, :], pek[:, cc:cc + 1], start=(cc == 0), stop=(cc == 31))
        kb.copy(ckc.v, bankA[:, 0:1], e='dve')
        for cc in range(32):
            kb.mm(bankOc[:, 0:NB], Wck[:, cc, :], KC2v[:, :, 2 * cc], start=(cc == 0), stop=(cc == 31))
        kb.ts(KcT.v, bankOc[:, 0:NB], ckc.v, None, op0=ALU.add)
        for cc in range(32):
            kb.mm(bankA[0:1, 0:64], pev[:, cc:cc + 1], Wcv[:, cc, :], start=(cc == 0), stop=(cc == 31))
        kb.copy(cvr.v, bankA[0:1, 0:64], e='dve')
        for cc in range(32):
            kb.mm(bankOc[:NB, 0:64], VC2v[:, :, 2 * cc], Wcv[:, cc, :], start=(cc == 0), stop=False)
        kb.mm(bankOc[:NB, 0:64], onesb[0:1, :NB], cvr[0:1, :], start=False, stop=True)
        kb.copy(Vc[:NB, :], bankOc[:NB, 0:64], e='dve')

        if LV <= 3:
            continue
        for i in range(NT if LV > 8 else 1):
            Q4 = qap.get()
            r0 = g_ * 256
            kb.dma('sp', Q4.v, g.zTb[r0:r0 + 256, i * 128:(i + 1) * 128].rr("(h d) t -> d h t", d=64))
            Qh = [Q4[:, h, :] for h in range(4)]
            hs = [slice(0, 64)] * 4
            GT = gtp.get()
            kb.dma('act', GT.v, g.zV[i * 128:(i + 1) * 128, 256 + g_ * 12:256 + g_ * 12 + 12])
            GS = gsp.get()
            kb.act(GS.v, GT.v, AF.Sigmoid)
            G3 = GS.v.rr("p (h k) -> p h k", k=3)
            s_ = stat.get()
            mx, nmx, sumC, rC = s_[:, 0:4], s_[:, 4:8], s_[:, 8:12], s_[:, 12:16]
            rS, rW, aC, aS, aW = s_[:, 16:20], s_[:, 20:24], s_[:, 24:28], s_[:, 28:32], s_[:, 32:36]
            m1, m2, thr = s_[:, 36:44], s_[:, 36:44], s_[:, 44:45]
            psC = bankA.v.rr("p (h n) -> p h n", h=4)[:, :, 0:NB]
            for h in range(4):
                kb.mm(psC[:, h, :], Qh[h], KcT[hs[h], :], start=(h == 0), stop=(h == 3), sgc=True)
            sC = scp.get()
            kb.tt(sC.v, psC, TCt[:, g_ * 4:(g_ + 1) * 4, 128 - 2 * i:128 - 2 * i + NB], ALU.add)
            sv, mv = A(sC.v), A(mx)
            kb.op('dve', lambda eng, sv=sv, mv=mv: eng.tensor_reduce(mv, sv, AX.X, ALU.max), [sC], [s_])
            kb.ts(nmx, mx, -1.0, None, op0=ALU.mult)
            eC = ecp.get()
            for h in range(4):
                kb.act(eC[:, h, :], sC[:, h, :], AF.Exp, bias=nmx[:, h:h + 1], accum_out=sumC[:, h:h + 1])
            kb.recip(rC, sumC)
            if i == 0:
                kb.ts(rC, rC, RV0.v, None, op0=ALU.mult)
            imp = impp.get()
            kb.ts(imp.v, eC[:, 0, :], rC[:, 0:1], None, op0=ALU.mult)
            for h in range(1, 4):
                kb.stt(imp.v, eC[:, h, :], rC[:, h:h + 1], imp.v, ALU.mult, ALU.add)
            if LV <= 4:
                continue
            eCb = ecbp.get()
            kb.copy(eCb.v, eC.v, e='pool')
            for h in range(4):
                kb.tr(bankT[:NB, h * 128:(h + 1) * 128], eCb[:, h, :], ident.v)
            eCT = ectp.get()
            kb.copy(eCT[:NB].rr("p h q -> p (h q)"), bankT[:NB, 0:512], e='dve')
            psOc = bankOc.v.rr("p (h d) -> p h d", h=4)[:, :, 0:64]
            for h in range(4):
                kb.mm(psOc[:, h, :], eCT[:NB, h, :], Vc[:NB, :], start=(h == 0), stop=(h == 3), sgc=True)
            if LV <= 5:
                continue
            sel = selp.get()
            kb.tt(sel.v, imp.v, ADDT[:, 128 - 2 * i:128 - 2 * i + NB], ALU.add)
            kb.memset(sel[:, 0:1], 1e4, e='dve')
            sa, m1a = A(sel.v), A(m1)
            kb.op('dve', lambda eng, sa=sa, m1a=m1a: eng.max(m1a, sa), [sel], [s_])
            if NB > 16:
                sel2 = sel2p.get()
                s2a = A(sel2.v)
                kb.op('dve', lambda eng, sa=sa, m1a=m1a, s2a=s2a: eng.match_replace(s2a, m1a, sa, -1e30), [sel, s_], [sel2])
                kb.op('dve', lambda eng, s2a=s2a, m1a=m1a: eng.max(m1a, s2a), [sel2], [s_])
                kb.ts(thr, m2[:, 7:8], -5e29, None, op0=ALU.max)
            else:
                kb.memset(thr, -5e29, e='dve')
            selm = selmp.get()
            kb.ts(selm.v, sel.v, thr, None, op0=ALU.is_ge)
            kb.tr(bankT[:NB, 512:640], selm.v, ident.v)
            negm = negp.get()
            for h in range(4):
                kb.ts(negm[:NB, h, :], bankT[:NB, 512:640], -1.0, 30000.0, op0=ALU.add, op1=ALU.mult)
            if LV <= 6:
                continue
            Os = bOs.v[:, 0:260].rr("p (h d) -> p h d", h=4)
            Ow = bOw.v[:, 0:260].rr("p (h d) -> p h d", h=4)
            for br in range(2):
                if br == 0:
                    js = list(range(0, i + 1))
                    KT, VA, O = KsT, VsA, Os
                else:
                    js = list(range(max(0, i - 4), i + 1))
                    KT, VA, O = KwT, VwA, Ow
                for j in js:
                    STb = stp.get()
                    ST = STb.v.rr("p (h q) -> p h q", h=4)
                    if br == 0:
                        kb.mm(STb.v, EX[:NB, j * 128:(j + 1) * 128], negm[:NB].rr("p h q -> p (h q)"), start=True, stop=False, sgc=True)
                    for h in range(4):
                        kb.mm(ST[:, h, :], KT[hs[h], j * 128:(j + 1) * 128], Qh[h],
                              start=(br == 1 and h == 0), stop=(h == 3), sgc=True)
                    P = pp.get()
                    tm = None
                    if j == i:
                        tm = DGs[:, g_ * 4:(g_ + 1) * 4, :]
                    elif j == i - 1:
                        tm = SDGs[:, g_ * 4:(g_ + 1) * 4, :]
                    elif br == 1 and j == i - 4:
                        tm = WEt.v
                    if tm is not None:
                        tp = tmpp.get()
                        kb.tt(tp.v, ST, tm, ALU.add)
                        kb.act(P.v, tp.v, AF.Exp)
                    else:
                        kb.act(P.v, ST, AF.Exp)
                    for h in range(4):
                        kb.mm(O[:, h, :], P[:, h, :], VA[:, j, :], start=(j == js[0] and h == 0),
                              stop=(j == js[-1] and h == 3), sgc=True)
            if LV <= 7:
                continue
            kb.recip(rS, Os[:, :, 64])
            kb.recip(rW, Ow[:, :, 64])
            kb.tt(aC, G3[:, :, 0], rC, ALU.mult)
            kb.tt(aS, G3[:, :, 1], rS, ALU.mult)
            kb.tt(aW, G3[:, :, 2], rW, ALU.mult)
            att = attp.get()
            for h in range(4):
                kb.ts(att[:, h, :], psOc[:, h, :], aC[:, h:h + 1], None, op0=ALU.mult)
                kb.stt(att[:, h, :], Os[:, h, 0:64], aS[:, h:h + 1], att[:, h, :], ALU.mult, ALU.add)
                kb.stt(att[:, h, :], Ow[:, h, 0:64], aW[:, h:h + 1], att[:, h, :], ALU.mult, ALU.add)
            attf = att.v.rr("p h d -> p (h d)")
            kb.act(junk.v, attf, AF.Square, accum_out=ssa[:, g_ * NT + i:g_ * NT + i + 1])
            attb = attbp.get()
            kb.copy(attb.v, attf, e='pool')
            for fc in range(2):
                kb.tr(bankT[:, 640 + fc * 128:640 + (fc + 1) * 128], attb[:, fc * 128:(fc + 1) * 128], ident.v)
            attT = attTp.get()
            kb.copy(attT.v.rr("p c q -> p (c q)"), bankT[:, 640:896], e='dve')
            kb.dma('act', g.catT[g_ * 256:(g_ + 1) * 256, i * 128:(i + 1) * 128].rr("(c p) t -> p c t", p=128), attT.v)
    kb.dma('sp', g.ssq[:, 0:2 * NT], ssa.v)
    phase_end(kb)


def bcast_row(view_1xn, p=128):
    ap = view_1xn.ap
    return View(view_1xn.buf, ap.broadcast_to([p, ap.shape[-1]]))


def phase_LRU(kb, g, l):
    T, NT = g.T, g.NT
    phase_begin(kb)
    TC = min(2048, T)
    nchunk = T // TC
    gc = kb.sb([128, NV], F32, "gc")
    kb.dma('sp', gc.v, g.gcols[l])
    onesf = kb.sb([128, 128], F32, "onesf")
    kb.dma('sp', onesf.v, g.ones_tab.v)
    WaBD = kb.sb([128, 4, 128], BF16, "WaBD")
    WxBD = kb.sb([128, 4, 128], BF16, "WxBD")
    kb.memset(WaBD.v, 0.0, e='pool')
    kb.memset(WxBD.v, 0.0, e='pool')
    wst = kb.sb([128, 2, 4, 64], F32, "wst")
    for wi, (src, dst) in enumerate(((g.lru_wa, WaBD), (g.lru_wx, WxBD))):
        for n_ in range(2):
            kb.dma('sp', wst[n_ * 64:(n_ + 1) * 64, wi, :, :], src[l].rr("(c n) d e -> n d c e", n=2)[n_])
        kb.copy(dst[0:64, :, 0:64], wst[0:64, wi, :, :], e='dve')
        kb.copy(dst[64:128, :, 64:128], wst[64:128, wi, :, :], e='dve')
    cA = kb.sb([128, 4], F32, "cA")
    kb.act(cA.v, gc[:, 68:72], AF.Exp, scale=-1.0)
    kb.act(cA.v, cA.v, AF.Ln, bias=1.0)
    kb.ts(cA.v, cA.v, -8.0, None, op0=ALU.mult)
    carry = kb.sb([128, 4], F32, "carry")
    ssl = kb.sb([128, NT], F32, "ssl")
    psS = kb.ps([128, 512], F32, "psS")
    psg = RPool(kb, 4, [128, 512], F32, "psg", ps=True)
    xgp = RPool(kb, 2, [128, TC], F32, "xg")
    xrp = RPool(kb, 2, [128, TC + 3], F32, "xr")
    xcp = RPool(kb, 2, [128, TC], F32, "xc")
    xcbp = RPool(kb, 2, [128, TC], BF16, "xcb")
    rp = RPool(kb, 2, [128, TC], F32, "r")
    igp = RPool(kb, 2, [128, TC], F32, "ig")
    t1p = RPool(kb, 2, [128, TC], F32, "t1")
    t2p = RPool(kb, 2, [128, TC], F32, "t2")
    hhp = RPool(kb, 2, [128, TC], F32, "hh")
    obp = RPool(kb, 2, [128, TC], BF16, "ob")
    for tc in range(nchunk):
        c0 = tc * TC
        for c in range(4):
            xg = xgp.get()
            kb.dma('sp', xg.v, g.zTf[c * 128:(c + 1) * 128, c0:c0 + TC])
            xr = xrp.get()
            if tc == 0:
                kb.memset(xr[:, 0:3], 0.0, e='pool')
                kb.dma('act', xr[:, 3:3 + TC], g.zTf[512 + c * 128:512 + (c + 1) * 128, c0:c0 + TC])
            else:
                kb.dma('act', xr.v, g.zTf[512 + c * 128:512 + (c + 1) * 128, c0 - 3:c0 + TC])
            xc = xcp.get()
            kb.ts(xc.v, xr[:, 3:3 + TC], gc[:, 40 + 3 * 4 + c:40 + 3 * 4 + c + 1], gc[:, 56 + c:57 + c], op0=ALU.mult, op1=ALU.add)
            for k in (2, 1, 0):
                kb.stt(xc.v, xr[:, k:k + TC], gc[:, 40 + k * 4 + c:40 + k * 4 + c + 1], xc.v, ALU.mult, ALU.add)
            xcb = xcbp.get()
            kb.copy(xcb.v, xc.v, e='act')
            r = rp.get()
            ig = igp.get()
            for blk in range(TC // 512):
                cs = slice(blk * 512, (blk + 1) * 512)
                pa = psg.get()
                kb.mm(pa.v, WaBD[:, c, :], xcb[:, cs], start=True, stop=True)
                kb.act(r[:, cs], pa.v, AF.Sigmoid, bias=gc[:, 60 + c:61 + c])
                px = psg.get()
                kb.mm(px.v, WxBD[:, c, :], xcb[:, cs], start=True, stop=True)
                kb.act(ig[:, cs], px.v, AF.Sigmoid, bias=gc[:, 64 + c:65 + c])
            kb.act(r.v, r.v, AF.Exp, scale=cA[:, c:c + 1])
            t1 = t1p.get()
            kb.act(t1.v, r.v, AF.Square)
            kb.act(t1.v, t1.v, AF.Sqrt, scale=-1.0, bias=1.0)
            kb.tt(ig.v, ig.v, xc.v, ALU.mult, e='pool')
            kb.tt(t1.v, t1.v, ig.v, ALU.mult)
            hh = hhp.get()
            init = 0.0 if tc == 0 else A(carry[:, c:c + 1])
            ha, ra, ba = A(hh.v), A(r.v), A(t1.v)
            kb.op('dve', lambda eng, ha=ha, ra=ra, ba=ba, init=init: eng.tensor_tensor_scan(ha, ra, ba, init, ALU.mult, ALU.add),
                  [r, t1, carry], [hh])
            kb.copy(carry[:, c:c + 1], hh[:, TC - 1:TC], e='dve')
            t2 = t2p.get()
            kb.tt(t2.v, xg.v, xg.v, ALU.mult, e='pool')
            kb.ts(t2.v, t2.v, 0.044715, 1.0, op0=ALU.mult, op1=ALU.add, e='pool')
            kb.tt(t2.v, t2.v, xg.v, ALU.mult, e='pool')
            kb.act(t2.v, t2.v, AF.Sigmoid, scale=1.5957691216057308)
            kb.tt(t2.v, t2.v, xg.v, ALU.mult, e='pool')
            kb.tt(hh.v, hh.v, t2.v, ALU.mult)
            ob = obp.get()
            kb.copy(ob.v, hh.v, e='act')
            kb.dma('sp', g.catT[512 + c * 128:512 + (c + 1) * 128, c0:c0 + TC], ob.v)
            kb.tt(t2.v, hh.v, hh.v, ALU.mult, e='pool')
            nb128 = TC // 128
            for b in range(nb128):
                kb.mm(psS[:, b:b + 1], t2[:, b * 128:(b + 1) * 128], onesf[:, 0:1],
                      start=(c == 0 and b == 0), stop=(c == 3 and b == nb128 - 1), sgc=True)
        kb.copy(ssl[:, tc * (TC // 128):(tc + 1) * (TC // 128)], psS[:, 0:TC // 128], e='dve')
    kb.dma('sp', g.ssq[:, 2 * NT:3 * NT], ssl.v)
    phase_end(kb)


def phase_XKV(kb, g, l):
    phase_begin(kb)
    c = Ctx()
    c.wstage = RPool(kb, 3, [128, 2048], F32, "wst")
    c.dq = ['sp', 'act']
    c.dqi = 0
    c.cei = 0
    gc = kb.sb([128, NV], F32, "gc")
    kb.dma('sp', gc.v, g.gcols[l])
    ident = load_const_bf16(kb, c, g.ident.v, [128, 128], "ident")
    Wkv = kb.sb([128, 8, 2048], BF16, "Wkv")
    for k in range(8):
        wload(kb, c, Wkv[:, k, :], g.xkv[l, k * 128:(k + 1) * 128, :], gc[:, 24 + k:25 + k])
    c.stat = RPool(kb, 2, [128, 4], F32, "stat")
    c.junk = RPool(kb, 1, [128, 1024], F32, "junk")
    c.hb = RPool(kb, 2, [128, 1024], BF16, "hb")
    c.pT = RPool(kb, 1, [128, 1024], BF16, "pT", ps=True)
    mnT = kb.sb([128, 8, 256], BF16, "mnT")
    mp = RPool(kb, 2, [128, 1024], F32, "memt")
    for s in range(2):
        mt = mp.get()
        kb.dma('sp', mt.v, g.mem[s * 128:(s + 1) * 128, :])
        norm_T(kb, c, mt.v, mnT[:, :, s * 128:(s + 1) * 128], ident)
    psp = RPool(kb, 3, [128, 512], F32, "psx", ps=True)
    ob = RPool(kb, 3, [128, 512], BF16, "ob")
    for fc in range(8):
        p = psp.get()
        for k in range(8):
            kb.mm(p[:, 0:256], Wkv[:, k, fc * 128:(fc + 1) * 128], mnT[:, k, :], start=(k == 0), stop=(k == 7))
        o = ob.get()
        kb.copy(o[:, 0:256], p[:, 0:256], e='dve')
        kb.dma('sp', g.ckT[fc * 128:(fc + 1) * 128, :], o[:, 0:256])
    for mc in range(2):
        for half in range(2):
            p = psp.get()
            for k in range(8):
                kb.mm(p.v, mnT[:, k, mc * 128:(mc + 1) * 128], Wkv[:, k, 1024 + half * 512:1024 + (half + 1) * 512],
                      start=(k == 0), stop=(k == 7))
            o = ob.get()
            kb.copy(o.v, p.v, e='act')
            kb.dma('sp', g.cv[mc * 128:(mc + 1) * 128, half * 512:(half + 1) * 512], o.v)
    phase_end(kb)


def phase_R1(kb, g, l, xsrc):
    T, NT = g.T, g.NT
    phase_begin(kb)
    c = Ctx()
    c.wstage = RPool(kb, 2, [128, 1024], F32, "wst")
    c.dq = ['sp', 'act']
    c.dqi = 0
    c.cei = 0
    gc = kb.sb([128, NV], F32, "gc")
    kb.dma('sp', gc.v, g.gcols[l])
    ident = load_const_bf16(kb, c, g.ident.v, [128, 128], "ident")
    onesb = load_const_bf16(kb, c, g.ones_tab.v, [128, 128], "onesb")
    WoA = kb.sb([128, 4, 1024], BF16, "WoA")
    WoL = kb.sb([128, 4, 1024], BF16, "WoL")
    Wq = kb.sb([128, 8, 1024], BF16, "Wq")
    Wo = kb.sb([128, 8, 1024], BF16, "Wo")
    for k in range(4):
        wload(kb, c, WoA[:, k, :], g.w_out[l, k * 128:(k + 1) * 128, :], gc[:, 32 + k:33 + k])
        wload(kb, c, WoL[:, k, :], g.w_out[l, 512 + k * 128:512 + (k + 1) * 128, :], gc[:, 36 + k:37 + k])
    for k in range(8):
        wload(kb, c, Wq[:, k, :], g.xq[l, k * 128:(k + 1) * 128, :], gc[:, 8 + k:9 + k], 1.0 / 16.0)
        wload(kb, c, Wo[:, k, :], g.xo[l, k * 128:(k + 1) * 128, :])
    ckT = kb.sb([128, 8, 256], BF16, "ckTs")
    kb.dma('sp', ckT.v, g.ckT.v.rr("(c p) m -> p c m", p=128))
    cv = kb.sb([128, 2, 1024], BF16, "cvs")
    kb.dma('act', cv.v, g.cv.v.rr("(c p) f -> p c f", p=128))
    GP1 = kb.sb([128, 1024], F32, "GP1")
    GP2 = kb.sb([128, 1024], F32, "GP2")
    kb.dma('sp', GP1.v, bcast_row(g.grows[l, 0:1, :]))
    kb.dma('act', GP2.v, bcast_row(g.grows[l, 1:2, :]))
    SS = kb.sb([128, 3 * NT], F32, "SS")
    kb.dma('sp', SS.v, g.ssq.v)
    rA = kb.sb([128, NT], F32, "rA")
    rL = kb.sb([128, NT], F32, "rL")
    kb.tt(rA.v, SS[:, 0:NT], SS[:, NT:2 * NT], ALU.add)
    rstd_from_ss(kb, rA.v, 512)
    kb.copy(rL.v, SS[:, 2 * NT:3 * NT], e='dve')
    rstd_from_ss(kb, rL.v, 512)
    c.stat = RPool(kb, 4, [128, 4], F32, "stat")
    c.junk = RPool(kb, 1, [128, 1024], F32, "junk")
    c.hb = RPool(kb, 2, [128, 1024], BF16, "hb")
    c.pT = RPool(kb, 1, [128, 1024], BF16, "pT", ps=True)
    MT = min(512, T)
    nsub = MT // 128
    P1 = kb.ps([128, 1024], F32, "P1")
    P2 = kb.ps([128, 1024], F32, "P2")
    psp = RPool(kb, 3, [128, 512], F32, "psr", ps=True)
    catp = RPool(kb, 2, [128, 8, MT], BF16, "cat")
    x4p = RPool(kb, 2, [128, nsub, 1024], F32, "x4")
    hTp = RPool(kb, 1, [128, 8, MT], BF16, "hT")
    cqp = RPool(kb, 1, [128, 8, MT], BF16, "cqT")
    cop = RPool(kb, 1, [128, 8, MT], BF16, "coT")
    ptp = RPool(kb, 4, [128, MT], BF16, "PT")
    rdp = RPool(kb, 2, [128, MT], F32, "rden")
    mxp = RPool(kb, 2, [128, 1024], F32, "mixed")
    xop = RPool(kb, 2, [128, 1024], F32, "xo")
    for m in range(T // MT):
        t0 = m * MT
        cat = catp.get()
        kb.dma('sp', cat.v, g.catT[:, t0:t0 + MT].rr("(c p) t -> p c t", p=128))
        x4 = x4p.get()
        kb.dma('act', x4.v, xsrc[t0:t0 + MT, :].rr("(s p) d -> p s d", p=128))
        hT = hTp.get()
        for s in range(nsub):
            ti = m * nsub + s
            ts_ = slice(s * 128, (s + 1) * 128)
            for half in range(2):
                hs = slice(half * 512, (half + 1) * 512)
                for k in range(4):
                    kb.mm(P1[:, hs], cat[:, k, ts_], WoA[:, k, hs], start=(k == 0), stop=(k == 3))
                for k in range(4):
                    kb.mm(P2[:, hs], cat[:, 4 + k, ts_], WoL[:, k, hs], start=(k == 0), stop=(k == 3))
            mx = mxp.get()
            kb.ts(mx.v, P1.v, rA[:, ti:ti + 1], None, op0=ALU.mult)
            kb.stt(mx.v, P2.v, rL[:, ti:ti + 1], mx.v, ALU.mult, ALU.add)
            st = c.stat.get()
            jk = c.junk.get()
            kb.act(jk.v, mx.v, AF.Square, accum_out=st[:, 0:1])
            rstd_from_ss(kb, st[:, 0:1], D)
            kb.stt(mx.v, mx.v, st[:, 0:1], GP1.v, ALU.mult, ALU.mult)
            kb.tt(x4[:, s, :], x4[:, s, :], mx.v, ALU.add, e='pool')
            norm_T(kb, c, x4[:, s, :], hT[:, :, ts_], ident)
        cqT = cqp.get()
        for fc in range(8):
            p = psp.get()
            for k in range(8):
                kb.mm(p[:, :MT], Wq[:, k, fc * 128:(fc + 1) * 128], hT[:, k, :], start=(k == 0), stop=(k == 7))
            kb.copy(cqT[:, fc, :], p[:, :MT], e=('dve' if fc % 2 else 'act'))
        coT = cop.get()
        for hx in range(4):
            PTs = []
            for mc in range(2):
                p = psp.get()
                for f2 in range(2):
                    kb.mm(p[:, :MT], ckT[:, 2 * hx + f2, mc * 128:(mc + 1) * 128], cqT[:, 2 * hx + f2, :],
                          start=(f2 == 0), stop=(f2 == 1))
                PT = ptp.get()
                kb.act(PT.v, p[:, :MT], AF.Exp)
                PTs.append(PT)
            p = psp.get()
            for mc in range(2):
                kb.mm(p[:, :MT], onesb.v, PTs[mc].v, start=(mc == 0), stop=(mc == 1))
            rden = rdp.get()
            kb.recip(rden.v, p[:, :MT])
            for dvc in range(2):
                p = psp.get()
                for mc in range(2):
                    kb.mm(p[:, :MT], cv[:, mc, hx * 256 + dvc * 128:hx * 256 + (dvc + 1) * 128], PTs[mc].v,
                          start=(mc == 0), stop=(mc == 1))
                kb.tt(coT[:, 2 * hx + dvc, :], p[:, :MT], rden.v, ALU.mult)
        for s in range(nsub):
            ts_ = slice(s * 128, (s + 1) * 128)
            for half in range(2):
                hs = slice(half * 512, (half + 1) * 512)
                for k in range(8):
                    kb.mm(P1[:, hs], coT[:, k, ts_], Wo[:, k, hs], start=(k == 0), stop=(k == 7))
            st = c.stat.get()
            jk = c.junk.get()
            kb.act(jk.v, P1.v, AF.Square, accum_out=st[:, 0:1])
            rstd_from_ss(kb, st[:, 0:1], D)
            xo = xop.get()
            kb.stt(xo.v, P1.v, st[:, 0:1], GP2.v, ALU.mult, ALU.mult)
            kb.tt(xo.v, xo.v, x4[:, s, :], ALU.add, e='pool')
            kb.dma('sp', g.xmid[t0 + s * 128:t0 + (s + 1) * 128, :], xo.v)
    phase_end(kb)


def phase_R2(kb, g, l, xdst):
    T, NT = g.T, g.NT
    phase_begin(kb)
    c = Ctx()
    c.wstage = RPool(kb, 2, [128, 1024], F32, "wst")
    c.dq = ['sp', 'act']
    c.dqi = 0
    c.cei = 0
    gc = kb.sb([128, NV], F32, "gc")
    kb.dma('sp', gc.v, g.gcols[l])
    ident = load_const_bf16(kb, c, g.ident.v, [128, 128], "ident")
    W1 = kb.sb([128, 8, 4096], BF16, "W1")
    W2 = kb.sb([128, 32, 1024], BF16, "W2")
    for k in range(8):
        for q in range(4):
            wload(kb, c, W1[:, k, q * 1024:(q + 1) * 1024], g.mlp_w1[l, k * 128:(k + 1) * 128, q * 1024:(q + 1) * 1024],
                  gc[:, 16 + k:17 + k])
    for k in range(32):
        wload(kb, c, W2[:, k, :], g.mlp_w2[l, k * 128:(k + 1) * 128, :])
    GP3 = kb.sb([128, 1024], F32, "GP3")
    kb.dma('sp', GP3.v, bcast_row(g.grows[l, 2:3, :]))
    c.stat = RPool(kb, 4, [128, 4], F32, "stat")
    c.junk = RPool(kb, 1, [128, 1024], F32, "junk")
    c.hb = RPool(kb, 2, [128, 1024], BF16, "hb")
    c.pT = RPool(kb, 1, [128, 1024], BF16, "pT", ps=True)
    MT = 128
    Py = kb.ps([128, 1024], F32, "Py")
    psp = RPool(kb, 4, [128, 512], F32, "psm", ps=True)
    x2p = RPool(kb, 2, [128, 1024], F32, "x2")
    hTp = RPool(kb, 2, [128, 8, MT], BF16, "hT")
    utp = RPool(kb, 2, [128, 32, MT], BF16, "uT")
    rlp = RPool(kb, 3, [128, MT], F32, "rl")
    xop = RPool(kb, 2, [128, 1024], F32, "xo")
    for m in range(T // MT):
        t0 = m * MT
        x2 = x2p.get()
        kb.dma('sp', x2.v, g.xmid[t0:t0 + MT, :])
        hT = hTp.get()
        norm_T(kb, c, x2.v, hT.v, ident)
        uT = utp.get()
        for fc in range(32):
            p = psp.get()
            for k in range(8):
                kb.mm(p[:, :MT], W1[:, k, fc * 128:(fc + 1) * 128], hT[:, k, :], start=(k == 0), stop=(k == 7))
            rl = rlp.get()
            kb.act(rl.v, p[:, :MT], AF.Relu)
            kb.tt(uT[:, fc, :], rl.v, rl.v, ALU.mult, e=('dve' if fc % 2 else 'pool'))
        for half in range(2):
            hs = slice(half * 512, (half + 1) * 512)
            for k in range(32):
                kb.mm(Py[:, hs], uT[:, k, :], W2[:, k, hs], start=(k == 0), stop=(k == 31))
        st = c.stat.get()
        jk = c.junk.get()
        kb.act(jk.v, Py.v, AF.Square, accum_out=st[:, 0:1])
        rstd_from_ss(kb, st[:, 0:1], D)
        xo = xop.get()
        kb.stt(xo.v, Py.v, st[:, 0:1], GP3.v, ALU.mult, ALU.mult)
        kb.tt(xo.v, xo.v, x2.v, ALU.add, e='pool')
        kb.dma('act', xdst[t0:t0 + MT, :], xo.v)
    phase_end(kb)


def t5_bucket_np(d):
    d = np.maximum(d, 0)
    df = np.maximum(d, 1).astype(np.float32)
    large = 16 + (np.log(df / np.float32(16)) / np.float32(math.log(8.0)) * np.float32(16)).astype(np.int32)
    large = np.minimum(large, 31)
    return np.where(d < 16, d, large)


def host_tables(rel_bias):
    rb = np.asarray(rel_bias, np.float32)
    q = np.arange(128)
    tabs = {}
    jp = np.arange(256)
    dist = q[:, None] + 64 * (128 - jp[None, :]) - 63
    bk = t5_bucket_np(dist)
    tc = rb[bk]
    tc = np.where((dist >= 0)[:, :, None], tc, np.float32(NEGM)).transpose(0, 2, 1)
    tabs["tc_tab"] = np.ascontiguousarray(tc, np.float32).reshape(128, 8 * 256)
    k = np.arange(128)
    dist = q[None, :] - k[:, None]
    dg = rb[t5_bucket_np(dist)]
    dg = np.where((dist >= 0)[:, :, None], dg, np.float32(NEGM)).transpose(0, 2, 1)
    tabs["dg_tab"] = np.ascontiguousarray(dg, np.float32).reshape(128, 8 * 128)
    dist = 128 + q[None, :] - k[:, None]
    sdg = rb[t5_bucket_np(dist)].transpose(0, 2, 1)
    tabs["sdg_tab"] = np.ascontiguousarray(sdg, np.float32).reshape(128, 8 * 128)
    c31 = np.broadcast_to(rb[31][None, :, None], (128, 8, 128))
    tabs["c31_tab"] = np.ascontiguousarray(c31, np.float32).reshape(128, 8 * 128)
    we = np.where(k[:, None] > q[None, :], np.float32(0), np.float32(NEGM))
    tabs["we_tab"] = np.ascontiguousarray(np.broadcast_to(we[:, None, :], (128, 4, 128)), np.float32).reshape(128, 512)
    rel = (jp[None, :] - 128) - (q[:, None] >= 64)
    addt = np.where(rel > 0, np.float32(-1e30), np.where(rel >= -1, np.float32(1e4), np.float32(0)))
    tabs["addt_tab"] = np.ascontiguousarray(addt, np.float32)
    tabs["rv0_tab"] = (q >= 63).astype(np.float32).reshape(128, 1)
    tabs["ident"] = np.eye(128, dtype=np.float32)
    bm = np.zeros((4, 4, 128), np.float32)
    for h in range(4):
        bm[h, h, :] = 1
    tabs["ones_tab"] = np.ones((128, 128), np.float32)
    return tabs


def host_layer_tables(inp, L):
    def col(v, nch):
        return np.asarray(v, np.float32)[:L].reshape(L, nch, 128).transpose(0, 2, 1)
    cols = [col(inp["ln_mix_pre"], 8), col(inp["ln_x_pre"], 8), col(inp["ln_mlp_pre"], 8), col(inp["ln_mem"], 8),
            col(inp["gn_attn"], 4), col(inp["gn_lru"], 4)]
    cw = np.asarray(inp["conv_w"], np.float32)[:L]
    cols.append(cw.reshape(L, 4, 4, 128).transpose(0, 3, 1, 2).reshape(L, 128, 16))
    for nm in ["conv_b", "lru_ba", "lru_bx", "lru_lambda"]:
        cols.append(col(inp[nm], 4))
    gcols = np.ascontiguousarray(np.concatenate(cols, axis=2), np.float32)
    assert gcols.shape == (L, 128, NV)
    grows = np.ascontiguousarray(np.stack([inp["ln_mix_post"][:L], inp["ln_x_post"][:L], inp["ln_mlp_post"][:L]], axis=1), np.float32)
    out = {"gcols": gcols, "grows": grows}
    for nm, key in [("pe2k", "cmp_pe_k"), ("pe2v", "cmp_pe_v")]:
        pe = np.asarray(inp[key], np.float32)[:L]
        out[nm] = np.ascontiguousarray(pe.reshape(L, 32, 2, 64).transpose(0, 2, 3, 1).reshape(L, 128, 32))
    return out


WNAMES = [("w_in", [1024, 2328]), ("cmp_w_k", [4096, 64]), ("cmp_w_v", [4096, 64]), ("lru_wa", [8, 64, 64]),
          ("lru_wx", [8, 64, 64]), ("w_out", [1024, 1024]), ("xq", [1024, 1024]), ("xkv", [1024, 2048]),
          ("xo", [1024, 1024]), ("mlp_w1", [1024, 4096]), ("mlp_w2", [4096, 1024])]
TABS = [("tc_tab", [128, 2048]), ("dg_tab", [128, 1024]), ("sdg_tab", [128, 1024]), ("c31_tab", [128, 1024]),
        ("we_tab", [128, 512]), ("addt_tab", [128, 256]), ("rv0_tab", [128, 1]), ("ident", [128, 128]),
        ("ones_tab", [128, 128])]


def build(T, L, debug=False, stop_after=None):
    nc = bass.Bass("TRN2", target_bir_lowering=False)
    es = ExitStack()
    with es:
        kb = KB(nc, es)
        g = Ctx()
        g.T, g.NT, g.NB, g.L = T, T // 128, T // 64, L
        g.x = kb.dram("x", [T, D], F32, kind="ExternalInput")
        g.mem = kb.dram("mem", [256, D], F32, kind="ExternalInput")
        for nm, shp in WNAMES:
            setattr(g, nm, kb.dram(nm, [L] + shp, F32, kind="ExternalInput"))
        for nm, shp in TABS:
            setattr(g, nm, kb.dram(nm, shp, F32, kind="ExternalInput"))
        g.gcols = kb.dram("gcols", [L, 128, NV], F32, kind="ExternalInput")
        g.grows = kb.dram("grows", [L, 3, D], F32, kind="ExternalInput")
        g.pe2k = kb.dram("pe2k", [L, 128, 32], F32, kind="ExternalInput")
        g.pe2v = kb.dram("pe2v", [L, 128, 32], F32, kind="ExternalInput")
        g.out = kb.dram("out", [T, D], F32, kind="ExternalOutput")
        sk = "ExternalOutput" if debug else "Internal"
        g.zTb = kb.dram("zTb", [1024, T], BF16, kind=sk)
        g.zTf = kb.dram("zTf", [1024, T], F32, kind=sk)
        g.zV = kb.dram("zV", [T, 280], F32, kind=sk)
        g.catT = kb.dram("catT", [1024, T], BF16, kind=sk)
        g.ssq = kb.dram("ssq", [128, 3 * g.NT], F32, kind=sk)
        g.ckT = kb.dram("ckT", [1024, 256], BF16, kind=sk)
        g.cv = kb.dram("cv", [256, 1024], BF16, kind=sk)
        g.xmid = kb.dram("xmid", [T, D], F32, kind=sk)
        g.xres = kb.dram("xres", [T, D], F32, kind=sk)
        kb.cur_phase = 0
        phases = []
        for l in range(L):
            xsrc = g.x if l == 0 else g.xres
            xdst = g.out if l == L - 1 else g.xres
            phases += [("M1", lambda l=l, xsrc=xsrc: phase_M1(kb, g, l, xsrc)),
                       ("ATT", lambda l=l: phase_ATT(kb, g, l)),
                       ("LRU", lambda l=l: phase_LRU(kb, g, l)),
                       ("XKV", lambda l=l: phase_XKV(kb, g, l)),
                       ("R1", lambda l=l, xsrc=xsrc: phase_R1(kb, g, l, xsrc)),
                       ("R2", lambda l=l, xdst=xdst: phase_R2(kb, g, l, xdst))]
        for i, (nm, fn) in enumerate(phases):
            kb.cur_phase = i + 1
            fn()
            if stop_after is not None and i + 1 >= stop_after:
                break
    return nc


def make_inmaps(inputs, T, L, ncores):
    tabs = host_tables(inputs["rel_bias"])
    lt = host_layer_tables(inputs, L)
    shared = {}
    for nm, shp in WNAMES:
        shared[nm] = np.ascontiguousarray(np.asarray(inputs[nm], np.float32)[:L].reshape([L] + shp))
    shared.update(tabs)
    shared.update(lt)
    maps = []
    B = inputs["x"].shape[0]
    for c in range(ncores):
        b = c % B
        m = dict(shared)
        m["x"] = np.ascontiguousarray(np.asarray(inputs["x"], np.float32)[b, :T])
        m["mem"] = np.ascontiguousarray(np.asarray(inputs["mem"], np.float32)[b])
        maps.append(m)
    return maps


_NC_CACHE = {}


def kernel(**inputs):
    T, L = 8192, 2
    B = inputs["x"].shape[0]
    key = (T, L)
    if key not in _NC_CACHE:
        _NC_CACHE[key] = build(T, L)
    nc = _NC_CACHE[key]
    ncores = 8
    maps = make_inmaps(inputs, T, L, ncores)
    res = run_bass_kernel_spmd(nc, maps, core_ids=list(range(ncores)))
    out = np.stack([np.asarray(res.results[b]["out"], np.float32) for b in range(B)], axis=0)
    return out
```

```python
import numpy as np
from contextlib import ExitStack
import concourse.bass as bass
import concourse.mybir as mybir
from concourse.bass_utils import run_bass_kernel_spmd

F32 = mybir.dt.float32
BF16 = mybir.dt.bfloat16
AF = mybir.ActivationFunctionType
ALU = mybir.AluOpType
AX = mybir.AxisListType


class TL:
    def __init__(self, sem, step):
        self.sem = sem
        self.step = step
        self.val = 0


class Buf:
    def __init__(self, t, name, space):
        self.t = t
        self.name = name
        self.space = space
        self.last_w = None
        self.readers = {}
        self.dtl = None

    def __getitem__(self, idx):
        return View(self, self.t[idx])

    @property
    def v(self):
        return View(self, self.t[:])


class View:
    def __init__(self, buf, ap):
        self.buf = buf
        self.ap = ap

    def __getitem__(self, idx):
        return View(self.buf, self.ap[idx])

    def rr(self, pat, **kw):
        return View(self.buf, self.ap.rearrange(pat, **kw))

    def bc(self, shape):
        return View(self.buf, self.ap.broadcast_to(shape))


def _bufs(views):
    out = []
    for v in views:
        if v is None:
            continue
        if isinstance(v, Buf):
            out.append(v)
        elif isinstance(v, View):
            out.append(v.buf)
    return out


def A(v):
    if isinstance(v, View):
        return v.ap
    if isinstance(v, Buf):
        return v.t[:]
    return v


ENGS = ['pe', 'act', 'dve', 'pool', 'sp']


class KB:
    def __init__(self, nc, es):
        self.nc = nc
        self.es = es
        self.tl = {}
        for e in ENGS:
            self.tl[e] = TL(es.enter_context(nc.semaphore("s_" + e)), 1)
        self.prog = {e: [] for e in ENGS}
        self.seen = {e: {} for e in ENGS}
        self.dtls = []
        self.free_dtls = []
        self.phase_dtls = []
        self.root_es = es
        self.nbuf = 0
        self.cur_phase = 0

    def sb(self, shape, dtype, name=None):
        self.nbuf += 1
        name = (name or "sb") + "_%d" % self.nbuf
        t = self.es.enter_context(self.nc.sbuf_tensor(name, list(shape), dtype))
        b = Buf(t, name, 'sb')
        b.phase = self.cur_phase
        return b

    def ps(self, shape, dtype, name=None):
        self.nbuf += 1
        name = (name or "ps") + "_%d" % self.nbuf
        t = self.es.enter_context(self.nc.psum_tensor(name, list(shape), dtype))
        b = Buf(t, name, 'ps')
        b.phase = self.cur_phase
        return b

    def dram(self, name, shape, dtype, kind="Internal"):
        t = self.nc.dram_tensor(name, list(shape), dtype, kind=kind)
        return Buf(t, name, 'dram')

    def sub(self, buf, idx, name=None):
        b = Buf(None, name or (buf.name + "_sub"), buf.space)
        b.t = _Sub(buf.t[idx])
        return b

    def _waits(self, e, reads, writes, own_tl=None):
        need = {}

        def add(tl, val):
            if need.get(tl, 0) < val:
                need[tl] = val
        mytl = self.tl[e]
        for b in reads:
            if b.last_w:
                add(*b.last_w)
        for b in writes:
            if b.last_w:
                tl, val = b.last_w
                if (tl is mytl and e == 'pe') or (own_tl is not None and tl is own_tl):
                    pass
                else:
                    add(tl, val)
            for tl, val in b.readers.items():
                if tl is mytl and e == 'pe':
                    continue
                add(tl, val)
        waits = []
        for tl, val in need.items():
            if tl.step == 16:
                val = tl.val
            if self.seen[e].get(tl, 0) >= val:
                continue
            self.seen[e][tl] = val
            waits.append((tl, val))
        return waits

    def op(self, e, fn, reads, writes):
        reads = _bufs(reads)
        writes = _bufs(writes)
        waits = self._waits(e, reads, writes)
        tl = self.tl[e]
        tl.val += 1
        v = tl.val
        self.prog[e].append((waits, fn, tl.sem, 1))
        for b in reads:
            b.readers[tl] = v
        for b in writes:
            b.last_w = (tl, v)
            b.readers = {}

    def dma(self, e, out, in_, **kw):
        ob = out.buf if isinstance(out, View) else out
        ib = in_.buf if isinstance(in_, View) else in_
        side = ob if ob.space != 'dram' else ib
        if side.dtl is None:
            if self.free_dtls:
                side.dtl = self.free_dtls.pop()
            else:
                side.dtl = TL(self.root_es.enter_context(self.nc.semaphore("d%d" % len(self.dtls))), 16)
                self.dtls.append(side.dtl)
            if getattr(side, 'phase', 0) == self.cur_phase and self.cur_phase > 0:
                self.phase_dtls.append(side.dtl)
        dtl = side.dtl
        waits = self._waits(e, [ib], [ob], own_tl=dtl)
        dtl.val += 16
        v = dtl.val
        oa, ia = A(out), A(in_)
        self.prog[e].append((waits, lambda eng: eng.dma_start(out=oa, in_=ia, **kw), dtl.sem, 16))
        ib.readers[dtl] = v
        ob.last_w = (dtl, v)
        ob.readers = {}

    def barrier(self):
        for e in ENGS:
            waits = []
            for tl in list(self.tl.values()) + self.dtls:
                if tl.val > 0 and self.seen[e].get(tl, 0) < tl.val and tl is not None:
                    self.seen[e][tl] = tl.val
                    waits.append((tl, tl.val))
            self.prog[e].append((waits, None, None, 0))

    def emit_block(self):
        nc = self.nc
        prog = self.prog
        self.prog = {e: [] for e in ENGS}
        self.free_dtls.extend(self.phase_dtls)
        self.phase_dtls = []
        with nc.Block() as block:
            def run(eng, lst):
                for waits, fn, sem, inc in lst:
                    for tl, val in waits:
                        eng.wait_ge(tl.sem, val)
                    if fn is not None:
                        ins = fn(eng)
                        ins.then_inc(sem, inc)

            @block.tensor
            def _(eng):
                run(eng, prog['pe'])

            @block.scalar
            def _(eng):
                run(eng, prog['act'])

            @block.vector
            def _(eng):
                run(eng, prog['dve'])

            @block.gpsimd
            def _(eng):
                run(eng, prog['pool'])

            @block.sync
            def _(eng):
                run(eng, prog['sp'])

    def mm(self, out, lhsT, rhs, start, stop=True, sgc=False, **kw):
        oa, la, ra = A(out), A(lhsT), A(rhs)
        if sgc:
            kw['skip_group_check'] = True
        self.op('pe', lambda eng: eng.matmul(oa, la, ra, start=start, stop=stop, **kw),
                [lhsT, rhs], [out])

    def tr(self, out, in_, ident):
        oa, ia, da = A(out), A(in_), A(ident)
        self.op('pe', lambda eng: eng.transpose(oa, ia, da), [in_, ident], [out])

    def act(self, out, in_, func, bias=None, scale=None, accum_out=None, e='act'):
        oa, ia = A(out), A(in_)
        kw = {}
        rd = [in_]
        if bias is not None:
            kw['bias'] = A(bias)
            rd.append(bias)
        if scale is not None:
            kw['scale'] = A(scale)
            rd.append(scale)
        wr = [out]
        if accum_out is not None:
            kw['accum_out'] = A(accum_out)
            wr.append(accum_out)
        self.op(e, lambda eng: eng.activation(oa, ia, func, **kw), rd, wr)

    def tt(self, out, in0, in1, op, e='dve'):
        oa, a0, a1 = A(out), A(in0), A(in1)
        self.op(e, lambda eng: eng.tensor_tensor(oa, a0, a1, op), [in0, in1], [out])

    def ts(self, out, in0, s1, s2=None, op0=None, op1=None, e='dve', accum_out=None):
        oa, a0 = A(out), A(in0)
        rd = [in0, s1, s2]
        kw = {}
        wr = [out]
        if accum_out is not None:
            kw['accum_out'] = A(accum_out)
            wr.append(accum_out)
        a1, a2 = A(s1), A(s2)
        if op1 is None:
            self.op(e, lambda eng: eng.tensor_scalar(oa, a0, a1, None, op0, **kw), rd, wr)
        else:
            self.op(e, lambda eng: eng.tensor_scalar(oa, a0, a1, a2, op0, op1, **kw), rd, wr)

    def stt(self, out, in0, scalar, in1, op0, op1, e='dve'):
        oa, a0, sc, a1 = A(out), A(in0), A(scalar), A(in1)
        self.op(e, lambda eng: eng.scalar_tensor_tensor(oa, a0, sc, a1, op0, op1), [in0, scalar, in1], [out])

    def copy(self, out, in_, e='dve'):
        oa, ia = A(out), A(in_)
        if e == 'act':
            self.op(e, lambda eng: eng.copy(oa, ia), [in_], [out])
        else:
            self.op(e, lambda eng: eng.tensor_copy(oa, ia), [in_], [out])

    def memset(self, out, val, e='pool'):
        oa = A(out)
        self.op(e, lambda eng: eng.memset(oa, val), [], [out])

    def recip(self, out, in_, e='dve'):
        oa, ia = A(out), A(in_)
        self.op(e, lambda eng: eng.reciprocal(oa, ia), [in_], [out])


class _Sub:
    def __init__(self, ap):
        self.ap = ap

    def __getitem__(self, idx):
        return self.ap[idx]

import math

EPS = 1e-6
NEGM = -30000.0
D = 1024
NV = 72
O_Q, O_KC, O_VC, O_KS, O_VS, O_KW, O_VW, O_GT, O_XG, O_XR = 0, 512, 640, 768, 896, 1024, 1152, 1280, 1304, 1816


class RPool:
    def __init__(self, kb, n, shape, dtype, name, ps=False):
        mk = kb.ps if ps else kb.sb
        self.bufs = [mk(shape, dtype, "%s%d" % (name, i)) for i in range(n)]
        self.i = 0

    def get(self):
        b = self.bufs[self.i % len(self.bufs)]
        self.i += 1
        return b


def phase_begin(kb):
    kb.pes = ExitStack()
    kb.pes.__enter__()
    kb.outer_es = kb.es
    kb.es = kb.pes
    kb.phase_dtls = []


def phase_end(kb):
    kb.barrier()
    kb.emit_block()
    kb.es = kb.outer_es
    kb.pes.__exit__(None, None, None)


class Ctx:
    pass


def wload(kb, c, dst, src, gcol=None, mul=None):
    n = src.ap.shape[-1]
    st = c.wstage.get()
    q = c.dq[c.dqi % 2]
    c.dqi += 1
    kb.dma(q, st[:, :n], src)
    e = ['dve', 'pool'][c.cei % 2]
    c.cei += 1
    if gcol is not None and mul is not None:
        kb.ts(dst, st[:, :n], gcol, float(mul), op0=ALU.mult, op1=ALU.mult, e=e)
    elif gcol is not None:
        kb.ts(dst, st[:, :n], gcol, None, op0=ALU.mult, e=e)
    elif mul is not None:
        kb.ts(dst, st[:, :n], float(mul), None, op0=ALU.mult, e=e)
    else:
        kb.copy(dst, st[:, :n], e=e)


def rstd_from_ss(kb, ss, n):
    kb.ts(ss, ss, 1.0 / n, EPS, op0=ALU.mult, op1=ALU.add)
    kb.act(ss, ss, AF.Sqrt)
    kb.recip(ss, ss)


def norm_T(kb, c, xv, hT_dst, ident):
    st = c.stat.get()
    junk = c.junk.get()
    kb.act(junk.v, xv, AF.Square, accum_out=st[:, 0:1])
    rstd_from_ss(kb, st[:, 0:1], D)
    hb = c.hb.get()
    kb.ts(hb.v, xv, st[:, 0:1], None, op0=ALU.mult)
    pT = c.pT.get()
    for k in range(8):
        kb.tr(pT[:, k * 128:(k + 1) * 128], hb[:, k * 128:(k + 1) * 128], ident.v)
    kb.copy(hT_dst, pT.v.rr("p (c t) -> p c t", c=8), e='act')


def phase_M1(kb, g, l, xsrc):
    T, NT = g.T, g.NT
    phase_begin(kb)
    c = Ctx()
    c.wstage = RPool(kb, 3, [128, 2048], F32, "wst")
    c.dq = ['sp', 'act']
    c.dqi = 0
    c.cei = 0
    gc = kb.sb([128, NV], F32, "gc")
    kb.dma('sp', gc.v, g.gcols[l])
    WF = kb.sb([128, 8, 2048], BF16, "WF")
    WT = kb.sb([128, 8, 280], BF16, "WT")
    w = g.w_in
    for k in range(8):
        rows = slice(k * 128, (k + 1) * 128)
        gk = gc[:, k:k + 1]
        wload(kb, c, WF[:, k, 0:512], w[l, rows, O_Q:O_Q + 512], gk, 0.125)
        wload(kb, c, WF[:, k, 512:768], w[l, rows, O_KC:O_KC + 256], gk)
        wload(kb, c, WF[:, k, 768:896], w[l, rows, O_KS:O_KS + 128], gk)
        wload(kb, c, WF[:, k, 896:1024], w[l, rows, O_KW:O_KW + 128], gk)
        wload(kb, c, WF[:, k, 1024:2048], w[l, rows, O_XG:O_XG + 1024], gk)
        wload(kb, c, WT[:, k, 0:128], w[l, rows, O_VS:O_VS + 128], gk)
        wload(kb, c, WT[:, k, 128:256], w[l, rows, O_VW:O_VW + 128], gk)
        wload(kb, c, WT[:, k, 256:280], w[l, rows, O_GT:O_GT + 24], gk)
    ident = kb.sb([128, 128], BF16, "identb")
    idf = kb.sb([128, 128], F32, "identf")
    kb.dma('sp', idf.v, g.ident.v)
    kb.copy(ident.v, idf.v)
    c.stat = RPool(kb, 4, [128, 4], F32, "stat")
    c.junk = RPool(kb, 2, [128, 1024], F32, "junk")
    c.hb = RPool(kb, 2, [128, 1024], BF16, "hb")
    c.pT = RPool(kb, 1, [128, 1024], BF16, "pT", ps=True)
    xp = RPool(kb, 3, [128, 1024], F32, "xin")
    hTp = RPool(kb, 2, [128, 8, 512], BF16, "hT")
    ptm = RPool(kb, 1, [128, 512], F32, "ptm", ps=True)
    pfm = RPool(kb, 3, [128, 512], F32, "pfm", ps=True)
    vst = RPool(kb, 2, [128, 280], F32, "vst")
    fb = RPool(kb, 3, [128, 512], BF16, "fb")
    ff = RPool(kb, 3, [128, 512], F32, "ff")
    MT = min(512, T)
    nsub = MT // 128
    ei = 0
    for m in range(T // MT):
        hT = hTp.get()
        for s in range(nsub):
            t0 = m * MT + s * 128
            xs = xp.get()
            kb.dma('sp', xs.v, xsrc[t0:t0 + 128, :])
            norm_T(kb, c, xs.v, hT[:, :, s * 128:(s + 1) * 128], ident)
            pt = ptm.get()
            for k in range(8):
                kb.mm(pt[:, 0:280], hT[:, k, s * 128:(s + 1) * 128], WT[:, k, :], start=(k == 0), stop=(k == 7))
            vs = vst.get()
            kb.copy(vs.v, pt[:, 0:280], e='dve')
            kb.dma('sp', g.zV[t0:t0 + 128, :], vs.v)
        for fc in range(16):
            pf = pfm.get()
            for k in range(8):
                kb.mm(pf[:, :MT], WF[:, k, fc * 128:(fc + 1) * 128], hT[:, k, :MT], start=(k == 0), stop=(k == 7))
            e = ['act', 'dve'][ei % 2]
            ei += 1
            if fc < 8:
                o = fb.get()
                kb.copy(o[:, :MT], pf[:, :MT], e=e)
                kb.dma('act' if fc % 2 else 'sp', g.zTb[fc * 128:(fc + 1) * 128, m * MT:(m + 1) * MT], o[:, :MT])
            else:
                o = ff.get()
                kb.copy(o[:, :MT], pf[:, :MT], e=e)
                kb.dma('act' if fc % 2 else 'sp', g.zTf[(fc - 8) * 128:(fc - 7) * 128, m * MT:(m + 1) * MT], o[:, :MT])
    phase_end(kb)


def load_const_bf16(kb, c, dram_view, shape, name):
    st = kb.sb(shape, F32, name + "_f")
    kb.dma('sp', st.v, dram_view)
    b = kb.sb(shape, BF16, name)
    kb.copy(b.v, st.v, e='pool')
    return b


def phase_ATT(kb, g, l):
    T, NT, NB = g.T, g.NT, g.NB
    phase_begin(kb)
    c = Ctx()
    c.wstage = RPool(kb, 1, [128, 2048], F32, "wst")
    c.dq = ['sp', 'act']
    c.dqi = 0
    c.cei = 0
    ident = load_const_bf16(kb, c, g.ident.v, [128, 128], "ident")
    onesb = load_const_bf16(kb, c, g.ones_tab.v, [128, 128], "onesb")
    TCt = kb.sb([128, 8, 256], F32, "TCt")
    kb.dma('sp', TCt.v.rr("p h j -> p (h j)"), g.tc_tab.v)
    C31 = kb.sb([128, 8, 128], F32, "C31")
    kb.dma('act', C31.v.rr("p h j -> p (h j)"), g.c31_tab.v)
    DGs = kb.sb([128, 8, 128], F32, "DGs")
    kb.dma('sp', DGs.v.rr("p h j -> p (h j)"), g.dg_tab.v)
    kb.tt(DGs.v, DGs.v, C31.v, ALU.subtract, e='pool')
    SDGs = kb.sb([128, 8, 128], F32, "SDGs")
    kb.dma('act', SDGs.v.rr("p h j -> p (h j)"), g.sdg_tab.v)
    kb.tt(SDGs.v, SDGs.v, C31.v, ALU.subtract, e='pool')
    WEt = kb.sb([128, 4, 128], F32, "WEt")
    kb.dma('sp', WEt.v.rr("p h j -> p (h j)"), g.we_tab.v)
    ADDT = kb.sb([128, 256], F32, "ADDT")
    kb.dma('sp', ADDT.v, g.addt_tab.v)
    RV0 = kb.sb([128, 1], F32, "RV0")
    kb.dma('sp', RV0.v, g.rv0_tab.v)
    EX = kb.sb([128, T], BF16, "EX")
    KC2 = kb.sb([128, T], BF16, "KC2")
    VC2 = kb.sb([128, T], BF16, "VC2")
    EXa, EXb = KC2, VC2
    kb.memset(EXa.v, 1.0, e='pool')
    a0, a1, a2 = A(EXa.v), A(EXb.v), A(EX.v)
    kb.op('pool', lambda eng: eng.affine_select(a1, a0, [[1, T]], ALU.is_ge, 0.0, base=0, channel_multiplier=-64),
          [EXa], [EXb])
    kb.op('pool', lambda eng: eng.affine_select(a2, a1, [[-1, T]], ALU.is_ge, 0.0, base=63, channel_multiplier=64),
          [EXb], [EX])
    ssa = kb.sb([128, 2 * NT], F32, "ssa")
    LV = 9
    if LV <= 1:
        phase_end(kb)
        return

    KsT = kb.sb([64, T], BF16, "KsT")
    KwT = kb.sb([64, T], BF16, "KwT")
    VsA = kb.sb([128, NT, 65], BF16, "VsA")
    VwA = kb.sb([128, NT, 65], BF16, "VwA")
    Wck = kb.sb([128, 32, 128], BF16, "Wck")
    Wcv = kb.sb([128, 32, 64], BF16, "Wcv")
    pek = kb.sb([128, 32], BF16, "pek")
    pev = kb.sb([128, 32], BF16, "pev")
    pef = kb.sb([128, 64], F32, "pef")
    ckc = kb.sb([128, 1], F32, "ckc")
    cvr = kb.sb([1, 64], BF16, "cvr")
    KcT = kb.sb([128, NB], BF16, "KcT")
    Vc = kb.sb([128, 64], BF16, "Vc")
    vstage = RPool(kb, 2, [128, 16, 64], F32, "vstg")
    bankA = kb.ps([128, 512], F32, "bankA")
    bankT = kb.ps([128, 1024], BF16, "bankT")
    bankOc = kb.ps([128, 512], F32, "bankOc")
    stp = RPool(kb, 3, [128, 512], F32, "ST", ps=True)
    bOs = kb.ps([128, 512], F32, "bOs")
    bOw = kb.ps([128, 512], F32, "bOw")
    qap = RPool(kb, 3, [64, 4, 128], BF16, "Q4")
    gtp = RPool(kb, 2, [128, 12], F32, "GT")
    gsp = RPool(kb, 2, [128, 12], F32, "GS")
    scp = RPool(kb, 2, [128, 4, NB], F32, "sC")
    ecp = RPool(kb, 2, [128, 4, NB], F32, "eC")
    ecbp = RPool(kb, 2, [128, 4, NB], BF16, "eCb")
    ectp = RPool(kb, 2, [128, 4, 128], BF16, "eCT")
    stat = RPool(kb, 3, [128, 48], F32, "st")
    impp = RPool(kb, 2, [128, NB], F32, "imp")
    selp = RPool(kb, 2, [128, NB], F32, "sel")
    sel2p = RPool(kb, 2, [128, NB], F32, "sel2")
    selmp = RPool(kb, 2, [128, NB], BF16, "selm")
    negp = RPool(kb, 2, [128, 4, 128], BF16, "negm")
    tmpp = RPool(kb, 2, [128, 4, 128], F32, "tmpl")
    pp = RPool(kb, 4, [128, 4, 128], BF16, "P")
    attp = RPool(kb, 2, [128, 4, 64], F32, "att")
    attbp = RPool(kb, 2, [128, 256], BF16, "attb")
    attTp = RPool(kb, 2, [128, 2, 128], BF16, "attT")
    junkp = RPool(kb, 2, [128, 256], F32, "junk")
    ocp = RPool(kb, 2, [128, 4, 64], F32, "Oc")
    osp = RPool(kb, 4, [65, 512], F32, "OsT")
    identf = kb.sb([128, 128], F32, "identf2")
    kb.dma('sp', identf.v, g.ident.v)

    kb.dma('sp', pef[:, 0:32], g.pe2k[l])
    kb.dma('sp', pef[:, 32:64], g.pe2v[l])
    kb.copy(pek.v, pef[:, 0:32], e='pool')
    kb.copy(pev.v, pef[:, 32:64], e='pool')
    st = c.wstage.get()
    kb.dma('sp', st.v.rr("p (c e) -> p c e", e=64), g.cmp_w_k[l].rr("(c p) e -> p c e", p=128))
    kb.copy(Wck[:, :, 0:64], st.v.rr("p (c e) -> p c e", e=64), e='dve')
    kb.copy(Wck[:, :, 64:128], st.v.rr("p (c e) -> p c e", e=64), e='pool')
    st = c.wstage.get()
    kb.dma('act', st.v.rr("p (c e) -> p c e", e=64), g.cmp_w_v[l].rr("(c p) e -> p c e", p=128))
    kb.copy(Wcv.v, st.v.rr("p (c e) -> p c e", e=64), e='dve')

    for g_ in range(2):
        kb.dma('sp', KsT[0:64, :], g.zTb[768 + g_ * 64:768 + g_ * 64 + 64, :])
        kb.dma('act', KwT[0:64, :], g.zTb[896 + g_ * 64:896 + g_ * 64 + 64, :])
        kb.dma('sp', KC2[0:64, :], g.zTb[512 + g_ * 64:512 + g_ * 64 + 64, :])
        kb.dma('sp', KC2[64:128, 0:T - 1], g.zTb[512 + g_ * 64:512 + g_ * 64 + 64, 1:T])
        kb.dma('act', VC2[0:64, :], g.zTb[640 + g_ * 64:640 + g_ * 64 + 64, :])
        kb.dma('act', VC2[64:128, 0:T - 1], g.zTb[640 + g_ * 64:640 + g_ * 64 + 64, 1:T])
        for (VA, coff) in ((VsA, g_ * 64), (VwA, 128 + g_ * 64)):
            kb.memset(VA[:, :, 64:65], 1.0, e='pool')
            for j0 in range(0, NT, 16):
                nj = min(16, NT - j0)
                vs = vstage.get()
                kb.dma('sp', vs[:, :nj, :], g.zV[j0 * 128:(j0 + nj) * 128, coff:coff + 64].rr("(j p) d -> p j d", p=128))
                kb.copy(VA[:, j0:j0 + nj, 0:64], vs[:, :nj, :], e='dve')
        if LV <= 2:
            continue
        KC2v = KC2.v.rr("p (n r) -> p n r", r=64)
        VC2v = VC2.v.rr("p (n r) -> p n r", r=64)
        for cc in range(32):
            kb.mm(bankA[:, 0:1], Wck[:, cc, :], pek[:, cc:cc + 1], start=(cc == 0), stop=(cc == 31))
        kb.copy(ckc.v, bankA[:, 0:1], e='dve')
        for cc in range(32):
            kb.mm(bankOc[:, 0:NB], Wck[:, cc, :], KC2v[:, :, 2 * cc], start=(cc == 0), stop=(cc == 31))
        kb.ts(KcT.v, bankOc[:, 0:NB], ckc.v, None, op0=ALU.add)
        for cc in range(32):
            kb.mm(bankA[0:1, 0:64], pev[:, cc:cc + 1], Wcv[:, cc, :], start=(cc == 0), stop=(cc == 31))
        kb.copy(cvr.v, bankA[0:1, 0:64], e='dve')
        for cc in range(32):
            kb.mm(bankOc[:NB, 0:64], VC2v[:, :, 2 * cc], Wcv[:, cc, :], start=(cc == 0), stop=False)
        kb.mm(bankOc[:NB, 0:64], onesb[0:1, :NB], cvr[0:1, :], start=False, stop=True)
        kb.copy(Vc[:NB, :], bankOc[:NB, 0:64], e='dve')

        if LV <= 3:
            continue
        hs0 = slice(0, 64)

        def prologue(i, g_=g_):
            S = Ctx()
            S.i = i

            def chunkA():
                Q4 = qap.get()
                r0 = g_ * 256
                kb.dma('sp', Q4[0:64, :, :], g.zTb[r0:r0 + 256, i * 128:(i + 1) * 128].rr("(h d) t -> d h t", d=64))
                S.Qh = [Q4[0:64, h, :] for h in range(4)]
                S.Q4 = Q4
                GT = gtp.get()
                kb.dma('act', GT.v, g.zV[i * 128:(i + 1) * 128, 256 + g_ * 12:256 + g_ * 12 + 12])
                GS = gsp.get()
                kb.act(GS.v, GT.v, AF.Exp, scale=-1.0)
                kb.ts(GS.v, GS.v, 1.0, None, op0=ALU.add)
                kb.recip(GS.v, GS.v)
                S.G3 = GS.v.rr("p (h k) -> p h k", k=3)
                s_ = stat.get()
                S.s_ = s_
                mx, nmx, sumC, rC = s_[:, 0:4], s_[:, 4:8], s_[:, 8:12], s_[:, 12:16]
                S.rC = rC
                m1, m2, thr = s_[:, 36:44], s_[:, 36:44], s_[:, 44:45]
                psC = bankA.v.rr("p (h n) -> p h n", h=4)[:, :, 0:NB]
                for h in range(4):
                    kb.mm(psC[:, h, :], S.Qh[h], KcT[hs0, :], start=(h == 0), stop=(h == 3), sgc=True)
                sC = scp.get()
                kb.tt(sC.v, psC, TCt[:, g_ * 4:(g_ + 1) * 4, 128 - 2 * i:128 - 2 * i + NB], ALU.add)
                sv, mv = A(sC.v), A(mx)
                kb.op('dve', lambda eng, sv=sv, mv=mv: eng.tensor_reduce(mv, sv, AX.X, ALU.max), [sC], [s_])
                kb.ts(nmx, mx, -1.0, None, op0=ALU.mult)
                eC = ecp.get()
                for h in range(4):
                    kb.act(eC[:, h, :], sC[:, h, :], AF.Exp, bias=nmx[:, h:h + 1], accum_out=sumC[:, h:h + 1])
                kb.recip(rC, sumC)
                if i == 0:
                    kb.ts(rC, rC, RV0.v, None, op0=ALU.mult)
                imp = impp.get()
                kb.ts(imp.v, eC[:, 0, :], rC[:, 0:1], None, op0=ALU.mult)
                for h in range(1, 4):
                    kb.stt(imp.v, eC[:, h, :], rC[:, h:h + 1], imp.v, ALU.mult, ALU.add)
                S.eCb = ecbp.get()
                kb.copy(S.eCb.v, eC.v, e='pool')
                sel = selp.get()
                kb.tt(sel.v, imp.v, ADDT[:, 128 - 2 * i:128 - 2 * i + NB], ALU.add)
                kb.memset(sel[:, 0:1], 1e4, e='dve')
                sa, m1a = A(sel.v), A(m1)
                kb.op('dve', lambda eng, sa=sa, m1a=m1a: eng.max(m1a, sa), [sel], [s_])
                if NB > 16:
                    sel2 = sel2p.get()
                    s2a = A(sel2.v)
                    kb.op('dve', lambda eng, sa=sa, m1a=m1a, s2a=s2a: eng.match_replace(s2a, m1a, sa, -1e30), [sel, s_], [sel2])
                    kb.op('dve', lambda eng, s2a=s2a, m1a=m1a: eng.max(m1a, s2a), [sel2], [s_])
                    kb.ts(thr, m2[:, 7:8], -5e29, None, op0=ALU.max)
                else:
                    kb.memset(thr, -5e29, e='dve')
                S.selm = selmp.get()
                kb.ts(S.selm.v, sel.v, thr, None, op0=ALU.is_ge)

            def chunkB():
                for h in range(4):
                    kb.tr(bankT[:NB, h * 128:(h + 1) * 128], S.eCb[:, h, :], ident.v)
                kb.tr(bankT[:NB, 512:640], S.selm.v, ident.v)
                S.eCT = ectp.get()
                kb.copy(S.eCT[:NB].rr("p h q -> p (h q)"), bankT[:NB, 0:512], e='dve')
                S.negm = negp.get()
                for h in range(4):
                    kb.ts(S.negm[:NB, h, :], bankT[:NB, 512:640], -1.0, 30000.0, op0=ALU.add, op1=ALU.mult,
                          e='dve')

            def chunkC():
                psOc = bankOc.v.rr("p (h d) -> p h d", h=4)[:, :, 0:64]
                for h in range(4):
                    kb.mm(psOc[:, h, :], S.eCT[:NB, h, :], Vc[:NB, :], start=(h == 0), stop=(h == 3), sgc=True)
                S.Oc = ocp.get()
                kb.copy(S.Oc.v, psOc, e='dve')

            return S, [chunkA, chunkB, chunkC]

        def dense_and_epilogue(S, hooks, g_=g_):
            i = S.i
            Os = bOs.v[:, 0:260].rr("p (h d) -> p h d", h=4)
            Ow = bOw.v[:, 0:260].rr("p (h d) -> p h d", h=4)
            tasks = [(0, j) for j in range(0, i + 1)] + [(1, j) for j in range(max(0, i - 4), i + 1)]
            firstj = {0: 0, 1: max(0, i - 4)}
            Pbuf = {}
            DEP = 2

            def emit_S(t):
                br, j = tasks[t]
                KT = KsT if br == 0 else KwT
                STb = stp.get()
                ST = STb.v.rr("p (h q) -> p h q", h=4)
                if br == 0:
                    kb.mm(STb.v, EX[:NB, j * 128:(j + 1) * 128], S.negm[:NB].rr("p h q -> p (h q)"), start=True, stop=False, sgc=True)
                kb.mm(STb.v, KT[hs0, j * 128:(j + 1) * 128], S.Q4.v.rr("d h q -> d (h q)"),
                      start=(br == 1), stop=True, sgc=True)
                P = pp.get()
                tm = None
                if j == i:
                    tm = DGs[:, g_ * 4:(g_ + 1) * 4, :]
                elif j == i - 1:
                    tm = SDGs[:, g_ * 4:(g_ + 1) * 4, :]
                elif br == 1 and j == i - 4:
                    tm = WEt.v
                if tm is not None:
                    tp = tmpp.get()
                    kb.tt(tp.v, ST, tm, ALU.add)
                    kb.act(P.v, tp.v, AF.Exp)
                else:
                    kb.act(P.v, ST, AF.Exp)
                Pbuf[t] = P

            def emit_PV(t):
                br, j = tasks[t]
                VA, O = (VsA, bOs) if br == 0 else (VwA, bOw)
                P = Pbuf.pop(t)
                kb.mm(O[0:65, :], VA[:, j, :], P.v.rr("p h q -> p (h q)"), start=(j == firstj[br]), stop=(j == i))

            nt = len(tasks)
            for t in range(nt + DEP):
                if t < nt:
                    emit_S(t)
                if t >= DEP:
                    emit_PV(t - DEP)
                if hooks and t in (1, 5, 9):
                    hooks.pop(0)()
            while hooks:
                hooks.pop(0)()
            s_ = S.s_
            rC = S.rC
            rS, rW, aC, aS, aW = s_[:, 16:20], s_[:, 20:24], s_[:, 24:28], s_[:, 28:32], s_[:, 32:36]
            OsT = osp.get()
            OwT = osp.get()
            kb.copy(OsT.v, bOs[0:65, :], e='dve')
            kb.copy(OwT.v, bOw[0:65, :], e='dve')
            for h in range(4):
                kb.tr(bankA[:, h * 65:(h + 1) * 65], OsT[0:65, h * 128:(h + 1) * 128], identf[0:65, 0:65])
            for h in range(4):
                kb.tr(bankOc[:, h * 65:(h + 1) * 65], OwT[0:65, h * 128:(h + 1) * 128], identf[0:65, 0:65])
            OsS = bankA.v[:, 0:260].rr("p (h d) -> p h d", h=4)
            OwS = bankOc.v[:, 0:260].rr("p (h d) -> p h d", h=4)
            kb.recip(rS, OsS[:, :, 64])
            kb.recip(rW, OwS[:, :, 64])
            kb.tt(aC, S.G3[:, :, 0], rC, ALU.mult)
            kb.tt(aS, S.G3[:, :, 1], rS, ALU.mult)
            kb.tt(aW, S.G3[:, :, 2], rW, ALU.mult)
            att = attp.get()
            for h in range(4):
                kb.ts(att[:, h, :], S.Oc[:, h, :], aC[:, h:h + 1], None, op0=ALU.mult, e='pool')
                kb.stt(att[:, h, :], OsS[:, h, 0:64], aS[:, h:h + 1], att[:, h, :], ALU.mult, ALU.add)
                kb.stt(att[:, h, :], OwS[:, h, 0:64], aW[:, h:h + 1], att[:, h, :], ALU.mult, ALU.add)
            attf = att.v.rr("p h d -> p (h d)")
            jk = junkp.get()
            ja, aa, sa_ = A(jk.v), A(attf), A(ssa[:, g_ * NT + i:g_ * NT + i + 1])
            kb.op('dve', lambda eng, ja=ja, aa=aa, sa_=sa_: eng.scalar_tensor_tensor(ja, aa, 1.0, aa, ALU.mult, ALU.mult, accum_out=sa_),
                  [att], [jk, ssa])
            attb = attbp.get()
            kb.copy(attb.v, attf, e='pool')
            for fc in range(2):
                kb.tr(bankT[:, 640 + fc * 128:640 + (fc + 1) * 128], attb[:, fc * 128:(fc + 1) * 128], ident.v)
            attT = attTp.get()
            kb.copy(attT.v.rr("p c q -> p (c q)"), bankT[:, 640:896], e='dve')
            kb.dma('act', g.catT[g_ * 256:(g_ + 1) * 256, i * 128:(i + 1) * 128].rr("(c p) t -> p c t", p=128), attT.v)

        S, hk = prologue(0)
        for f in hk:
            f()
        for i in range(NT):
            if i + 1 < NT:
                Sn, hooks = prologue(i + 1)
            else:
                Sn, hooks = None, []
            dense_and_epilogue(S, hooks)
            S = Sn
    kb.dma('sp', g.ssq[:, 0:2 * NT], ssa.v)
    phase_end(kb)


def bcast_row(view_1xn, p=128):
    ap = view_1xn.ap
    return View(view_1xn.buf, ap.broadcast_to([p, ap.shape[-1]]))


def phase_LRU(kb, g, l):
    T, NT = g.T, g.NT
    phase_begin(kb)
    TC = min(2048, T)
    nchunk = T // TC
    gc = kb.sb([128, NV], F32, "gc")
    kb.dma('sp', gc.v, g.gcols[l])
    onesf = kb.sb([128, 128], F32, "onesf")
    kb.dma('sp', onesf.v, g.ones_tab.v)
    WaBD = kb.sb([128, 4, 128], BF16, "WaBD")
    WxBD = kb.sb([128, 4, 128], BF16, "WxBD")
    kb.memset(WaBD.v, 0.0, e='pool')
    kb.memset(WxBD.v, 0.0, e='pool')
    wst = kb.sb([128, 2, 4, 64], F32, "wst")
    for wi, (src, dst) in enumerate(((g.lru_wa, WaBD), (g.lru_wx, WxBD))):
        for n_ in range(2):
            kb.dma('sp', wst[n_ * 64:(n_ + 1) * 64, wi, :, :], src[l].rr("(c n) d e -> n d c e", n=2)[n_])
        kb.copy(dst[0:64, :, 0:64], wst[0:64, wi, :, :], e='dve')
        kb.copy(dst[64:128, :, 64:128], wst[64:128, wi, :, :], e='dve')
    cA = kb.sb([128, 4], F32, "cA")
    kb.act(cA.v, gc[:, 68:72], AF.Exp, scale=-1.0)
    kb.act(cA.v, cA.v, AF.Ln, bias=1.0)
    kb.ts(cA.v, cA.v, -8.0, None, op0=ALU.mult)
    carry = kb.sb([128, 4], F32, "carry")
    ssl = kb.sb([128, NT], F32, "ssl")
    psS = kb.ps([128, 512], F32, "psS")
    psg = RPool(kb, 4, [128, 512], F32, "psg", ps=True)
    xgp = RPool(kb, 2, [128, TC], F32, "xg")
    xrp = RPool(kb, 2, [128, TC + 3], F32, "xr")
    xcp = RPool(kb, 2, [128, TC], F32, "xc")
    xcbp = RPool(kb, 2, [128, TC], BF16, "xcb")
    rp = RPool(kb, 2, [128, TC], F32, "r")
    igp = RPool(kb, 2, [128, TC], F32, "ig")
    t1p = RPool(kb, 2, [128, TC], F32, "t1")
    t2p = RPool(kb, 2, [128, TC], F32, "t2")
    hhp = RPool(kb, 2, [128, TC], F32, "hh")
    obp = RPool(kb, 2, [128, TC], BF16, "ob")
    for tc in range(nchunk):
        c0 = tc * TC
        for c in range(4):
            xg = xgp.get()
            kb.dma('sp', xg.v, g.zTf[c * 128:(c + 1) * 128, c0:c0 + TC])
            xr = xrp.get()
            if tc == 0:
                kb.memset(xr[:, 0:3], 0.0, e='pool')
                kb.dma('act', xr[:, 3:3 + TC], g.zTf[512 + c * 128:512 + (c + 1) * 128, c0:c0 + TC])
            else:
                kb.dma('act', xr.v, g.zTf[512 + c * 128:512 + (c + 1) * 128, c0 - 3:c0 + TC])
            xc = xcp.get()
            kb.ts(xc.v, xr[:, 3:3 + TC], gc[:, 40 + 3 * 4 + c:40 + 3 * 4 + c + 1], gc[:, 56 + c:57 + c], op0=ALU.mult, op1=ALU.add)
            for k in (2, 1, 0):
                kb.stt(xc.v, xr[:, k:k + TC], gc[:, 40 + k * 4 + c:40 + k * 4 + c + 1], xc.v, ALU.mult, ALU.add)
            xcb = xcbp.get()
            kb.copy(xcb.v, xc.v, e='act')
            r = rp.get()
            ig = igp.get()
            for blk in range(TC // 512):
                cs = slice(blk * 512, (blk + 1) * 512)
                pa = psg.get()
                kb.mm(pa.v, WaBD[:, c, :], xcb[:, cs], start=True, stop=True)
                kb.act(r[:, cs], pa.v, AF.Sigmoid, bias=gc[:, 60 + c:61 + c])
                px = psg.get()
                kb.mm(px.v, WxBD[:, c, :], xcb[:, cs], start=True, stop=True)
                kb.act(ig[:, cs], px.v, AF.Sigmoid, bias=gc[:, 64 + c:65 + c])
            kb.act(r.v, r.v, AF.Exp, scale=cA[:, c:c + 1])
            t1 = t1p.get()
            kb.act(t1.v, r.v, AF.Square)
            kb.act(t1.v, t1.v, AF.Sqrt, scale=-1.0, bias=1.0)
            kb.tt(ig.v, ig.v, xc.v, ALU.mult, e='pool')
            kb.tt(t1.v, t1.v, ig.v, ALU.mult)
            hh = hhp.get()
            init = 0.0 if tc == 0 else A(carry[:, c:c + 1])
            ha, ra, ba = A(hh.v), A(r.v), A(t1.v)
            kb.op('dve', lambda eng, ha=ha, ra=ra, ba=ba, init=init: eng.tensor_tensor_scan(ha, ra, ba, init, ALU.mult, ALU.add),
                  [r, t1, carry], [hh])
            kb.copy(carry[:, c:c + 1], hh[:, TC - 1:TC], e='dve')
            t2 = t2p.get()
            kb.tt(t2.v, xg.v, xg.v, ALU.mult, e='pool')
            kb.ts(t2.v, t2.v, 0.044715, 1.0, op0=ALU.mult, op1=ALU.add, e='pool')
            kb.tt(t2.v, t2.v, xg.v, ALU.mult, e='pool')
            kb.act(t2.v, t2.v, AF.Sigmoid, scale=1.5957691216057308)
            kb.tt(t2.v, t2.v, xg.v, ALU.mult, e='pool')
            kb.tt(hh.v, hh.v, t2.v, ALU.mult)
            ob = obp.get()
            kb.copy(ob.v, hh.v, e='act')
            kb.dma('sp', g.catT[512 + c * 128:512 + (c + 1) * 128, c0:c0 + TC], ob.v)
            kb.tt(t2.v, hh.v, hh.v, ALU.mult, e='pool')
            nb128 = TC // 128
            for b in range(nb128):
                kb.mm(psS[:, b:b + 1], t2[:, b * 128:(b + 1) * 128], onesf[:, 0:1],
                      start=(c == 0 and b == 0), stop=(c == 3 and b == nb128 - 1), sgc=True)
        kb.copy(ssl[:, tc * (TC // 128):(tc + 1) * (TC // 128)], psS[:, 0:TC // 128], e='dve')
    kb.dma('sp', g.ssq[:, 2 * NT:3 * NT], ssl.v)
    phase_end(kb)


def phase_XKV(kb, g, l):
    phase_begin(kb)
    c = Ctx()
    c.wstage = RPool(kb, 3, [128, 2048], F32, "wst")
    c.dq = ['sp', 'act']
    c.dqi = 0
    c.cei = 0
    gc = kb.sb([128, NV], F32, "gc")
    kb.dma('sp', gc.v, g.gcols[l])
    ident = load_const_bf16(kb, c, g.ident.v, [128, 128], "ident")
    Wkv = kb.sb([128, 8, 2048], BF16, "Wkv")
    for k in range(8):
        wload(kb, c, Wkv[:, k, :], g.xkv[l, k * 128:(k + 1) * 128, :], gc[:, 24 + k:25 + k])
    c.stat = RPool(kb, 2, [128, 4], F32, "stat")
    c.junk = RPool(kb, 1, [128, 1024], F32, "junk")
    c.hb = RPool(kb, 2, [128, 1024], BF16, "hb")
    c.pT = RPool(kb, 1, [128, 1024], BF16, "pT", ps=True)
    mnT = kb.sb([128, 8, 256], BF16, "mnT")
    mp = RPool(kb, 2, [128, 1024], F32, "memt")
    for s in range(2):
        mt = mp.get()
        kb.dma('sp', mt.v, g.mem[s * 128:(s + 1) * 128, :])
        norm_T(kb, c, mt.v, mnT[:, :, s * 128:(s + 1) * 128], ident)
    psp = RPool(kb, 3, [128, 512], F32, "psx", ps=True)
    ob = RPool(kb, 3, [128, 512], BF16, "ob")
    for fc in range(8):
        p = psp.get()
        for k in range(8):
            kb.mm(p[:, 0:256], Wkv[:, k, fc * 128:(fc + 1) * 128], mnT[:, k, :], start=(k == 0), stop=(k == 7))
        o = ob.get()
        kb.copy(o[:, 0:256], p[:, 0:256], e='dve')
        kb.dma('sp', g.ckT[fc * 128:(fc + 1) * 128, :], o[:, 0:256])
    for mc in range(2):
        for half in range(2):
            p = psp.get()
            for k in range(8):
                kb.mm(p.v, mnT[:, k, mc * 128:(mc + 1) * 128], Wkv[:, k, 1024 + half * 512:1024 + (half + 1) * 512],
                      start=(k == 0), stop=(k == 7))
            o = ob.get()
            kb.copy(o.v, p.v, e='act')
            kb.dma('sp', g.cv[mc * 128:(mc + 1) * 128, half * 512:(half + 1) * 512], o.v)
    phase_end(kb)


def phase_R1(kb, g, l, xsrc):
    T, NT = g.T, g.NT
    phase_begin(kb)
    c = Ctx()
    c.wstage = RPool(kb, 2, [128, 1024], F32, "wst")
    c.dq = ['sp', 'act']
    c.dqi = 0
    c.cei = 0
    gc = kb.sb([128, NV], F32, "gc")
    kb.dma('sp', gc.v, g.gcols[l])
    ident = load_const_bf16(kb, c, g.ident.v, [128, 128], "ident")
    onesb = load_const_bf16(kb, c, g.ones_tab.v, [128, 128], "onesb")
    WoA = kb.sb([128, 4, 1024], BF16, "WoA")
    WoL = kb.sb([128, 4, 1024], BF16, "WoL")
    Wq = kb.sb([128, 8, 1024], BF16, "Wq")
    Wo = kb.sb([128, 8, 1024], BF16, "Wo")
    for k in range(4):
        wload(kb, c, WoA[:, k, :], g.w_out[l, k * 128:(k + 1) * 128, :], gc[:, 32 + k:33 + k])
        wload(kb, c, WoL[:, k, :], g.w_out[l, 512 + k * 128:512 + (k + 1) * 128, :], gc[:, 36 + k:37 + k])
    for k in range(8):
        wload(kb, c, Wq[:, k, :], g.xq[l, k * 128:(k + 1) * 128, :], gc[:, 8 + k:9 + k], 1.0 / 16.0)
        wload(kb, c, Wo[:, k, :], g.xo[l, k * 128:(k + 1) * 128, :])
    ckT = kb.sb([128, 8, 256], BF16, "ckTs")
    kb.dma('sp', ckT.v, g.ckT.v.rr("(c p) m -> p c m", p=128))
    cv = kb.sb([128, 2, 1024], BF16, "cvs")
    kb.dma('act', cv.v, g.cv.v.rr("(c p) f -> p c f", p=128))
    GP1 = kb.sb([128, 1024], F32, "GP1")
    GP2 = kb.sb([128, 1024], F32, "GP2")
    kb.dma('sp', GP1.v, bcast_row(g.grows[l, 0:1, :]))
    kb.dma('act', GP2.v, bcast_row(g.grows[l, 1:2, :]))
    SS = kb.sb([128, 3 * NT], F32, "SS")
    kb.dma('sp', SS.v, g.ssq.v)
    rA = kb.sb([128, NT], F32, "rA")
    rL = kb.sb([128, NT], F32, "rL")
    kb.tt(rA.v, SS[:, 0:NT], SS[:, NT:2 * NT], ALU.add)
    rstd_from_ss(kb, rA.v, 512)
    kb.copy(rL.v, SS[:, 2 * NT:3 * NT], e='dve')
    rstd_from_ss(kb, rL.v, 512)
    c.stat = RPool(kb, 4, [128, 4], F32, "stat")
    c.junk = RPool(kb, 1, [128, 1024], F32, "junk")
    c.hb = RPool(kb, 2, [128, 1024], BF16, "hb")
    c.pT = RPool(kb, 1, [128, 1024], BF16, "pT", ps=True)
    MT = min(512, T)
    nsub = MT // 128
    P1 = kb.ps([128, 1024], F32, "P1")
    P2 = kb.ps([128, 1024], F32, "P2")
    psp = RPool(kb, 3, [128, 512], F32, "psr", ps=True)
    catp = RPool(kb, 2, [128, 8, MT], BF16, "cat")
    x4p = RPool(kb, 2, [128, nsub, 1024], F32, "x4")
    hTp = RPool(kb, 1, [128, 8, MT], BF16, "hT")
    cqp = RPool(kb, 1, [128, 8, MT], BF16, "cqT")
    cop = RPool(kb, 1, [128, 8, MT], BF16, "coT")
    ptp = RPool(kb, 4, [128, MT], BF16, "PT")
    rdp = RPool(kb, 2, [128, MT], F32, "rden")
    mxp = RPool(kb, 2, [128, 1024], F32, "mixed")
    xop = RPool(kb, 2, [128, 1024], F32, "xo")
    for m in range(T // MT):
        t0 = m * MT
        cat = catp.get()
        kb.dma('sp', cat.v, g.catT[:, t0:t0 + MT].rr("(c p) t -> p c t", p=128))
        x4 = x4p.get()
        kb.dma('act', x4.v, xsrc[t0:t0 + MT, :].rr("(s p) d -> p s d", p=128))
        hT = hTp.get()
        for s in range(nsub):
            ti = m * nsub + s
            ts_ = slice(s * 128, (s + 1) * 128)
            for half in range(2):
                hs = slice(half * 512, (half + 1) * 512)
                for k in range(4):
                    kb.mm(P1[:, hs], cat[:, k, ts_], WoA[:, k, hs], start=(k == 0), stop=(k == 3))
                for k in range(4):
                    kb.mm(P2[:, hs], cat[:, 4 + k, ts_], WoL[:, k, hs], start=(k == 0), stop=(k == 3))
            mx = mxp.get()
            kb.ts(mx.v, P1.v, rA[:, ti:ti + 1], None, op0=ALU.mult)
            kb.stt(mx.v, P2.v, rL[:, ti:ti + 1], mx.v, ALU.mult, ALU.add)
            st = c.stat.get()
            jk = c.junk.get()
            kb.act(jk.v, mx.v, AF.Square, accum_out=st[:, 0:1])
            rstd_from_ss(kb, st[:, 0:1], D)
            kb.stt(mx.v, mx.v, st[:, 0:1], GP1.v, ALU.mult, ALU.mult)
            kb.tt(x4[:, s, :], x4[:, s, :], mx.v, ALU.add, e='pool')
            norm_T(kb, c, x4[:, s, :], hT[:, :, ts_], ident)
        cqT = cqp.get()
        for fc in range(8):
            p = psp.get()
            for k in range(8):
                kb.mm(p[:, :MT], Wq[:, k, fc * 128:(fc + 1) * 128], hT[:, k, :], start=(k == 0), stop=(k == 7))
            kb.copy(cqT[:, fc, :], p[:, :MT], e=('dve' if fc % 2 else 'act'))
        coT = cop.get()
        for hx in range(4):
            PTs = []
            for mc in range(2):
                p = psp.get()
                for f2 in range(2):
                    kb.mm(p[:, :MT], ckT[:, 2 * hx + f2, mc * 128:(mc + 1) * 128], cqT[:, 2 * hx + f2, :],
                          start=(f2 == 0), stop=(f2 == 1))
                PT = ptp.get()
                kb.act(PT.v, p[:, :MT], AF.Exp)
                PTs.append(PT)
            p = psp.get()
            for mc in range(2):
                kb.mm(p[:, :MT], onesb.v, PTs[mc].v, start=(mc == 0), stop=(mc == 1))
            rden = rdp.get()
            kb.recip(rden.v, p[:, :MT])
            for dvc in range(2):
                p = psp.get()
                for mc in range(2):
                    kb.mm(p[:, :MT], cv[:, mc, hx * 256 + dvc * 128:hx * 256 + (dvc + 1) * 128], PTs[mc].v,
                          start=(mc == 0), stop=(mc == 1))
                kb.tt(coT[:, 2 * hx + dvc, :], p[:, :MT], rden.v, ALU.mult)
        for s in range(nsub):
            ts_ = slice(s * 128, (s + 1) * 128)
            for half in range(2):
                hs = slice(half * 512, (half + 1) * 512)
                for k in range(8):
                    kb.mm(P1[:, hs], coT[:, k, ts_], Wo[:, k, hs], start=(k == 0), stop=(k == 7))
            st = c.stat.get()
            jk = c.junk.get()
            kb.act(jk.v, P1.v, AF.Square, accum_out=st[:, 0:1])
            rstd_from_ss(kb, st[:, 0:1], D)
            xo = xop.get()
            kb.stt(xo.v, P1.v, st[:, 0:1], GP2.v, ALU.mult, ALU.mult)
            kb.tt(xo.v, xo.v, x4[:, s, :], ALU.add, e='pool')
            kb.dma('sp', g.xmid[t0 + s * 128:t0 + (s + 1) * 128, :], xo.v)
    phase_end(kb)


def phase_R2(kb, g, l, xdst):
    T, NT = g.T, g.NT
    phase_begin(kb)
    c = Ctx()
    c.wstage = RPool(kb, 2, [128, 1024], F32, "wst")
    c.dq = ['sp', 'act']
    c.dqi = 0
    c.cei = 0
    gc = kb.sb([128, NV], F32, "gc")
    kb.dma('sp', gc.v, g.gcols[l])
    ident = load_const_bf16(kb, c, g.ident.v, [128, 128], "ident")
    W1 = kb.sb([128, 8, 4096], BF16, "W1")
    W2 = kb.sb([128, 32, 1024], BF16, "W2")
    for k in range(8):
        for q in range(4):
            wload(kb, c, W1[:, k, q * 1024:(q + 1) * 1024], g.mlp_w1[l, k * 128:(k + 1) * 128, q * 1024:(q + 1) * 1024],
                  gc[:, 16 + k:17 + k])
    for k in range(32):
        wload(kb, c, W2[:, k, :], g.mlp_w2[l, k * 128:(k + 1) * 128, :])
    GP3 = kb.sb([128, 1024], F32, "GP3")
    kb.dma('sp', GP3.v, bcast_row(g.grows[l, 2:3, :]))
    c.stat = RPool(kb, 4, [128, 4], F32, "stat")
    c.junk = RPool(kb, 1, [128, 1024], F32, "junk")
    c.hb = RPool(kb, 2, [128, 1024], BF16, "hb")
    c.pT = RPool(kb, 1, [128, 1024], BF16, "pT", ps=True)
    MT = 128
    Py = kb.ps([128, 1024], F32, "Py")
    psp = RPool(kb, 4, [128, 512], F32, "psm", ps=True)
    x2p = RPool(kb, 2, [128, 1024], F32, "x2")
    hTp = RPool(kb, 2, [128, 8, MT], BF16, "hT")
    utp = RPool(kb, 2, [128, 32, MT], BF16, "uT")
    rlp = RPool(kb, 3, [128, MT], F32, "rl")
    xop = RPool(kb, 2, [128, 1024], F32, "xo")
    for m in range(T // MT):
        t0 = m * MT
        x2 = x2p.get()
        kb.dma('sp', x2.v, g.xmid[t0:t0 + MT, :])
        hT = hTp.get()
        norm_T(kb, c, x2.v, hT.v, ident)
        uT = utp.get()
        for fc in range(32):
            p = psp.get()
            for k in range(8):
                kb.mm(p[:, :MT], W1[:, k, fc * 128:(fc + 1) * 128], hT[:, k, :], start=(k == 0), stop=(k == 7))
            rl = rlp.get()
            kb.act(rl.v, p[:, :MT], AF.Relu)
            kb.tt(uT[:, fc, :], rl.v, rl.v, ALU.mult, e=('dve' if fc % 2 else 'pool'))
        for half in range(2):
            hs = slice(half * 512, (half + 1) * 512)
            for k in range(32):
                kb.mm(Py[:, hs], uT[:, k, :], W2[:, k, hs], start=(k == 0), stop=(k == 31))
        st = c.stat.get()
        jk = c.junk.get()
        kb.act(jk.v, Py.v, AF.Square, accum_out=st[:, 0:1])
        rstd_from_ss(kb, st[:, 0:1], D)
        xo = xop.get()
        kb.stt(xo.v, Py.v, st[:, 0:1], GP3.v, ALU.mult, ALU.mult)
        kb.tt(xo.v, xo.v, x2.v, ALU.add, e='pool')
        kb.dma('act', xdst[t0:t0 + MT, :], xo.v)
    phase_end(kb)


def t5_bucket_np(d):
    d = np.maximum(d, 0)
    df = np.maximum(d, 1).astype(np.float32)
    large = 16 + (np.log(df / np.float32(16)) / np.float32(math.log(8.0)) * np.float32(16)).astype(np.int32)
    large = np.minimum(large, 31)
    return np.where(d < 16, d, large)


def host_tables(rel_bias):
    rb = np.asarray(rel_bias, np.float32)
    q = np.arange(128)
    tabs = {}
    jp = np.arange(256)
    dist = q[:, None] + 64 * (128 - jp[None, :]) - 63
    bk = t5_bucket_np(dist)
    tc = rb[bk]
    tc = np.where((dist >= 0)[:, :, None], tc, np.float32(NEGM)).transpose(0, 2, 1)
    tabs["tc_tab"] = np.ascontiguousarray(tc, np.float32).reshape(128, 8 * 256)
    k = np.arange(128)
    dist = q[None, :] - k[:, None]
    dg = rb[t5_bucket_np(dist)]
    dg = np.where((dist >= 0)[:, :, None], dg, np.float32(NEGM)).transpose(0, 2, 1)
    tabs["dg_tab"] = np.ascontiguousarray(dg, np.float32).reshape(128, 8 * 128)
    dist = 128 + q[None, :] - k[:, None]
    sdg = rb[t5_bucket_np(dist)].transpose(0, 2, 1)
    tabs["sdg_tab"] = np.ascontiguousarray(sdg, np.float32).reshape(128, 8 * 128)
    c31 = np.broadcast_to(rb[31][None, :, None], (128, 8, 128))
    tabs["c31_tab"] = np.ascontiguousarray(c31, np.float32).reshape(128, 8 * 128)
    we = np.where(k[:, None] > q[None, :], np.float32(0), np.float32(NEGM))
    tabs["we_tab"] = np.ascontiguousarray(np.broadcast_to(we[:, None, :], (128, 4, 128)), np.float32).reshape(128, 512)
    rel = (jp[None, :] - 128) - (q[:, None] >= 64)
    addt = np.where(rel > 0, np.float32(-1e30), np.where(rel >= -1, np.float32(1e4), np.float32(0)))
    tabs["addt_tab"] = np.ascontiguousarray(addt, np.float32)
    tabs["rv0_tab"] = (q >= 63).astype(np.float32).reshape(128, 1)
    tabs["ident"] = np.eye(128, dtype=np.float32)
    bm = np.zeros((4, 4, 128), np.float32)
    for h in range(4):
        bm[h, h, :] = 1
    tabs["ones_tab"] = np.ones((128, 128), np.float32)
    return tabs


def host_layer_tables(inp, L):
    def col(v, nch):
        return np.asarray(v, np.float32)[:L].reshape(L, nch, 128).transpose(0, 2, 1)
    cols = [col(inp["ln_mix_pre"], 8), col(inp["ln_x_pre"], 8), col(inp["ln_mlp_pre"], 8), col(inp["ln_mem"], 8),
            col(inp["gn_attn"], 4), col(inp["gn_lru"], 4)]
    cw = np.asarray(inp["conv_w"], np.float32)[:L]
    cols.append(cw.reshape(L, 4, 4, 128).transpose(0, 3, 1, 2).reshape(L, 128, 16))
    for nm in ["conv_b", "lru_ba", "lru_bx", "lru_lambda"]:
        cols.append(col(inp[nm], 4))
    gcols = np.ascontiguousarray(np.concatenate(cols, axis=2), np.float32)
    assert gcols.shape == (L, 128, NV)
    grows = np.ascontiguousarray(np.stack([inp["ln_mix_post"][:L], inp["ln_x_post"][:L], inp["ln_mlp_post"][:L]], axis=1), np.float32)
    out = {"gcols": gcols, "grows": grows}
    for nm, key in [("pe2k", "cmp_pe_k"), ("pe2v", "cmp_pe_v")]:
        pe = np.asarray(inp[key], np.float32)[:L]
        out[nm] = np.ascontiguousarray(pe.reshape(L, 32, 2, 64).transpose(0, 2, 3, 1).reshape(L, 128, 32))
    return out


WNAMES = [("w_in", [1024, 2328]), ("cmp_w_k", [4096, 64]), ("cmp_w_v", [4096, 64]), ("lru_wa", [8, 64, 64]),
          ("lru_wx", [8, 64, 64]), ("w_out", [1024, 1024]), ("xq", [1024, 1024]), ("xkv", [1024, 2048]),
          ("xo", [1024, 1024]), ("mlp_w1", [1024, 4096]), ("mlp_w2", [4096, 1024])]
TABS = [("tc_tab", [128, 2048]), ("dg_tab", [128, 1024]), ("sdg_tab", [128, 1024]), ("c31_tab", [128, 1024]),
        ("we_tab", [128, 512]), ("addt_tab", [128, 256]), ("rv0_tab", [128, 1]), ("ident", [128, 128]),
        ("ones_tab", [128, 128])]


def build(T, L, debug=False, stop_after=None):
    nc = bass.Bass("TRN2", target_bir_lowering=False)
    es = ExitStack()
    with es:
        kb = KB(nc, es)
        g = Ctx()
        g.T, g.NT, g.NB, g.L = T, T // 128, T // 64, L
        g.x = kb.dram("x", [T, D], F32, kind="ExternalInput")
        g.mem = kb.dram("mem", [256, D], F32, kind="ExternalInput")
        for nm, shp in WNAMES:
            setattr(g, nm, kb.dram(nm, [L] + shp, F32, kind="ExternalInput"))
        for nm, shp in TABS:
            setattr(g, nm, kb.dram(nm, shp, F32, kind="ExternalInput"))
        g.gcols = kb.dram("gcols", [L, 128, NV], F32, kind="ExternalInput")
        g.grows = kb.dram("grows", [L, 3, D], F32, kind="ExternalInput")
        g.pe2k = kb.dram("pe2k", [L, 128, 32], F32, kind="ExternalInput")
        g.pe2v = kb.dram("pe2v", [L, 128, 32], F32, kind="ExternalInput")
        g.out = kb.dram("out", [T, D], F32, kind="ExternalOutput")
        sk = "ExternalOutput" if debug else "Internal"
        g.zTb = kb.dram("zTb", [1024, T], BF16, kind=sk)
        g.zTf = kb.dram("zTf", [1024, T], F32, kind=sk)
        g.zV = kb.dram("zV", [T, 280], F32, kind=sk)
        g.catT = kb.dram("catT", [1024, T], BF16, kind=sk)
        g.ssq = kb.dram("ssq", [128, 3 * g.NT], F32, kind=sk)
        g.ckT = kb.dram("ckT", [1024, 256], BF16, kind=sk)
        g.cv = kb.dram("cv", [256, 1024], BF16, kind=sk)
        g.xmid = kb.dram("xmid", [T, D], F32, kind=sk)
        g.xres = kb.dram("xres", [T, D], F32, kind=sk)
        kb.cur_phase = 0
        phases = []
        for l in range(L):
            xsrc = g.x if l == 0 else g.xres
            xdst = g.out if l == L - 1 else g.xres
            phases += [("M1", lambda l=l, xsrc=xsrc: phase_M1(kb, g, l, xsrc)),
                       ("ATT", lambda l=l: phase_ATT(kb, g, l)),
                       ("LRU", lambda l=l: phase_LRU(kb, g, l)),
                       ("XKV", lambda l=l: phase_XKV(kb, g, l)),
                       ("R1", lambda l=l, xsrc=xsrc: phase_R1(kb, g, l, xsrc)),
                       ("R2", lambda l=l, xdst=xdst: phase_R2(kb, g, l, xdst))]
        for i, (nm, fn) in enumerate(phases):
            kb.cur_phase = i + 1
            fn()
            if stop_after is not None and i + 1 >= stop_after:
                break
    return nc


def make_inmaps(inputs, T, L, ncores):
    tabs = host_tables(inputs["rel_bias"])
    lt = host_layer_tables(inputs, L)
    shared = {}
    for nm, shp in WNAMES:
        shared[nm] = np.ascontiguousarray(np.asarray(inputs[nm], np.float32)[:L].reshape([L] + shp))
    shared.update(tabs)
    shared.update(lt)
    maps = []
    B = inputs["x"].shape[0]
    for c in range(ncores):
        b = c % B
        m = dict(shared)
        m["x"] = np.ascontiguousarray(np.asarray(inputs["x"], np.float32)[b, :T])
        m["mem"] = np.ascontiguousarray(np.asarray(inputs["mem"], np.float32)[b])
        maps.append(m)
    return maps


_NC_CACHE = {}


def kernel(**inputs):
    T, L = 8192, 2
    B = inputs["x"].shape[0]
    key = (T, L)
    if key not in _NC_CACHE:
        _NC_CACHE[key] = build(T, L)
    nc = _NC_CACHE[key]
    ncores = 8
    maps = make_inmaps(inputs, T, L, ncores)
    res = run_bass_kernel_spmd(nc, maps, core_ids=list(range(ncores)))
    out = np.stack([np.asarray(res.results[b]["out"], np.float32) for b in range(B)], axis=0)
    return out
```

```python
import numpy as np
from contextlib import ExitStack
import concourse.bass as bass
import concourse.mybir as mybir
from concourse.bass_utils import run_bass_kernel_spmd

F32 = mybir.dt.float32
BF16 = mybir.dt.bfloat16
AF = mybir.ActivationFunctionType
ALU = mybir.AluOpType
AX = mybir.AxisListType


class TL:
    def __init__(self, sem, step):
        self.sem = sem
        self.step = step
        self.val = 0


class Buf:
    def __init__(self, t, name, space):
        self.t = t
        self.name = name
        self.space = space
        self.last_w = None
        self.readers = {}
        self.dtl = None

    def __getitem__(self, idx):
        return View(self, self.t[idx])

    @property
    def v(self):
        return View(self, self.t[:])


class View:
    def __init__(self, buf, ap):
        self.buf = buf
        self.ap = ap

    def __getitem__(self, idx):
        return View(self.buf, self.ap[idx])

    def rr(self, pat, **kw):
        return View(self.buf, self.ap.rearrange(pat, **kw))

    def bc(self, shape):
        return View(self.buf, self.ap.broadcast_to(shape))


def _bufs(views):
    out = []
    for v in views:
        if v is None:
            continue
        if isinstance(v, Buf):
            out.append(v)
        elif isinstance(v, View):
            out.append(v.buf)
    return out


def A(v):
    if isinstance(v, View):
        return v.ap
    if isinstance(v, Buf):
        return v.t[:]
    return v


ENGS = ['pe', 'act', 'dve', 'pool', 'sp']


class KB:
    def __init__(self, nc, es):
        self.nc = nc
        self.es = es
        self.tl = {}
        for e in ENGS:
            self.tl[e] = TL(es.enter_context(nc.semaphore("s_" + e)), 1)
        self.prog = {e: [] for e in ENGS}
        self.seen = {e: {} for e in ENGS}
        self.dtls = []
        self.free_dtls = []
        self.phase_dtls = []
        self.root_es = es
        self.nbuf = 0
        self.cur_phase = 0

    def sb(self, shape, dtype, name=None):
        self.nbuf += 1
        name = (name or "sb") + "_%d" % self.nbuf
        t = self.es.enter_context(self.nc.sbuf_tensor(name, list(shape), dtype))
        b = Buf(t, name, 'sb')
        b.phase = self.cur_phase
        return b

    def ps(self, shape, dtype, name=None):
        self.nbuf += 1
        name = (name or "ps") + "_%d" % self.nbuf
        t = self.es.enter_context(self.nc.psum_tensor(name, list(shape), dtype))
        b = Buf(t, name, 'ps')
        b.phase = self.cur_phase
        return b

    def dram(self, name, shape, dtype, kind="Internal"):
        t = self.nc.dram_tensor(name, list(shape), dtype, kind=kind)
        return Buf(t, name, 'dram')

    def sub(self, buf, idx, name=None):
        b = Buf(None, name or (buf.name + "_sub"), buf.space)
        b.t = _Sub(buf.t[idx])
        return b

    def _waits(self, e, reads, writes, own_tl=None):
        need = {}

        def add(tl, val):
            if need.get(tl, 0) < val:
                need[tl] = val
        mytl = self.tl[e]
        for b in reads:
            if b.last_w:
                add(*b.last_w)
        for b in writes:
            if b.last_w:
                tl, val = b.last_w
                if (tl is mytl and e == 'pe') or (own_tl is not None and tl is own_tl):
                    pass
                else:
                    add(tl, val)
            for tl, val in b.readers.items():
                if tl is mytl and e == 'pe':
                    continue
                add(tl, val)
        waits = []
        for tl, val in need.items():
            if tl.step == 16:
                val = tl.val
            if self.seen[e].get(tl, 0) >= val:
                continue
            self.seen[e][tl] = val
            waits.append((tl, val))
        return waits

    def op(self, e, fn, reads, writes):
        reads = _bufs(reads)
        writes = _bufs(writes)
        waits = self._waits(e, reads, writes)
        tl = self.tl[e]
        tl.val += 1
        v = tl.val
        self.prog[e].append((waits, fn, tl.sem, 1))
        for b in reads:
            b.readers[tl] = v
        for b in writes:
            b.last_w = (tl, v)
            b.readers = {}

    def dma(self, e, out, in_, **kw):
        ob = out.buf if isinstance(out, View) else out
        ib = in_.buf if isinstance(in_, View) else in_
        side = ob if ob.space != 'dram' else ib
        if side.dtl is None:
            if self.free_dtls:
                side.dtl = self.free_dtls.pop()
            else:
                side.dtl = TL(self.root_es.enter_context(self.nc.semaphore("d%d" % len(self.dtls))), 16)
                self.dtls.append(side.dtl)
            if getattr(side, 'phase', 0) == self.cur_phase and self.cur_phase > 0:
                self.phase_dtls.append(side.dtl)
        dtl = side.dtl
        waits = self._waits(e, [ib], [ob], own_tl=dtl)
        dtl.val += 16
        v = dtl.val
        oa, ia = A(out), A(in_)
        self.prog[e].append((waits, lambda eng: eng.dma_start(out=oa, in_=ia, **kw), dtl.sem, 16))
        ib.readers[dtl] = v
        ob.last_w = (dtl, v)
        ob.readers = {}

    def collective(self, out, in_, groups, kind="AllGather"):
        ob, ib = out.buf, in_.buf
        if ob.dtl is None:
            ob.dtl = TL(self.root_es.enter_context(self.nc.semaphore("cc%d" % len(self.dtls))), 16)
            self.dtls.append(ob.dtl)
        dtl = ob.dtl
        waits = self._waits('pool', [ib], [ob], own_tl=None)
        dtl.val += 16
        v = dtl.val
        oa, ia = A(out), A(in_)
        self.prog['pool'].append((waits, lambda eng: eng.collective_compute(kind, ALU.bypass, replica_groups=groups,
                                                                          ins=[ia], outs=[oa]), dtl.sem, 16))
        ib.readers[dtl] = v
        ob.last_w = (dtl, v)
        ob.readers = {}

    def barrier(self):
        for e in ENGS:
            waits = []
            for tl in list(self.tl.values()) + self.dtls:
                if tl.val > 0 and self.seen[e].get(tl, 0) < tl.val and tl is not None:
                    self.seen[e][tl] = tl.val
                    waits.append((tl, tl.val))
            self.prog[e].append((waits, None, None, 0))

    def emit_block(self):
        nc = self.nc
        prog = self.prog
        self.prog = {e: [] for e in ENGS}
        self.free_dtls.extend(self.phase_dtls)
        self.phase_dtls = []
        with nc.Block() as block:
            def run(eng, lst):
                for waits, fn, sem, inc in lst:
                    for tl, val in waits:
                        eng.wait_ge(tl.sem, val)
                    if fn is not None:
                        ins = fn(eng)
                        ins.then_inc(sem, inc)

            @block.tensor
            def _(eng):
                run(eng, prog['pe'])

            @block.scalar
            def _(eng):
                run(eng, prog['act'])

            @block.vector
            def _(eng):
                run(eng, prog['dve'])

            @block.gpsimd
            def _(eng):
                run(eng, prog['pool'])

            @block.sync
            def _(eng):
                run(eng, prog['sp'])

    def mm(self, out, lhsT, rhs, start, stop=True, sgc=False, **kw):
        oa, la, ra = A(out), A(lhsT), A(rhs)
        if sgc:
            kw['skip_group_check'] = True
        self.op('pe', lambda eng: eng.matmul(oa, la, ra, start=start, stop=stop, **kw),
                [lhsT, rhs], [out])

    def tr(self, out, in_, ident):
        oa, ia, da = A(out), A(in_), A(ident)
        self.op('pe', lambda eng: eng.transpose(oa, ia, da), [in_, ident], [out])

    def act(self, out, in_, func, bias=None, scale=None, accum_out=None, e='act'):
        oa, ia = A(out), A(in_)
        kw = {}
        rd = [in_]
        if bias is not None:
            kw['bias'] = A(bias)
            rd.append(bias)
        if scale is not None:
            kw['scale'] = A(scale)
            rd.append(scale)
        wr = [out]
        if accum_out is not None:
            kw['accum_out'] = A(accum_out)
            wr.append(accum_out)
        self.op(e, lambda eng: eng.activation(oa, ia, func, **kw), rd, wr)

    def tt(self, out, in0, in1, op, e='dve'):
        oa, a0, a1 = A(out), A(in0), A(in1)
        self.op(e, lambda eng: eng.tensor_tensor(oa, a0, a1, op), [in0, in1], [out])

    def ts(self, out, in0, s1, s2=None, op0=None, op1=None, e='dve', accum_out=None):
        oa, a0 = A(out), A(in0)
        rd = [in0, s1, s2]
        kw = {}
        wr = [out]
        if accum_out is not None:
            kw['accum_out'] = A(accum_out)
            wr.append(accum_out)
        a1, a2 = A(s1), A(s2)
        if op1 is None:
            self.op(e, lambda eng: eng.tensor_scalar(oa, a0, a1, None, op0, **kw), rd, wr)
        else:
            self.op(e, lambda eng: eng.tensor_scalar(oa, a0, a1, a2, op0, op1, **kw), rd, wr)

    def stt(self, out, in0, scalar, in1, op0, op1, e='dve'):
        oa, a0, sc, a1 = A(out), A(in0), A(scalar), A(in1)
        self.op(e, lambda eng: eng.scalar_tensor_tensor(oa, a0, sc, a1, op0, op1), [in0, scalar, in1], [out])

    def copy(self, out, in_, e='dve'):
        oa, ia = A(out), A(in_)
        if e == 'act':
            self.op(e, lambda eng: eng.copy(oa, ia), [in_], [out])
        else:
            self.op(e, lambda eng: eng.tensor_copy(oa, ia), [in_], [out])

    def memset(self, out, val, e='pool'):
        oa = A(out)
        self.op(e, lambda eng: eng.memset(oa, val), [], [out])

    def recip(self, out, in_, e='dve'):
        oa, ia = A(out), A(in_)
        self.op(e, lambda eng: eng.reciprocal(oa, ia), [in_], [out])


class _Sub:
    def __init__(self, ap):
        self.ap = ap

    def __getitem__(self, idx):
        return self.ap[idx]

import math

EPS = 1e-6
NEGM = -30000.0
D = 1024
NV = 72
O_Q, O_KC, O_VC, O_KS, O_VS, O_KW, O_VW, O_GT, O_XG, O_XR = 0, 512, 640, 768, 896, 1024, 1152, 1280, 1304, 1816


class RPool:
    def __init__(self, kb, n, shape, dtype, name, ps=False):
        mk = kb.ps if ps else kb.sb
        self.bufs = [mk(shape, dtype, "%s%d" % (name, i)) for i in range(n)]
        self.i = 0

    def get(self):
        b = self.bufs[self.i % len(self.bufs)]
        self.i += 1
        return b


def phase_begin(kb):
    kb.pes = ExitStack()
    kb.pes.__enter__()
    kb.outer_es = kb.es
    kb.es = kb.pes
    kb.phase_dtls = []


def phase_end(kb):
    kb.barrier()
    kb.emit_block()
    kb.es = kb.outer_es
    kb.pes.__exit__(None, None, None)


class Ctx:
    pass


def wload(kb, c, dst, src, gcol=None, mul=None):
    n = src.ap.shape[-1]
    st = c.wstage.get()
    q = c.dq[c.dqi % 2]
    c.dqi += 1
    kb.dma(q, st[:, :n], src)
    e = ['dve', 'pool'][c.cei % 2]
    c.cei += 1
    if gcol is not None and mul is not None:
        kb.ts(dst, st[:, :n], gcol, float(mul), op0=ALU.mult, op1=ALU.mult, e=e)
    elif gcol is not None:
        kb.ts(dst, st[:, :n], gcol, None, op0=ALU.mult, e=e)
    elif mul is not None:
        kb.ts(dst, st[:, :n], float(mul), None, op0=ALU.mult, e=e)
    else:
        kb.copy(dst, st[:, :n], e=e)


def rstd_from_ss(kb, ss, n):
    kb.ts(ss, ss, 1.0 / n, EPS, op0=ALU.mult, op1=ALU.add)
    kb.act(ss, ss, AF.Sqrt)
    kb.recip(ss, ss)


def norm_T1(kb, c, xv):
    st = c.stat.get()
    junk = c.junk.get()
    kb.act(junk.v, xv, AF.Square, accum_out=st[:, 0:1])
    rstd_from_ss(kb, st[:, 0:1], D)
    hb = c.hb.get()
    kb.ts(hb.v, xv, st[:, 0:1], None, op0=ALU.mult)
    return hb


def norm_T2(kb, c, hb, hT_dst, ident):
    pT = c.pT.get()
    for k in range(8):
        kb.tr(pT[:, k * 128:(k + 1) * 128], hb[:, k * 128:(k + 1) * 128], ident.v)
    kb.copy(hT_dst, pT.v.rr("p (c t) -> p c t", c=8), e='act')


def norm_T(kb, c, xv, hT_dst, ident):
    norm_T2(kb, c, norm_T1(kb, c, xv), hT_dst, ident)


def phase_M1(kb, g, l, xsrc):
    T, NT = g.T, g.NT
    phase_begin(kb)
    c = Ctx()
    c.wstage = RPool(kb, 3, [128, 2048], F32, "wst")
    c.dq = ['sp', 'act']
    c.dqi = 0
    c.cei = 0
    gc = kb.sb([128, NV], F32, "gc")
    kb.dma('sp', gc.v, g.gcols[l])
    WF = kb.sb([128, 8, 2048], BF16, "WF")
    WT = kb.sb([128, 8, 280], BF16, "WT")
    w = g.w_in
    for k in range(8):
        rows = slice(k * 128, (k + 1) * 128)
        gk = gc[:, k:k + 1]
        wload(kb, c, WF[:, k, 0:512], w[l, rows, O_Q:O_Q + 512], gk, 0.125)
        wload(kb, c, WF[:, k, 512:768], w[l, rows, O_KC:O_KC + 256], gk)
        wload(kb, c, WF[:, k, 768:896], w[l, rows, O_KS:O_KS + 128], gk)
        wload(kb, c, WF[:, k, 896:1024], w[l, rows, O_KW:O_KW + 128], gk)
        wload(kb, c, WF[:, k, 1024:2048], w[l, rows, O_XG:O_XG + 1024], gk)
        wload(kb, c, WT[:, k, 0:128], w[l, rows, O_VS:O_VS + 128], gk)
        wload(kb, c, WT[:, k, 128:256], w[l, rows, O_VW:O_VW + 128], gk)
        wload(kb, c, WT[:, k, 256:280], w[l, rows, O_GT:O_GT + 24], gk)
    ident = kb.sb([128, 128], BF16, "identb")
    idf = kb.sb([128, 128], F32, "identf")
    kb.dma('sp', idf.v, g.ident.v)
    kb.copy(ident.v, idf.v)
    c.stat = RPool(kb, 8, [128, 4], F32, "stat")
    c.junk = RPool(kb, 2, [128, 1024], F32, "junk")
    c.hb = RPool(kb, 6, [128, 1024], BF16, "hb")
    c.pT = RPool(kb, 1, [128, 1024], BF16, "pT", ps=True)
    xp = RPool(kb, 6, [128, 1024], F32, "xin")
    hTp = RPool(kb, 2, [128, 8, 512], BF16, "hT")
    ptm = RPool(kb, 1, [128, 512], F32, "ptm", ps=True)
    pfm = RPool(kb, 3, [128, 512], F32, "pfm", ps=True)
    vst = RPool(kb, 2, [128, 280], F32, "vst")
    fb = RPool(kb, 3, [128, 512], BF16, "fb")
    ff = RPool(kb, 3, [128, 512], F32, "ff")
    MT = min(512, T)
    nsub = MT // 128
    ei = [0]
    NM = T // MT

    def A1(m):
        hbs = []
        for s_ in range(nsub):
            t0 = m * MT + s_ * 128
            xs = xp.get()
            kb.dma('sp', xs.v, xsrc[t0:t0 + 128, :])
            hbs.append(norm_T1(kb, c, xs.v))
        return hbs

    def A2(m, hbs):
        hT = hTp.get()
        for s_ in range(nsub):
            t0 = m * MT + s_ * 128
            norm_T2(kb, c, hbs[s_], hT[:, :, s_ * 128:(s_ + 1) * 128], ident)
            pt = ptm.get()
            for k in range(8):
                kb.mm(pt[:, 0:280], hT[:, k, s_ * 128:(s_ + 1) * 128], WT[:, k, :], start=(k == 0), stop=(k == 7))
            vs = vst.get()
            kb.copy(vs.v, pt[:, 0:280], e='dve')
            kb.dma('sp', g.zV[t0:t0 + 128, :], vs.v)
        return hT

    def Bfc(m, hT, fc):
        pf = pfm.get()
        for k in range(8):
            kb.mm(pf[:, :MT], WF[:, k, fc * 128:(fc + 1) * 128], hT[:, k, :MT], start=(k == 0), stop=(k == 7))
        e = ['act', 'dve'][ei[0] % 2]
        ei[0] += 1
        if fc < 8:
            o = fb.get()
            kb.copy(o[:, :MT], pf[:, :MT], e=e)
            kb.dma('act' if fc % 2 else 'sp', g.zTb[fc * 128:(fc + 1) * 128, m * MT:(m + 1) * MT], o[:, :MT])
        else:
            o = ff.get()
            kb.copy(o[:, :MT], pf[:, :MT], e=e)
            kb.dma('act' if fc % 2 else 'sp', g.zTf[(fc - 8) * 128:(fc - 7) * 128, m * MT:(m + 1) * MT], o[:, :MT])

    hT = A2(0, A1(0))
    for m in range(NM):
        hbs = None
        for fc in range(16):
            Bfc(m, hT, fc)
            if fc == 2 and m + 1 < NM:
                hbs = A1(m + 1)
        if m + 1 < NM:
            hT = A2(m + 1, hbs)
    phase_end(kb)


def load_const_bf16(kb, c, dram_view, shape, name):
    st = kb.sb(shape, F32, name + "_f")
    kb.dma('sp', st.v, dram_view)
    b = kb.sb(shape, BF16, name)
    kb.copy(b.v, st.v, e='pool')
    return b


def phase_ATT(kb, g, l):
    T, NT, NB = g.T, g.NT, g.NB
    phase_begin(kb)
    c = Ctx()
    c.wstage = RPool(kb, 1, [128, 2048], F32, "wst")
    c.dq = ['sp', 'act']
    c.dqi = 0
    c.cei = 0
    ident = load_const_bf16(kb, c, g.ident.v, [128, 128], "ident")
    onesb = load_const_bf16(kb, c, g.ones_tab.v, [128, 128], "onesb")
    TCt = kb.sb([128, 8, 256], F32, "TCt")
    kb.dma('sp', TCt.v.rr("p h j -> p (h j)"), g.tc_tab.v)
    C31 = kb.sb([128, 8, 128], F32, "C31")
    kb.dma('act', C31.v.rr("p h j -> p (h j)"), g.c31_tab.v)
    DGs = kb.sb([128, 8, 128], F32, "DGs")
    kb.dma('sp', DGs.v.rr("p h j -> p (h j)"), g.dg_tab.v)
    kb.tt(DGs.v, DGs.v, C31.v, ALU.subtract, e='pool')
    SDGs = kb.sb([128, 8, 128], F32, "SDGs")
    kb.dma('act', SDGs.v.rr("p h j -> p (h j)"), g.sdg_tab.v)
    kb.tt(SDGs.v, SDGs.v, C31.v, ALU.subtract, e='pool')
    WEt = kb.sb([128, 4, 128], F32, "WEt")
    kb.dma('sp', WEt.v.rr("p h j -> p (h j)"), g.we_tab.v)
    ADDT = kb.sb([128, 256], F32, "ADDT")
    kb.dma('sp', ADDT.v, g.addt_tab.v)
    RV0 = kb.sb([128, 1], F32, "RV0")
    kb.dma('sp', RV0.v, g.rv0_tab.v)
    EX = kb.sb([128, T], BF16, "EX")
    KC2 = kb.sb([128, T], BF16, "KC2")
    VC2 = kb.sb([128, T], BF16, "VC2")
    EXa, EXb = KC2, VC2
    kb.memset(EXa.v, 1.0, e='pool')
    a0, a1, a2 = A(EXa.v), A(EXb.v), A(EX.v)
    kb.op('pool', lambda eng: eng.affine_select(a1, a0, [[1, T]], ALU.is_ge, 0.0, base=0, channel_multiplier=-64),
          [EXa], [EXb])
    kb.op('pool', lambda eng: eng.affine_select(a2, a1, [[-1, T]], ALU.is_ge, 0.0, base=63, channel_multiplier=64),
          [EXb], [EX])
    ssa = kb.sb([128, 2 * NT], F32, "ssa")
    LV = 9
    if LV <= 1:
        phase_end(kb)
        return

    KsT = kb.sb([128, T], BF16, "KsT")
    kb.memset(KsT[64:128, :], 0.0, e='pool')
    KwT = kb.sb([128, T], BF16, "KwT")
    kb.memset(KwT[64:128, :], 0.0, e='pool')
    VsA = kb.sb([128, NT, 65], BF16, "VsA")
    VwA = kb.sb([128, NT, 65], BF16, "VwA")
    Wck = kb.sb([128, 32, 128], BF16, "Wck")
    Wcv = kb.sb([128, 32, 64], BF16, "Wcv")
    pek = kb.sb([128, 32], BF16, "pek")
    pev = kb.sb([128, 32], BF16, "pev")
    pef = kb.sb([128, 64], F32, "pef")
    ckc = kb.sb([128, 1], F32, "ckc")
    cvr = kb.sb([1, 64], BF16, "cvr")
    KcT = kb.sb([128, NB], BF16, "KcT")
    Vc = kb.sb([128, 64], BF16, "Vc")
    vstage = RPool(kb, 2, [128, 16, 64], F32, "vstg")
    bankA = kb.ps([128, 512], F32, "bankA")
    bankT = kb.ps([128, 1024], BF16, "bankT")
    bankOc = kb.ps([128, 512], F32, "bankOc")
    stp = RPool(kb, 3, [128, 512], F32, "ST", ps=True)
    bOs = kb.ps([128, 512], F32, "bOs")
    bOw = kb.ps([128, 512], F32, "bOw")
    qap = RPool(kb, 3, [128, 4, 128], BF16, "Q4")
    for qb_ in qap.bufs:
        kb.memset(qb_[64:128, :, :], 0.0, e='pool')
    gtp = RPool(kb, 2, [128, 12], F32, "GT")
    gsp = RPool(kb, 2, [128, 12], F32, "GS")
    scp = RPool(kb, 2, [128, 4, NB], F32, "sC")
    ecp = RPool(kb, 2, [128, 4, NB], F32, "eC")
    ecbp = RPool(kb, 2, [128, 4, NB], BF16, "eCb")
    ectp = RPool(kb, 2, [128, 4, 128], BF16, "eCT")
    stat = RPool(kb, 3, [128, 48], F32, "st")
    impp = RPool(kb, 2, [128, NB], F32, "imp")
    selp = RPool(kb, 2, [128, NB], F32, "sel")
    sel2p = RPool(kb, 2, [128, NB], F32, "sel2")
    selmp = RPool(kb, 2, [128, NB], BF16, "selm")
    negp = RPool(kb, 2, [128, 4, 128], BF16, "negm")
    tmpp = RPool(kb, 2, [128, 4, 128], F32, "tmpl")
    pp = RPool(kb, 4, [128, 4, 128], BF16, "P")
    attp = RPool(kb, 2, [128, 4, 64], F32, "att")
    attbp = RPool(kb, 2, [128, 256], BF16, "attb")
    attTp = RPool(kb, 2, [128, 2, 128], BF16, "attT")
    junkp = RPool(kb, 2, [128, 256], F32, "junk")
    ocp = RPool(kb, 2, [128, 4, 64], F32, "Oc")
    osp = RPool(kb, 4, [65, 512], F32, "OsT")
    identf = kb.sb([128, 128], F32, "identf2")
    kb.dma('sp', identf.v, g.ident.v)

    kb.dma('sp', pef[:, 0:32], g.pe2k[l])
    kb.dma('sp', pef[:, 32:64], g.pe2v[l])
    kb.copy(pek.v, pef[:, 0:32], e='pool')
    kb.copy(pev.v, pef[:, 32:64], e='pool')
    st = c.wstage.get()
    kb.dma('sp', st.v.rr("p (c e) -> p c e", e=64), g.cmp_w_k[l].rr("(c p) e -> p c e", p=128))
    kb.copy(Wck[:, :, 0:64], st.v.rr("p (c e) -> p c e", e=64), e='dve')
    kb.copy(Wck[:, :, 64:128], st.v.rr("p (c e) -> p c e", e=64), e='pool')
    st = c.wstage.get()
    kb.dma('act', st.v.rr("p (c e) -> p c e", e=64), g.cmp_w_v[l].rr("(c p) e -> p c e", p=128))
    kb.copy(Wcv.v, st.v.rr("p (c e) -> p c e", e=64), e='dve')

    for g_ in range(2):
        kb.dma('sp', KsT[0:64, :], g.zTb[768 + g_ * 64:768 + g_ * 64 + 64, :])
        kb.dma('act', KwT[0:64, :], g.zTb[896 + g_ * 64:896 + g_ * 64 + 64, :])
        kb.dma('sp', KC2[0:64, :], g.zTb[512 + g_ * 64:512 + g_ * 64 + 64, :])
        kb.dma('sp', KC2[64:128, 0:T - 1], g.zTb[512 + g_ * 64:512 + g_ * 64 + 64, 1:T])
        kb.dma('act', VC2[0:64, :], g.zTb[640 + g_ * 64:640 + g_ * 64 + 64, :])
        kb.dma('act', VC2[64:128, 0:T - 1], g.zTb[640 + g_ * 64:640 + g_ * 64 + 64, 1:T])
        for (VA, coff) in ((VsA, g_ * 64), (VwA, 128 + g_ * 64)):
            kb.memset(VA[:, :, 64:65], 1.0, e='pool')
            for j0 in range(0, NT, 16):
                nj = min(16, NT - j0)
                vs = vstage.get()
                kb.dma('sp', vs[:, :nj, :], g.zV[j0 * 128:(j0 + nj) * 128, coff:coff + 64].rr("(j p) d -> p j d", p=128))
                kb.copy(VA[:, j0:j0 + nj, 0:64], vs[:, :nj, :], e='dve')
        if LV <= 2:
            continue
        KC2v = KC2.v.rr("p (n r) -> p n r", r=64)
        VC2v = VC2.v.rr("p (n r) -> p n r", r=64)
        for cc in range(32):
            kb.mm(bankA[:, 0:1], Wck[:, cc, :], pek[:, cc:cc + 1], start=(cc == 0), stop=(cc == 31))
        kb.copy(ckc.v, bankA[:, 0:1], e='dve')
        for cc in range(32):
            kb.mm(bankOc[:, 0:NB], Wck[:, cc, :], KC2v[:, :, 2 * cc], start=(cc == 0), stop=(cc == 31))
        kb.ts(KcT.v, bankOc[:, 0:NB], ckc.v, None, op0=ALU.add)
        for cc in range(32):
            kb.mm(bankA[0:1, 0:64], pev[:, cc:cc + 1], Wcv[:, cc, :], start=(cc == 0), stop=(cc == 31))
        kb.copy(cvr.v, bankA[0:1, 0:64], e='dve')
        for cc in range(32):
            kb.mm(bankOc[:NB, 0:64], VC2v[:, :, 2 * cc], Wcv[:, cc, :], start=(cc == 0), stop=False)
        kb.mm(bankOc[:NB, 0:64], onesb[0:1, :NB], cvr[0:1, :], start=False, stop=True)
        kb.copy(Vc[:NB, :], bankOc[:NB, 0:64], e='dve')

        if LV <= 3:
            continue
        hs0 = slice(0, 64)

        def prologue(i, g_=g_):
            S = Ctx()
            S.i = i

            def chunkA():
                Q4 = qap.get()
                r0 = g_ * 256
                kb.dma('sp', Q4[0:64, :, :], g.zTb[r0:r0 + 256, i * 128:(i + 1) * 128].rr("(h d) t -> d h t", d=64))
                S.Qh = [Q4[0:64, h, :] for h in range(4)]
                S.Q4 = Q4
                GT = gtp.get()
                kb.dma('act', GT.v, g.zV[i * 128:(i + 1) * 128, 256 + g_ * 12:256 + g_ * 12 + 12])
                GS = gsp.get()
                kb.act(GS.v, GT.v, AF.Exp, scale=-1.0)
                kb.ts(GS.v, GS.v, 1.0, None, op0=ALU.add)
                kb.recip(GS.v, GS.v)
                S.G3 = GS.v.rr("p (h k) -> p h k", k=3)
                s_ = stat.get()
                S.s_ = s_
                mx, nmx, sumC, rC = s_[:, 0:4], s_[:, 4:8], s_[:, 8:12], s_[:, 12:16]
                S.rC = rC
                m1, m2, thr = s_[:, 36:44], s_[:, 36:44], s_[:, 44:45]
                psC = bankA.v.rr("p (h n) -> p h n", h=4)[:, :, 0:NB]
                for h in range(4):
                    kb.mm(psC[:, h, :], S.Qh[h], KcT[hs0, :], start=(h == 0), stop=(h == 3), sgc=True)
                sC = scp.get()
                kb.tt(sC.v, psC, TCt[:, g_ * 4:(g_ + 1) * 4, 128 - 2 * i:128 - 2 * i + NB], ALU.add)
                sv, mv = A(sC.v), A(mx)
                kb.op('dve', lambda eng, sv=sv, mv=mv: eng.tensor_reduce(mv, sv, AX.X, ALU.max), [sC], [s_])
                kb.ts(nmx, mx, -1.0, None, op0=ALU.mult)
                eC = ecp.get()
                for h in range(4):
                    kb.act(eC[:, h, :], sC[:, h, :], AF.Exp, bias=nmx[:, h:h + 1], accum_out=sumC[:, h:h + 1])
                kb.recip(rC, sumC)
                if i == 0:
                    kb.ts(rC, rC, RV0.v, None, op0=ALU.mult)
                imp = impp.get()
                kb.ts(imp.v, eC[:, 0, :], rC[:, 0:1], None, op0=ALU.mult)
                for h in range(1, 4):
                    kb.stt(imp.v, eC[:, h, :], rC[:, h:h + 1], imp.v, ALU.mult, ALU.add)
                S.eCb = ecbp.get()
                kb.copy(S.eCb.v, eC.v, e='pool')
                sel = selp.get()
                kb.tt(sel.v, imp.v, ADDT[:, 128 - 2 * i:128 - 2 * i + NB], ALU.add)
                kb.memset(sel[:, 0:1], 1e4, e='dve')
                sa, m1a = A(sel.v), A(m1)
                kb.op('dve', lambda eng, sa=sa, m1a=m1a: eng.max(m1a, sa), [sel], [s_])
                if NB > 16:
                    sel2 = sel2p.get()
                    s2a = A(sel2.v)
                    kb.op('dve', lambda eng, sa=sa, m1a=m1a, s2a=s2a: eng.match_replace(s2a, m1a, sa, -1e30), [sel, s_], [sel2])
                    kb.op('dve', lambda eng, s2a=s2a, m1a=m1a: eng.max(m1a, s2a), [sel2], [s_])
                    kb.ts(thr, m2[:, 7:8], -5e29, None, op0=ALU.max)
                else:
                    kb.memset(thr, -5e29, e='dve')
                S.selm = selmp.get()
                kb.ts(S.selm.v, sel.v, thr, None, op0=ALU.is_ge)

            def chunkB():
                for h in range(4):
                    kb.tr(bankT[:NB, h * 128:(h + 1) * 128], S.eCb[:, h, :], ident.v)
                kb.tr(bankT[:NB, 512:640], S.selm.v, ident.v)
                S.eCT = ectp.get()
                kb.copy(S.eCT[:NB].rr("p h q -> p (h q)"), bankT[:NB, 0:512], e='dve')
                S.negm = negp.get()
                for h in range(4):
                    kb.ts(S.negm[:NB, h, :], bankT[:NB, 512:640], -1.0, 30000.0, op0=ALU.add, op1=ALU.mult,
                          e='dve')

            def chunkC():
                psOc = bankOc.v.rr("p (h d) -> p h d", h=4)[:, :, 0:64]
                for h in range(4):
                    kb.mm(psOc[:, h, :], S.eCT[:NB, h, :], Vc[:NB, :], start=(h == 0), stop=(h == 3), sgc=True)
                S.Oc = ocp.get()
                kb.copy(S.Oc.v, psOc, e='dve')

            return S, [chunkA, chunkB, chunkC]

        def dense_and_epilogue(S, hooks, g_=g_):
            i = S.i
            Os = bOs.v[:, 0:260].rr("p (h d) -> p h d", h=4)
            Ow = bOw.v[:, 0:260].rr("p (h d) -> p h d", h=4)
            tasks = [(0, j) for j in range(0, i + 1)] + [(1, j) for j in range(max(0, i - 4), i + 1)]
            firstj = {0: 0, 1: max(0, i - 4)}
            Pbuf = {}
            DEP = 2

            def emit_S(t):
                br, j = tasks[t]
                KT = KsT if br == 0 else KwT
                STb = stp.get()
                ST = STb.v.rr("p (h q) -> p h q", h=4)
                if br == 0:
                    kb.mm(STb.v, EX[:NB, j * 128:(j + 1) * 128], S.negm[:NB].rr("p h q -> p (h q)"), start=True, stop=False, sgc=True)
                kb.mm(STb.v, KT[:, j * 128:(j + 1) * 128], S.Q4.v.rr("d h q -> d (h q)"),
                      start=(br == 1), stop=True, sgc=True)
                P = pp.get()
                tm = None
                if j == i:
                    tm = DGs[:, g_ * 4:(g_ + 1) * 4, :]
                elif j == i - 1:
                    tm = SDGs[:, g_ * 4:(g_ + 1) * 4, :]
                elif br == 1 and j == i - 4:
                    tm = WEt.v
                if tm is not None:
                    tp = tmpp.get()
                    kb.tt(tp.v, ST, tm, ALU.add)
                    kb.act(P.v, tp.v, AF.Exp)
                else:
                    kb.act(P.v, ST, AF.Exp)
                Pbuf[t] = P

            def emit_PV(t):
                br, j = tasks[t]
                VA, O = (VsA, bOs) if br == 0 else (VwA, bOw)
                P = Pbuf.pop(t)
                kb.mm(O[0:65, :], VA[:, j, :], P.v.rr("p h q -> p (h q)"), start=(j == firstj[br]), stop=(j == i))

            nt = len(tasks)
            for t in range(nt + DEP):
                if t < nt:
                    emit_S(t)
                if t >= DEP:
                    emit_PV(t - DEP)
                if hooks and t in (1, 5, 9):
                    hooks.pop(0)()
            while hooks:
                hooks.pop(0)()
            s_ = S.s_
            rC = S.rC
            rS, rW, aC, aS, aW = s_[:, 16:20], s_[:, 20:24], s_[:, 24:28], s_[:, 28:32], s_[:, 32:36]
            OsT = osp.get()
            OwT = osp.get()
            kb.copy(OsT.v, bOs[0:65, :], e='dve')
            kb.copy(OwT.v, bOw[0:65, :], e='dve')
            for h in range(4):
                kb.tr(bankA[:, h * 65:(h + 1) * 65], OsT[0:65, h * 128:(h + 1) * 128], identf[0:65, 0:65])
            for h in range(4):
                kb.tr(bankOc[:, h * 65:(h + 1) * 65], OwT[0:65, h * 128:(h + 1) * 128], identf[0:65, 0:65])
            OsS = bankA.v[:, 0:260].rr("p (h d) -> p h d", h=4)
            OwS = bankOc.v[:, 0:260].rr("p (h d) -> p h d", h=4)
            kb.recip(rS, OsS[:, :, 64])
            kb.recip(rW, OwS[:, :, 64])
            kb.tt(aC, S.G3[:, :, 0], rC, ALU.mult)
            kb.tt(aS, S.G3[:, :, 1], rS, ALU.mult)
            kb.tt(aW, S.G3[:, :, 2], rW, ALU.mult)
            att = attp.get()
            for h in range(4):
                kb.ts(att[:, h, :], S.Oc[:, h, :], aC[:, h:h + 1], None, op0=ALU.mult, e='pool')
                kb.stt(att[:, h, :], OsS[:, h, 0:64], aS[:, h:h + 1], att[:, h, :], ALU.mult, ALU.add)
                kb.stt(att[:, h, :], OwS[:, h, 0:64], aW[:, h:h + 1], att[:, h, :], ALU.mult, ALU.add)
            attf = att.v.rr("p h d -> p (h d)")
            jk = junkp.get()
            ja, aa, sa_ = A(jk.v), A(attf), A(ssa[:, g_ * NT + i:g_ * NT + i + 1])
            kb.op('dve', lambda eng, ja=ja, aa=aa, sa_=sa_: eng.scalar_tensor_tensor(ja, aa, 1.0, aa, ALU.mult, ALU.mult, accum_out=sa_),
                  [att], [jk, ssa])
            attb = attbp.get()
            kb.copy(attb.v, attf, e='pool')
            for fc in range(2):
                kb.tr(bankT[:, 640 + fc * 128:640 + (fc + 1) * 128], attb[:, fc * 128:(fc + 1) * 128], ident.v)
            attT = attTp.get()
            kb.copy(attT.v.rr("p c q -> p (c q)"), bankT[:, 640:896], e='dve')
            kb.dma('act', g.catT[g_ * 256:(g_ + 1) * 256, i * 128:(i + 1) * 128].rr("(c p) t -> p c t", p=128), attT.v)

        S, hk = prologue(0)
        for f in hk:
            f()
        for i in range(NT):
            if i + 1 < NT:
                Sn, hooks = prologue(i + 1)
            else:
                Sn, hooks = None, []
            dense_and_epilogue(S, hooks)
            S = Sn
    kb.dma('sp', g.ssq[:, 0:2 * NT], ssa.v)
    phase_end(kb)


def bcast_row(view_1xn, p=128):
    ap = view_1xn.ap
    return View(view_1xn.buf, ap.broadcast_to([p, ap.shape[-1]]))


def phase_LRU(kb, g, l):
    T, NT = g.T, g.NT
    phase_begin(kb)
    TC = min(2048, T)
    nchunk = T // TC
    gc = kb.sb([128, NV], F32, "gc")
    kb.dma('sp', gc.v, g.gcols[l])
    onesf = kb.sb([128, 128], F32, "onesf")
    kb.dma('sp', onesf.v, g.ones_tab.v)
    WaBD = kb.sb([128, 4, 128], BF16, "WaBD")
    WxBD = kb.sb([128, 4, 128], BF16, "WxBD")
    kb.memset(WaBD.v, 0.0, e='pool')
    kb.memset(WxBD.v, 0.0, e='pool')
    wst = kb.sb([128, 2, 4, 64], F32, "wst")
    for wi, (src, dst) in enumerate(((g.lru_wa, WaBD), (g.lru_wx, WxBD))):
        for n_ in range(2):
            kb.dma('sp', wst[n_ * 64:(n_ + 1) * 64, wi, :, :], src[l].rr("(c n) d e -> n d c e", n=2)[n_])
        kb.copy(dst[0:64, :, 0:64], wst[0:64, wi, :, :], e='dve')
        kb.copy(dst[64:128, :, 64:128], wst[64:128, wi, :, :], e='dve')
    cA = kb.sb([128, 4], F32, "cA")
    kb.act(cA.v, gc[:, 68:72], AF.Exp, scale=-1.0)
    kb.act(cA.v, cA.v, AF.Ln, bias=1.0)
    kb.ts(cA.v, cA.v, -8.0, None, op0=ALU.mult)
    carry = kb.sb([128, 4], F32, "carry")
    ssl = kb.sb([128, NT], F32, "ssl")
    psS = kb.ps([128, 512], F32, "psS")
    psg = RPool(kb, 4, [128, 512], F32, "psg", ps=True)
    xgp = RPool(kb, 2, [128, TC], F32, "xg")
    xrp = RPool(kb, 2, [128, TC + 3], F32, "xr")
    xcp = RPool(kb, 2, [128, TC], F32, "xc")
    xcbp = RPool(kb, 2, [128, TC], BF16, "xcb")
    rp = RPool(kb, 2, [128, TC], F32, "r")
    igp = RPool(kb, 2, [128, TC], F32, "ig")
    t1p = RPool(kb, 2, [128, TC], F32, "t1")
    t2p = RPool(kb, 2, [128, TC], F32, "t2")
    hhp = RPool(kb, 2, [128, TC], F32, "hh")
    obp = RPool(kb, 2, [128, TC], BF16, "ob")
    for tc in range(nchunk):
        c0 = tc * TC
        for c in range(4):
            xg = xgp.get()
            kb.dma('sp', xg.v, g.zTf[c * 128:(c + 1) * 128, c0:c0 + TC])
            xr = xrp.get()
            if tc == 0:
                kb.memset(xr[:, 0:3], 0.0, e='pool')
                kb.dma('act', xr[:, 3:3 + TC], g.zTf[512 + c * 128:512 + (c + 1) * 128, c0:c0 + TC])
            else:
                kb.dma('act', xr.v, g.zTf[512 + c * 128:512 + (c + 1) * 128, c0 - 3:c0 + TC])
            xc = xcp.get()
            kb.ts(xc.v, xr[:, 3:3 + TC], gc[:, 40 + 3 * 4 + c:40 + 3 * 4 + c + 1], gc[:, 56 + c:57 + c], op0=ALU.mult, op1=ALU.add)
            for k in (2, 1, 0):
                kb.stt(xc.v, xr[:, k:k + TC], gc[:, 40 + k * 4 + c:40 + k * 4 + c + 1], xc.v, ALU.mult, ALU.add)
            xcb = xcbp.get()
            kb.copy(xcb.v, xc.v, e='act')
            r = rp.get()
            ig = igp.get()
            for blk in range(TC // 512):
                cs = slice(blk * 512, (blk + 1) * 512)
                pa = psg.get()
                kb.mm(pa.v, WaBD[:, c, :], xcb[:, cs], start=True, stop=True)
                kb.act(r[:, cs], pa.v, AF.Sigmoid, bias=gc[:, 60 + c:61 + c])
                px = psg.get()
                kb.mm(px.v, WxBD[:, c, :], xcb[:, cs], start=True, stop=True)
                kb.act(ig[:, cs], px.v, AF.Sigmoid, bias=gc[:, 64 + c:65 + c])
            kb.act(r.v, r.v, AF.Exp, scale=cA[:, c:c + 1])
            t1 = t1p.get()
            kb.act(t1.v, r.v, AF.Square)
            kb.act(t1.v, t1.v, AF.Sqrt, scale=-1.0, bias=1.0)
            kb.tt(ig.v, ig.v, xc.v, ALU.mult, e='pool')
            kb.tt(t1.v, t1.v, ig.v, ALU.mult)
            hh = hhp.get()
            init = 0.0 if tc == 0 else A(carry[:, c:c + 1])
            ha, ra, ba = A(hh.v), A(r.v), A(t1.v)
            kb.op('dve', lambda eng, ha=ha, ra=ra, ba=ba, init=init: eng.tensor_tensor_scan(ha, ra, ba, init, ALU.mult, ALU.add),
                  [r, t1, carry], [hh])
            kb.copy(carry[:, c:c + 1], hh[:, TC - 1:TC], e='dve')
            t2 = t2p.get()
            kb.tt(t2.v, xg.v, xg.v, ALU.mult, e='pool')
            kb.ts(t2.v, t2.v, 0.044715, 1.0, op0=ALU.mult, op1=ALU.add, e='pool')
            kb.tt(t2.v, t2.v, xg.v, ALU.mult, e='pool')
            kb.act(t2.v, t2.v, AF.Sigmoid, scale=1.5957691216057308)
            kb.tt(t2.v, t2.v, xg.v, ALU.mult, e='pool')
            kb.tt(hh.v, hh.v, t2.v, ALU.mult)
            ob = obp.get()
            kb.copy(ob.v, hh.v, e='act')
            kb.dma('sp', g.catT[512 + c * 128:512 + (c + 1) * 128, c0:c0 + TC], ob.v)
            kb.tt(t2.v, hh.v, hh.v, ALU.mult, e='pool')
            nb128 = TC // 128
            for b in range(nb128):
                kb.mm(psS[:, b:b + 1], t2[:, b * 128:(b + 1) * 128], onesf[:, 0:1],
                      start=(c == 0 and b == 0), stop=(c == 3 and b == nb128 - 1), sgc=True)
        kb.copy(ssl[:, tc * (TC // 128):(tc + 1) * (TC // 128)], psS[:, 0:TC // 128], e='dve')
    kb.dma('sp', g.ssq[:, 2 * NT:3 * NT], ssl.v)
    phase_end(kb)


def phase_XKV(kb, g, l):
    phase_begin(kb)
    c = Ctx()
    c.wstage = RPool(kb, 3, [128, 2048], F32, "wst")
    c.dq = ['sp', 'act']
    c.dqi = 0
    c.cei = 0
    gc = kb.sb([128, NV], F32, "gc")
    kb.dma('sp', gc.v, g.gcols[l])
    ident = load_const_bf16(kb, c, g.ident.v, [128, 128], "ident")
    Wkv = kb.sb([128, 8, 2048], BF16, "Wkv")
    for k in range(8):
        wload(kb, c, Wkv[:, k, :], g.xkv[l, k * 128:(k + 1) * 128, :], gc[:, 24 + k:25 + k])
    c.stat = RPool(kb, 2, [128, 4], F32, "stat")
    c.junk = RPool(kb, 1, [128, 1024], F32, "junk")
    c.hb = RPool(kb, 2, [128, 1024], BF16, "hb")
    c.pT = RPool(kb, 1, [128, 1024], BF16, "pT", ps=True)
    mnT = kb.sb([128, 8, 256], BF16, "mnT")
    mp = RPool(kb, 2, [128, 1024], F32, "memt")
    for s in range(2):
        mt = mp.get()
        kb.dma('sp', mt.v, g.mem[s * 128:(s + 1) * 128, :])
        norm_T(kb, c, mt.v, mnT[:, :, s * 128:(s + 1) * 128], ident)
    psp = RPool(kb, 3, [128, 512], F32, "psx", ps=True)
    ob = RPool(kb, 3, [128, 512], BF16, "ob")
    for fc in range(8):
        p = psp.get()
        for k in range(8):
            kb.mm(p[:, 0:256], Wkv[:, k, fc * 128:(fc + 1) * 128], mnT[:, k, :], start=(k == 0), stop=(k == 7))
        o = ob.get()
        kb.copy(o[:, 0:256], p[:, 0:256], e='dve')
        kb.dma('sp', g.ckT[fc * 128:(fc + 1) * 128, :], o[:, 0:256])
    for mc in range(2):
        for half in range(2):
            p = psp.get()
            for k in range(8):
                kb.mm(p.v, mnT[:, k, mc * 128:(mc + 1) * 128], Wkv[:, k, 1024 + half * 512:1024 + (half + 1) * 512],
                      start=(k == 0), stop=(k == 7))
            o = ob.get()
            kb.copy(o.v, p.v, e='act')
            kb.dma('sp', g.cv[mc * 128:(mc + 1) * 128, half * 512:(half + 1) * 512], o.v)
    phase_end(kb)


def phase_R1(kb, g, l, xsrc):
    T, NT = g.T, g.NT
    phase_begin(kb)
    c = Ctx()
    c.wstage = RPool(kb, 2, [128, 1024], F32, "wst")
    c.dq = ['sp', 'act']
    c.dqi = 0
    c.cei = 0
    gc = kb.sb([128, NV], F32, "gc")
    kb.dma('sp', gc.v, g.gcols[l])
    ident = load_const_bf16(kb, c, g.ident.v, [128, 128], "ident")
    onesb = load_const_bf16(kb, c, g.ones_tab.v, [128, 128], "onesb")
    WoA = kb.sb([128, 4, 1024], BF16, "WoA")
    WoL = kb.sb([128, 4, 1024], BF16, "WoL")
    Wq = kb.sb([128, 8, 1024], BF16, "Wq")
    Wo = kb.sb([128, 8, 1024], BF16, "Wo")
    for k in range(4):
        wload(kb, c, WoA[:, k, :], g.w_out[l, k * 128:(k + 1) * 128, :], gc[:, 32 + k:33 + k])
        wload(kb, c, WoL[:, k, :], g.w_out[l, 512 + k * 128:512 + (k + 1) * 128, :], gc[:, 36 + k:37 + k])
    for k in range(8):
        wload(kb, c, Wq[:, k, :], g.xq[l, k * 128:(k + 1) * 128, :], gc[:, 8 + k:9 + k], 1.0 / 16.0)
        wload(kb, c, Wo[:, k, :], g.xo[l, k * 128:(k + 1) * 128, :])
    ckT = kb.sb([128, 8, 256], BF16, "ckTs")
    kb.dma('sp', ckT.v, g.ckT.v.rr("(c p) m -> p c m", p=128))
    cv = kb.sb([128, 2, 1024], BF16, "cvs")
    kb.dma('act', cv.v, g.cv.v.rr("(c p) f -> p c f", p=128))
    GP1 = kb.sb([128, 1024], F32, "GP1")
    GP2 = kb.sb([128, 1024], F32, "GP2")
    kb.dma('sp', GP1.v, bcast_row(g.grows[l, 0:1, :]))
    kb.dma('act', GP2.v, bcast_row(g.grows[l, 1:2, :]))
    SS = kb.sb([128, 3 * NT], F32, "SS")
    kb.dma('sp', SS.v, g.ssq.v)
    rA = kb.sb([128, NT], F32, "rA")
    rL = kb.sb([128, NT], F32, "rL")
    kb.tt(rA.v, SS[:, 0:NT], SS[:, NT:2 * NT], ALU.add)
    rstd_from_ss(kb, rA.v, 512)
    kb.copy(rL.v, SS[:, 2 * NT:3 * NT], e='dve')
    rstd_from_ss(kb, rL.v, 512)
    c.stat = RPool(kb, 4, [128, 4], F32, "stat")
    c.junk = RPool(kb, 1, [128, 1024], F32, "junk")
    c.hb = RPool(kb, 2, [128, 1024], BF16, "hb")
    c.pT = RPool(kb, 1, [128, 1024], BF16, "pT", ps=True)
    MT = min(512, T)
    nsub = MT // 128
    P1 = kb.ps([128, 1024], F32, "P1")
    P2 = kb.ps([128, 1024], F32, "P2")
    psp = RPool(kb, 3, [128, 512], F32, "psr", ps=True)
    catp = RPool(kb, 2, [128, 8, MT], BF16, "cat")
    x4p = RPool(kb, 2, [128, nsub, 1024], F32, "x4")
    hTp = RPool(kb, 1, [128, 8, MT], BF16, "hT")
    cqp = RPool(kb, 1, [128, 8, MT], BF16, "cqT")
    cop = RPool(kb, 1, [128, 8, MT], BF16, "coT")
    ptp = RPool(kb, 4, [128, MT], BF16, "PT")
    rdp = RPool(kb, 2, [128, MT], F32, "rden")
    mxp = RPool(kb, 2, [128, 1024], F32, "mixed")
    xop = RPool(kb, 2, [128, 1024], F32, "xo")
    for m in range(T // MT):
        t0 = m * MT
        cat = catp.get()
        kb.dma('sp', cat.v, g.catT[:, t0:t0 + MT].rr("(c p) t -> p c t", p=128))
        x4 = x4p.get()
        kb.dma('act', x4.v, xsrc[t0:t0 + MT, :].rr("(s p) d -> p s d", p=128))
        hT = hTp.get()

        def Wout(s):
            ts_ = slice(s * 128, (s + 1) * 128)
            for half in range(2):
                hs = slice(half * 512, (half + 1) * 512)
                for k in range(4):
                    kb.mm(P1[:, hs], cat[:, k, ts_], WoA[:, k, hs], start=(k == 0), stop=(k == 3))
                for k in range(4):
                    kb.mm(P2[:, hs], cat[:, 4 + k, ts_], WoL[:, k, hs], start=(k == 0), stop=(k == 3))

        def chainA(s):
            ti = m * nsub + s
            mx = mxp.get()
            kb.act(mx.v, P1.v, AF.Copy, scale=rA[:, ti:ti + 1])
            kb.stt(mx.v, P2.v, rL[:, ti:ti + 1], mx.v, ALU.mult, ALU.add)
            st = c.stat.get()
            jk = c.junk.get()
            kb.act(jk.v, mx.v, AF.Square, accum_out=st[:, 0:1])
            rstd_from_ss(kb, st[:, 0:1], D)
            kb.stt(mx.v, mx.v, st[:, 0:1], GP1.v, ALU.mult, ALU.mult)
            kb.tt(x4[:, s, :], x4[:, s, :], mx.v, ALU.add, e='dve')
            return norm_T1(kb, c, x4[:, s, :])

        Wout(0)
        for s in range(nsub):
            hb_ = chainA(s)
            if s + 1 < nsub:
                Wout(s + 1)
            norm_T2(kb, c, hb_, hT[:, :, s * 128:(s + 1) * 128], ident)
        cqT = cqp.get()
        for fc in range(8):
            p = psp.get()
            for k in range(8):
                kb.mm(p[:, :MT], Wq[:, k, fc * 128:(fc + 1) * 128], hT[:, k, :], start=(k == 0), stop=(k == 7))
            kb.copy(cqT[:, fc, :], p[:, :MT], e=('dve' if fc % 2 else 'act'))
        coT = cop.get()
        for hx in range(4):
            PTs = []
            for mc in range(2):
                p = psp.get()
                for f2 in range(2):
                    kb.mm(p[:, :MT], ckT[:, 2 * hx + f2, mc * 128:(mc + 1) * 128], cqT[:, 2 * hx + f2, :],
                          start=(f2 == 0), stop=(f2 == 1))
                PT = ptp.get()
                kb.act(PT.v, p[:, :MT], AF.Exp)
                PTs.append(PT)
            p = psp.get()
            for mc in range(2):
                kb.mm(p[:, :MT], onesb.v, PTs[mc].v, start=(mc == 0), stop=(mc == 1))
            rden = rdp.get()
            kb.recip(rden.v, p[:, :MT])
            for dvc in range(2):
                p = psp.get()
                for mc in range(2):
                    kb.mm(p[:, :MT], cv[:, mc, hx * 256 + dvc * 128:hx * 256 + (dvc + 1) * 128], PTs[mc].v,
                          start=(mc == 0), stop=(mc == 1))
                kb.tt(coT[:, 2 * hx + dvc, :], p[:, :MT], rden.v, ALU.mult)
        for s in range(nsub):
            ts_ = slice(s * 128, (s + 1) * 128)
            for half in range(2):
                hs = slice(half * 512, (half + 1) * 512)
                for k in range(8):
                    kb.mm(P1[:, hs], coT[:, k, ts_], Wo[:, k, hs], start=(k == 0), stop=(k == 7))
            st = c.stat.get()
            jk = c.junk.get()
            kb.act(jk.v, P1.v, AF.Square, accum_out=st[:, 0:1])
            rstd_from_ss(kb, st[:, 0:1], D)
            xo = xop.get()
            kb.stt(xo.v, P1.v, st[:, 0:1], GP2.v, ALU.mult, ALU.mult)
            kb.tt(xo.v, xo.v, x4[:, s, :], ALU.add, e='dve')
            kb.dma('sp', g.xmid[t0 + s * 128:t0 + (s + 1) * 128, :], xo.v)
    phase_end(kb)


def phase_R2(kb, g, l, xdst):
    T, NT = g.T, g.NT
    phase_begin(kb)
    c = Ctx()
    c.wstage = RPool(kb, 2, [128, 1024], F32, "wst")
    c.dq = ['sp', 'act']
    c.dqi = 0
    c.cei = 0
    gc = kb.sb([128, NV], F32, "gc")
    kb.dma('sp', gc.v, g.gcols[l])
    ident = load_const_bf16(kb, c, g.ident.v, [128, 128], "ident")
    W1 = kb.sb([128, 8, 4096], BF16, "W1")
    W2 = kb.sb([128, 32, 1024], BF16, "W2")
    for k in range(8):
        for q in range(4):
            wload(kb, c, W1[:, k, q * 1024:(q + 1) * 1024], g.mlp_w1[l, k * 128:(k + 1) * 128, q * 1024:(q + 1) * 1024],
                  gc[:, 16 + k:17 + k])
    for k in range(32):
        wload(kb, c, W2[:, k, :], g.mlp_w2[l, k * 128:(k + 1) * 128, :])
    GP3 = kb.sb([128, 1024], F32, "GP3")
    kb.dma('sp', GP3.v, bcast_row(g.grows[l, 2:3, :]))
    c.stat = RPool(kb, 8, [128, 4], F32, "stat")
    c.junk = RPool(kb, 1, [128, 1024], F32, "junk")
    c.hb = RPool(kb, 4, [128, 1024], BF16, "hb")
    c.pT = RPool(kb, 1, [128, 1024], BF16, "pT", ps=True)
    MT = 256 if T % 256 == 0 else 128
    nsub = MT // 128
    Py = kb.ps([128, 1024], F32, "Py")
    psp = RPool(kb, 4, [128, 512], F32, "psm", ps=True)
    x2p = RPool(kb, 2, [128, nsub, 1024], F32, "x2")
    hTp = RPool(kb, 2, [128, 8, MT], BF16, "hT")
    utp = RPool(kb, 1, [128, 32, MT], BF16, "uT")
    rlp = RPool(kb, 3, [128, MT], F32, "rl")
    xop = RPool(kb, 2, [128, 1024], F32, "xo")
    NM = T // MT

    def A1(m):
        x2 = x2p.get()
        kb.dma('sp', x2.v, g.xmid[m * MT:(m + 1) * MT, :].rr("(s p) d -> p s d", p=128))
        return x2, [norm_T1(kb, c, x2[:, s_, :]) for s_ in range(nsub)]

    def A2(hbs):
        hT = hTp.get()
        for s_ in range(nsub):
            norm_T2(kb, c, hbs[s_], hT[:, :, s_ * 128:(s_ + 1) * 128], ident)
        return hT

    x2, hbs = A1(0)
    hT = A2(hbs)
    for m in range(NM):
        t0 = m * MT
        uT = utp.get()
        nxt = None
        for fc in range(32):
            p = psp.get()
            for k in range(8):
                kb.mm(p[:, :MT], W1[:, k, fc * 128:(fc + 1) * 128], hT[:, k, :], start=(k == 0), stop=(k == 7))
            rl = rlp.get()
            kb.act(rl.v, p[:, :MT], AF.Relu)
            kb.tt(uT[:, fc, :], rl.v, rl.v, ALU.mult, e='dve')
            if fc == 6 and m + 1 < NM:
                nxt = A1(m + 1)
        if nxt is not None:
            hTn = A2(nxt[1])
        for s_ in range(nsub):
            for half in range(2):
                hs = slice(half * 512, (half + 1) * 512)
                for k in range(32):
                    kb.mm(Py[:, hs], uT[:, k, s_ * 128:(s_ + 1) * 128], W2[:, k, hs], start=(k == 0), stop=(k == 31))
            st = c.stat.get()
            jk = c.junk.get()
            kb.act(jk.v, Py.v, AF.Square, accum_out=st[:, 0:1])
            rstd_from_ss(kb, st[:, 0:1], D)
            xo = xop.get()
            kb.stt(xo.v, Py.v, st[:, 0:1], GP3.v, ALU.mult, ALU.mult)
            kb.tt(xo.v, xo.v, x2[:, s_, :], ALU.add, e='dve')
            kb.dma('act', xdst[t0 + s_ * 128:t0 + (s_ + 1) * 128, :], xo.v)
        if nxt is not None:
            x2, hT = nxt[0], hTn
    phase_end(kb)


def t5_bucket_np(d):
    d = np.maximum(d, 0)
    df = np.maximum(d, 1).astype(np.float32)
    large = 16 + (np.log(df / np.float32(16)) / np.float32(math.log(8.0)) * np.float32(16)).astype(np.int32)
    large = np.minimum(large, 31)
    return np.where(d < 16, d, large)


def host_tables(rel_bias):
    rb = np.asarray(rel_bias, np.float32)
    q = np.arange(128)
    tabs = {}
    jp = np.arange(256)
    dist = q[:, None] + 64 * (128 - jp[None, :]) - 63
    bk = t5_bucket_np(dist)
    tc = rb[bk]
    tc = np.where((dist >= 0)[:, :, None], tc, np.float32(NEGM)).transpose(0, 2, 1)
    tabs["tc_tab"] = np.ascontiguousarray(tc, np.float32).reshape(128, 8 * 256)
    k = np.arange(128)
    dist = q[None, :] - k[:, None]
    dg = rb[t5_bucket_np(dist)]
    dg = np.where((dist >= 0)[:, :, None], dg, np.float32(NEGM)).transpose(0, 2, 1)
    tabs["dg_tab"] = np.ascontiguousarray(dg, np.float32).reshape(128, 8 * 128)
    dist = 128 + q[None, :] - k[:, None]
    sdg = rb[t5_bucket_np(dist)].transpose(0, 2, 1)
    tabs["sdg_tab"] = np.ascontiguousarray(sdg, np.float32).reshape(128, 8 * 128)
    c31 = np.broadcast_to(rb[31][None, :, None], (128, 8, 128))
    tabs["c31_tab"] = np.ascontiguousarray(c31, np.float32).reshape(128, 8 * 128)
    we = np.where(k[:, None] > q[None, :], np.float32(0), np.float32(NEGM))
    tabs["we_tab"] = np.ascontiguousarray(np.broadcast_to(we[:, None, :], (128, 4, 128)), np.float32).reshape(128, 512)
    rel = (jp[None, :] - 128) - (q[:, None] >= 64)
    addt = np.where(rel > 0, np.float32(-1e30), np.where(rel >= -1, np.float32(1e4), np.float32(0)))
    tabs["addt_tab"] = np.ascontiguousarray(addt, np.float32)
    tabs["rv0_tab"] = (q >= 63).astype(np.float32).reshape(128, 1)
    tabs["ident"] = np.eye(128, dtype=np.float32)
    bm = np.zeros((4, 4, 128), np.float32)
    for h in range(4):
        bm[h, h, :] = 1
    tabs["ones_tab"] = np.ones((128, 128), np.float32)
    return tabs


def host_layer_tables(inp, L):
    def col(v, nch):
        return np.asarray(v, np.float32)[:L].reshape(L, nch, 128).transpose(0, 2, 1)
    cols = [col(inp["ln_mix_pre"], 8), col(inp["ln_x_pre"], 8), col(inp["ln_mlp_pre"], 8), col(inp["ln_mem"], 8),
            col(inp["gn_attn"], 4), col(inp["gn_lru"], 4)]
    cw = np.asarray(inp["conv_w"], np.float32)[:L]
    cols.append(cw.reshape(L, 4, 4, 128).transpose(0, 3, 1, 2).reshape(L, 128, 16))
    for nm in ["conv_b", "lru_ba", "lru_bx", "lru_lambda"]:
        cols.append(col(inp[nm], 4))
    gcols = np.ascontiguousarray(np.concatenate(cols, axis=2), np.float32)
    assert gcols.shape == (L, 128, NV)
    grows = np.ascontiguousarray(np.stack([inp["ln_mix_post"][:L], inp["ln_x_post"][:L], inp["ln_mlp_post"][:L]], axis=1), np.float32)
    out = {"gcols": gcols, "grows": grows}
    for nm, key in [("pe2k", "cmp_pe_k"), ("pe2v", "cmp_pe_v")]:
        pe = np.asarray(inp[key], np.float32)[:L]
        out[nm] = np.ascontiguousarray(pe.reshape(L, 32, 2, 64).transpose(0, 2, 3, 1).reshape(L, 128, 32))
    return out


WNAMES = [("w_in", [1024, 2328]), ("cmp_w_k", [4096, 64]), ("cmp_w_v", [4096, 64]), ("lru_wa", [8, 64, 64]),
          ("lru_wx", [8, 64, 64]), ("w_out", [1024, 1024]), ("xq", [1024, 1024]), ("xkv", [1024, 2048]),
          ("xo", [1024, 1024]), ("mlp_w1", [1024, 4096]), ("mlp_w2", [4096, 1024])]
TABS = [("tc_tab", [128, 2048]), ("dg_tab", [128, 1024]), ("sdg_tab", [128, 1024]), ("c31_tab", [128, 1024]),
        ("we_tab", [128, 512]), ("addt_tab", [128, 256]), ("rv0_tab", [128, 1]), ("ident", [128, 128]),
        ("ones_tab", [128, 128])]


def build(T, L, debug=False, stop_after=None):
    nc = bass.Bass("TRN2", target_bir_lowering=False)
    es = ExitStack()
    with es:
        kb = KB(nc, es)
        g = Ctx()
        g.T, g.NT, g.NB, g.L = T, T // 128, T // 64, L
        g.x = kb.dram("x", [T, D], F32, kind="ExternalInput")
        g.mem = kb.dram("mem", [256, D], F32, kind="ExternalInput")
        for nm, shp in WNAMES:
            setattr(g, nm, kb.dram(nm, [L] + shp, F32, kind="ExternalInput"))
        for nm, shp in TABS:
            setattr(g, nm, kb.dram(nm, shp, F32, kind="ExternalInput"))
        g.gcols = kb.dram("gcols", [L, 128, NV], F32, kind="ExternalInput")
        g.grows = kb.dram("grows", [L, 3, D], F32, kind="ExternalInput")
        g.pe2k = kb.dram("pe2k", [L, 128, 32], F32, kind="ExternalInput")
        g.pe2v = kb.dram("pe2v", [L, 128, 32], F32, kind="ExternalInput")
        g.out = kb.dram("out", [T, D], F32, kind="ExternalOutput")
        sk = "ExternalOutput" if debug else "Internal"
        g.zTb = kb.dram("zTb", [1024, T], BF16, kind=sk)
        g.zTf = kb.dram("zTf", [1024, T], F32, kind=sk)
        g.zV = kb.dram("zV", [T, 280], F32, kind=sk)
        g.catT = kb.dram("catT", [1024, T], BF16, kind=sk)
        g.ssq = kb.dram("ssq", [128, 3 * g.NT], F32, kind=sk)
        g.ckT = kb.dram("ckT", [1024, 256], BF16, kind=sk)
        g.cv = kb.dram("cv", [256, 1024], BF16, kind=sk)
        g.xmid = kb.dram("xmid", [T, D], F32, kind=sk)
        g.xres = kb.dram("xres", [T, D], F32, kind=sk)
        kb.cur_phase = 0
        phases = []
        for l in range(L):
            xsrc = g.x if l == 0 else g.xres
            xdst = g.out if l == L - 1 else g.xres
            phases += [("M1", lambda l=l, xsrc=xsrc: phase_M1(kb, g, l, xsrc)),
                       ("ATT", lambda l=l: phase_ATT(kb, g, l)),
                       ("LRU", lambda l=l: phase_LRU(kb, g, l)),
                       ("XKV", lambda l=l: phase_XKV(kb, g, l)),
                       ("R1", lambda l=l, xsrc=xsrc: phase_R1(kb, g, l, xsrc)),
                       ("R2", lambda l=l, xdst=xdst: phase_R2(kb, g, l, xdst))]
        for i, (nm, fn) in enumerate(phases):
            kb.cur_phase = i + 1
            fn()
            if stop_after is not None and i + 1 >= stop_after:
                break
    return nc


def make_inmaps(inputs, T, L, ncores):
    tabs = host_tables(inputs["rel_bias"])
    lt = host_layer_tables(inputs, L)
    shared = {}
    for nm, shp in WNAMES:
        shared[nm] = np.ascontiguousarray(np.asarray(inputs[nm], np.float32)[:L].reshape([L] + shp))
    shared.update(tabs)
    shared.update(lt)
    maps = []
    B = inputs["x"].shape[0]
    for c in range(ncores):
        b = c % B
        m = dict(shared)
        m["x"] = np.ascontiguousarray(np.asarray(inputs["x"], np.float32)[b, :T])
        m["mem"] = np.ascontiguousarray(np.asarray(inputs["mem"], np.float32)[b])
        maps.append(m)
    return maps


_NC_CACHE = {}


def kernel(**inputs):
    T, L = 8192, 2
    B = inputs["x"].shape[0]
    key = (T, L)
    if key not in _NC_CACHE:
        _NC_CACHE[key] = build(T, L)
    nc = _NC_CACHE[key]
    ncores = 8
    maps = make_inmaps(inputs, T, L, ncores)
    res = run_bass_kernel_spmd(nc, maps, core_ids=list(range(ncores)))
    out = np.stack([np.asarray(res.results[b]["out"], np.float32) for b in range(B)], axis=0)
    return out
```

```python
import numpy as np
from contextlib import ExitStack
import concourse.bass as bass
import concourse.mybir as mybir
from concourse.bass_utils import run_bass_kernel_spmd

F32 = mybir.dt.float32
BF16 = mybir.dt.bfloat16
AF = mybir.ActivationFunctionType
ALU = mybir.AluOpType
AX = mybir.AxisListType


class TL:
    def __init__(self, sem, step):
        self.sem = sem
        self.step = step
        self.val = 0


class Buf:
    def __init__(self, t, name, space):
        self.t = t
        self.name = name
        self.space = space
        self.last_w = None
        self.readers = {}
        self.dtl = None

    def __getitem__(self, idx):
        return View(self, self.t[idx])

    @property
    def v(self):
        return View(self, self.t[:])


class View:
    def __init__(self, buf, ap):
        self.buf = buf
        self.ap = ap

    def __getitem__(self, idx):
        return View(self.buf, self.ap[idx])

    def rr(self, pat, **kw):
        return View(self.buf, self.ap.rearrange(pat, **kw))

    def bc(self, shape):
        return View(self.buf, self.ap.broadcast_to(shape))


def _bufs(views):
    out = []
    for v in views:
        if v is None:
            continue
        if isinstance(v, Buf):
            out.append(v)
        elif isinstance(v, View):
            out.append(v.buf)
    return out


def A(v):
    if isinstance(v, View):
        return v.ap
    if isinstance(v, Buf):
        return v.t[:]
    return v


ENGS = ['pe', 'act', 'dve', 'pool', 'sp']


class KB:
    def __init__(self, nc, es):
        self.nc = nc
        self.es = es
        self.tl = {}
        for e in ENGS:
            self.tl[e] = TL(es.enter_context(nc.semaphore("s_" + e)), 1)
        self.prog = {e: [] for e in ENGS}
        self.seen = {e: {} for e in ENGS}
        self.dtls = []
        self.free_dtls = []
        self.phase_dtls = []
        self.root_es = es
        self.nbuf = 0
        self.cur_phase = 0

    def sb(self, shape, dtype, name=None):
        self.nbuf += 1
        name = (name or "sb") + "_%d" % self.nbuf
        t = self.es.enter_context(self.nc.sbuf_tensor(name, list(shape), dtype))
        b = Buf(t, name, 'sb')
        b.phase = self.cur_phase
        return b

    def ps(self, shape, dtype, name=None):
        self.nbuf += 1
        name = (name or "ps") + "_%d" % self.nbuf
        t = self.es.enter_context(self.nc.psum_tensor(name, list(shape), dtype))
        b = Buf(t, name, 'ps')
        b.phase = self.cur_phase
        return b

    def dram(self, name, shape, dtype, kind="Internal"):
        t = self.nc.dram_tensor(name, list(shape), dtype, kind=kind)
        return Buf(t, name, 'dram')

    def sub(self, buf, idx, name=None):
        b = Buf(None, name or (buf.name + "_sub"), buf.space)
        b.t = _Sub(buf.t[idx])
        return b

    def _waits(self, e, reads, writes, own_tl=None):
        need = {}

        def add(tl, val):
            if need.get(tl, 0) < val:
                need[tl] = val
        mytl = self.tl[e]
        for b in reads:
            if b.last_w:
                add(*b.last_w)
        for b in writes:
            if b.last_w:
                tl, val = b.last_w
                if (tl is mytl and e == 'pe') or (own_tl is not None and tl is own_tl):
                    pass
                else:
                    add(tl, val)
            for tl, val in b.readers.items():
                if tl is mytl and e == 'pe':
                    continue
                add(tl, val)
        waits = []
        for tl, val in need.items():
            if tl.step == 16:
                val = tl.val
            if self.seen[e].get(tl, 0) >= val:
                continue
            self.seen[e][tl] = val
            waits.append((tl, val))
        return waits

    def op(self, e, fn, reads, writes):
        reads = _bufs(reads)
        writes = _bufs(writes)
        waits = self._waits(e, reads, writes)
        tl = self.tl[e]
        tl.val += 1
        v = tl.val
        self.prog[e].append((waits, fn, tl.sem, 1))
        for b in reads:
            b.readers[tl] = v
        for b in writes:
            b.last_w = (tl, v)
            b.readers = {}

    def dma(self, e, out, in_, **kw):
        ob = out.buf if isinstance(out, View) else out
        ib = in_.buf if isinstance(in_, View) else in_
        side = ob if ob.space != 'dram' else ib
        if side.dtl is None:
            if self.free_dtls:
                side.dtl = self.free_dtls.pop()
            else:
                side.dtl = TL(self.root_es.enter_context(self.nc.semaphore("d%d" % len(self.dtls))), 16)
                self.dtls.append(side.dtl)
            if getattr(side, 'phase', 0) == self.cur_phase and self.cur_phase > 0:
                self.phase_dtls.append(side.dtl)
        dtl = side.dtl
        waits = self._waits(e, [ib], [ob], own_tl=dtl)
        dtl.val += 16
        v = dtl.val
        oa, ia = A(out), A(in_)
        self.prog[e].append((waits, lambda eng: eng.dma_start(out=oa, in_=ia, **kw), dtl.sem, 16))
        ib.readers[dtl] = v
        ob.last_w = (dtl, v)
        ob.readers = {}

    def collective(self, out, in_, groups, kind="AllGather"):
        ob, ib = out.buf, in_.buf
        if ob.dtl is None:
            ob.dtl = TL(self.root_es.enter_context(self.nc.semaphore("cc%d" % len(self.dtls))), 16)
            self.dtls.append(ob.dtl)
        dtl = ob.dtl
        waits = self._waits('pool', [ib], [ob], own_tl=None)
        dtl.val += 16
        v = dtl.val
        oa, ia = A(out), A(in_)
        self.prog['pool'].append((waits, lambda eng: eng.collective_compute(kind, ALU.bypass, replica_groups=groups,
                                                                          ins=[ia], outs=[oa]), dtl.sem, 16))
        ib.readers[dtl] = v
        ob.last_w = (dtl, v)
        ob.readers = {}

    def barrier(self):
        for e in ENGS:
            waits = []
            for tl in list(self.tl.values()) + self.dtls:
                if tl.val > 0 and self.seen[e].get(tl, 0) < tl.val and tl is not None:
                    self.seen[e][tl] = tl.val
                    waits.append((tl, tl.val))
            self.prog[e].append((waits, None, None, 0))

    def emit_block(self):
        nc = self.nc
        prog = self.prog
        self.prog = {e: [] for e in ENGS}
        self.free_dtls.extend(self.phase_dtls)
        self.phase_dtls = []
        with nc.Block() as block:
            def run(eng, lst):
                for waits, fn, sem, inc in lst:
                    for tl, val in waits:
                        eng.wait_ge(tl.sem, val)
                    if fn is not None:
                        ins = fn(eng)
                        ins.then_inc(sem, inc)

            @block.tensor
            def _(eng):
                run(eng, prog['pe'])

            @block.scalar
            def _(eng):
                run(eng, prog['act'])

            @block.vector
            def _(eng):
                run(eng, prog['dve'])

            @block.gpsimd
            def _(eng):
                run(eng, prog['pool'])

            @block.sync
            def _(eng):
                run(eng, prog['sp'])

    def mm(self, out, lhsT, rhs, start, stop=True, sgc=False, **kw):
        oa, la, ra = A(out), A(lhsT), A(rhs)
        if sgc:
            kw['skip_group_check'] = True
        self.op('pe', lambda eng: eng.matmul(oa, la, ra, start=start, stop=stop, **kw),
                [lhsT, rhs], [out])

    def tr(self, out, in_, ident):
        oa, ia, da = A(out), A(in_), A(ident)
        self.op('pe', lambda eng: eng.transpose(oa, ia, da), [in_, ident], [out])

    def act(self, out, in_, func, bias=None, scale=None, accum_out=None, e='act'):
        oa, ia = A(out), A(in_)
        kw = {}
        rd = [in_]
        if bias is not None:
            kw['bias'] = A(bias)
            rd.append(bias)
        if scale is not None:
            kw['scale'] = A(scale)
            rd.append(scale)
        wr = [out]
        if accum_out is not None:
            kw['accum_out'] = A(accum_out)
            wr.append(accum_out)
        self.op(e, lambda eng: eng.activation(oa, ia, func, **kw), rd, wr)

    def tt(self, out, in0, in1, op, e='dve'):
        oa, a0, a1 = A(out), A(in0), A(in1)
        self.op(e, lambda eng: eng.tensor_tensor(oa, a0, a1, op), [in0, in1], [out])

    def ts(self, out, in0, s1, s2=None, op0=None, op1=None, e='dve', accum_out=None):
        oa, a0 = A(out), A(in0)
        rd = [in0, s1, s2]
        kw = {}
        wr = [out]
        if accum_out is not None:
            kw['accum_out'] = A(accum_out)
            wr.append(accum_out)
        a1, a2 = A(s1), A(s2)
        if op1 is None:
            self.op(e, lambda eng: eng.tensor_scalar(oa, a0, a1, None, op0, **kw), rd, wr)
        else:
            self.op(e, lambda eng: eng.tensor_scalar(oa, a0, a1, a2, op0, op1, **kw), rd, wr)

    def stt(self, out, in0, scalar, in1, op0, op1, e='dve'):
        oa, a0, sc, a1 = A(out), A(in0), A(scalar), A(in1)
        self.op(e, lambda eng: eng.scalar_tensor_tensor(oa, a0, sc, a1, op0, op1), [in0, scalar, in1], [out])

    def copy(self, out, in_, e='dve'):
        oa, ia = A(out), A(in_)
        if e == 'act':
            self.op(e, lambda eng: eng.copy(oa, ia), [in_], [out])
        else:
            self.op(e, lambda eng: eng.tensor_copy(oa, ia), [in_], [out])

    def memset(self, out, val, e='pool'):
        oa = A(out)
        self.op(e, lambda eng: eng.memset(oa, val), [], [out])

    def recip(self, out, in_, e='dve'):
        oa, ia = A(out), A(in_)
        self.op(e, lambda eng: eng.reciprocal(oa, ia), [in_], [out])


class _Sub:
    def __init__(self, ap):
        self.ap = ap

    def __getitem__(self, idx):
        return self.ap[idx]

import math

EPS = 1e-6
NEGM = -30000.0
D = 1024
NV = 72
O_Q, O_KC, O_VC, O_KS, O_VS, O_KW, O_VW, O_GT, O_XG, O_XR = 0, 512, 640, 768, 896, 1024, 1152, 1280, 1304, 1816


class RPool:
    def __init__(self, kb, n, shape, dtype, name, ps=False):
        mk = kb.ps if ps else kb.sb
        self.bufs = [mk(shape, dtype, "%s%d" % (name, i)) for i in range(n)]
        self.i = 0

    def get(self):
        b = self.bufs[self.i % len(self.bufs)]
        self.i += 1
        return b


def phase_begin(kb):
    kb.pes = ExitStack()
    kb.pes.__enter__()
    kb.outer_es = kb.es
    kb.es = kb.pes
    kb.phase_dtls = []


def phase_end(kb):
    kb.barrier()
    kb.emit_block()
    kb.es = kb.outer_es
    kb.pes.__exit__(None, None, None)


class Ctx:
    pass


def wload(kb, c, dst, src, gcol=None, mul=None):
    n = src.ap.shape[-1]
    st = c.wstage.get()
    q = c.dq[c.dqi % 2]
    c.dqi += 1
    kb.dma(q, st[:, :n], src)
    e = ['dve', 'pool'][c.cei % 2]
    c.cei += 1
    if gcol is not None and mul is not None:
        kb.ts(dst, st[:, :n], gcol, float(mul), op0=ALU.mult, op1=ALU.mult, e=e)
    elif gcol is not None:
        kb.ts(dst, st[:, :n], gcol, None, op0=ALU.mult, e=e)
    elif mul is not None:
        kb.ts(dst, st[:, :n], float(mul), None, op0=ALU.mult, e=e)
    else:
        kb.copy(dst, st[:, :n], e=e)


def rstd_from_ss(kb, ss, n):
    kb.ts(ss, ss, 1.0 / n, EPS, op0=ALU.mult, op1=ALU.add)
    kb.act(ss, ss, AF.Sqrt)
    kb.recip(ss, ss)


def norm_T1(kb, c, xv):
    st = c.stat.get()
    junk = c.junk.get()
    kb.act(junk.v, xv, AF.Square, accum_out=st[:, 0:1])
    rstd_from_ss(kb, st[:, 0:1], D)
    hb = c.hb.get()
    kb.ts(hb.v, xv, st[:, 0:1], None, op0=ALU.mult)
    return hb


def norm_T2(kb, c, hb, hT_dst, ident):
    pT = c.pT.get()
    for k in range(8):
        kb.tr(pT[:, k * 128:(k + 1) * 128], hb[:, k * 128:(k + 1) * 128], ident.v)
    kb.copy(hT_dst, pT.v.rr("p (c t) -> p c t", c=8), e='act')


def norm_T(kb, c, xv, hT_dst, ident):
    norm_T2(kb, c, norm_T1(kb, c, xv), hT_dst, ident)


def phase_M1(kb, g, l, xsrc):
    T, NT = g.T, g.NT
    phase_begin(kb)
    c = Ctx()
    c.wstage = RPool(kb, 3, [128, 2048], F32, "wst")
    c.dq = ['sp', 'act']
    c.dqi = 0
    c.cei = 0
    gc = kb.sb([128, NV], F32, "gc")
    kb.dma('sp', gc.v, g.gcols[l])
    WF = kb.sb([128, 8, 2048], BF16, "WF")
    WT = kb.sb([128, 8, 280], BF16, "WT")
    w = g.w_in
    for k in range(8):
        rows = slice(k * 128, (k + 1) * 128)
        gk = gc[:, k:k + 1]
        wload(kb, c, WF[:, k, 0:512], w[l, rows, O_Q:O_Q + 512], gk, 0.125)
        wload(kb, c, WF[:, k, 512:768], w[l, rows, O_KC:O_KC + 256], gk)
        wload(kb, c, WF[:, k, 768:896], w[l, rows, O_KS:O_KS + 128], gk)
        wload(kb, c, WF[:, k, 896:1024], w[l, rows, O_KW:O_KW + 128], gk)
        wload(kb, c, WF[:, k, 1024:2048], w[l, rows, O_XG:O_XG + 1024], gk)
        wload(kb, c, WT[:, k, 0:128], w[l, rows, O_VS:O_VS + 128], gk)
        wload(kb, c, WT[:, k, 128:256], w[l, rows, O_VW:O_VW + 128], gk)
        wload(kb, c, WT[:, k, 256:280], w[l, rows, O_GT:O_GT + 24], gk)
    ident = kb.sb([128, 128], BF16, "identb")
    idf = kb.sb([128, 128], F32, "identf")
    kb.dma('sp', idf.v, g.ident.v)
    kb.copy(ident.v, idf.v)
    c.stat = RPool(kb, 8, [128, 4], F32, "stat")
    c.junk = RPool(kb, 2, [128, 1024], F32, "junk")
    c.hb = RPool(kb, 6, [128, 1024], BF16, "hb")
    c.pT = RPool(kb, 1, [128, 1024], BF16, "pT", ps=True)
    xp = RPool(kb, 6, [128, 1024], F32, "xin")
    hTp = RPool(kb, 2, [128, 8, 512], BF16, "hT")
    ptm = RPool(kb, 1, [128, 512], F32, "ptm", ps=True)
    pfm = RPool(kb, 3, [128, 512], F32, "pfm", ps=True)
    vst = RPool(kb, 2, [128, 280], F32, "vst")
    fb = RPool(kb, 3, [128, 512], BF16, "fb")
    ff = RPool(kb, 3, [128, 512], F32, "ff")
    MT = min(512, T)
    nsub = MT // 128
    ei = [0]
    NM = T // MT

    def A1(m):
        hbs = []
        for s_ in range(nsub):
            t0 = m * MT + s_ * 128
            xs = xp.get()
            kb.dma('sp', xs.v, xsrc[t0:t0 + 128, :])
            hbs.append(norm_T1(kb, c, xs.v))
        return hbs

    def A2(m, hbs):
        hT = hTp.get()
        for s_ in range(nsub):
            t0 = m * MT + s_ * 128
            norm_T2(kb, c, hbs[s_], hT[:, :, s_ * 128:(s_ + 1) * 128], ident)
            pt = ptm.get()
            for k in range(8):
                kb.mm(pt[:, 0:280], hT[:, k, s_ * 128:(s_ + 1) * 128], WT[:, k, :], start=(k == 0), stop=(k == 7))
            vs = vst.get()
            kb.copy(vs.v, pt[:, 0:280], e='dve')
            kb.dma('sp', g.zV[t0:t0 + 128, :], vs.v)
        return hT

    def Bfc(m, hT, fc):
        pf = pfm.get()
        for k in range(8):
            kb.mm(pf[:, :MT], WF[:, k, fc * 128:(fc + 1) * 128], hT[:, k, :MT], start=(k == 0), stop=(k == 7))
        e = ['act', 'dve'][ei[0] % 2]
        ei[0] += 1
        if fc < 8:
            o = fb.get()
            kb.copy(o[:, :MT], pf[:, :MT], e=e)
            kb.dma('act' if fc % 2 else 'sp', g.zTb[fc * 128:(fc + 1) * 128, m * MT:(m + 1) * MT], o[:, :MT])
        else:
            o = ff.get()
            kb.copy(o[:, :MT], pf[:, :MT], e=e)
            kb.dma('act' if fc % 2 else 'sp', g.zTf[(fc - 8) * 128:(fc - 7) * 128, m * MT:(m + 1) * MT], o[:, :MT])

    hT = A2(0, A1(0))
    for m in range(NM):
        hbs = None
        for fc in range(16):
            Bfc(m, hT, fc)
            if fc == 2 and m + 1 < NM:
                hbs = A1(m + 1)
        if m + 1 < NM:
            hT = A2(m + 1, hbs)
    phase_end(kb)


def load_const_bf16(kb, c, dram_view, shape, name):
    st = kb.sb(shape, F32, name + "_f")
    kb.dma('sp', st.v, dram_view)
    b = kb.sb(shape, BF16, name)
    kb.copy(b.v, st.v, e='pool')
    return b


def phase_ATT(kb, g, l):
    T, NT, NB = g.T, g.NT, g.NB
    phase_begin(kb)
    c = Ctx()
    c.wstage = RPool(kb, 1, [128, 2048], F32, "wst")
    c.dq = ['sp', 'act']
    c.dqi = 0
    c.cei = 0
    ident = load_const_bf16(kb, c, g.ident.v, [128, 128], "ident")
    onesb = load_const_bf16(kb, c, g.ones_tab.v, [128, 128], "onesb")
    TCt = kb.sb([128, 8, 256], F32, "TCt")
    kb.dma('sp', TCt.v.rr("p h j -> p (h j)"), g.tc_tab.v)
    C31 = kb.sb([128, 8, 128], F32, "C31")
    kb.dma('act', C31.v.rr("p h j -> p (h j)"), g.c31_tab.v)
    DGs = kb.sb([128, 8, 128], F32, "DGs")
    kb.dma('sp', DGs.v.rr("p h j -> p (h j)"), g.dg_tab.v)
    kb.tt(DGs.v, DGs.v, C31.v, ALU.subtract, e='pool')
    SDGs = kb.sb([128, 8, 128], F32, "SDGs")
    kb.dma('act', SDGs.v.rr("p h j -> p (h j)"), g.sdg_tab.v)
    kb.tt(SDGs.v, SDGs.v, C31.v, ALU.subtract, e='pool')
    WEt = kb.sb([128, 4, 128], F32, "WEt")
    kb.dma('sp', WEt.v.rr("p h j -> p (h j)"), g.we_tab.v)
    ADDT = kb.sb([128, 256], F32, "ADDT")
    kb.dma('sp', ADDT.v, g.addt_tab.v)
    RV0 = kb.sb([128, 1], F32, "RV0")
    kb.dma('sp', RV0.v, g.rv0_tab.v)
    EX = kb.sb([128, T], BF16, "EX")
    KC2 = kb.sb([128, T], BF16, "KC2")
    VC2 = kb.sb([128, T], BF16, "VC2")
    EXa, EXb = KC2, VC2
    kb.memset(EXa.v, 1.0, e='pool')
    a0, a1, a2 = A(EXa.v), A(EXb.v), A(EX.v)
    kb.op('pool', lambda eng: eng.affine_select(a1, a0, [[1, T]], ALU.is_ge, 0.0, base=0, channel_multiplier=-64),
          [EXa], [EXb])
    kb.op('pool', lambda eng: eng.affine_select(a2, a1, [[-1, T]], ALU.is_ge, 0.0, base=63, channel_multiplier=64),
          [EXb], [EX])
    ssa = kb.sb([128, 2 * NT], F32, "ssa")
    LV = 9
    if LV <= 1:
        phase_end(kb)
        return

    KsT = kb.sb([128, T], BF16, "KsT")
    kb.memset(KsT[64:128, :], 0.0, e='pool')
    KwT = kb.sb([128, T], BF16, "KwT")
    kb.memset(KwT[64:128, :], 0.0, e='pool')
    VsA = kb.sb([128, NT, 65], BF16, "VsA")
    VwA = kb.sb([128, NT, 65], BF16, "VwA")
    Wck = kb.sb([128, 32, 128], BF16, "Wck")
    Wcv = kb.sb([128, 32, 64], BF16, "Wcv")
    pek = kb.sb([128, 32], BF16, "pek")
    pev = kb.sb([128, 32], BF16, "pev")
    pef = kb.sb([128, 64], F32, "pef")
    ckc = kb.sb([128, 1], F32, "ckc")
    cvr = kb.sb([1, 64], BF16, "cvr")
    KcT = kb.sb([128, NB], BF16, "KcT")
    Vc = kb.sb([128, 64], BF16, "Vc")
    vstage = RPool(kb, 2, [128, 16, 64], F32, "vstg")
    bankA = kb.ps([128, 512], F32, "bankA")
    bankT = kb.ps([128, 1024], BF16, "bankT")
    bankOc = kb.ps([128, 512], F32, "bankOc")
    stp = RPool(kb, 3, [128, 512], F32, "ST", ps=True)
    bOs = kb.ps([128, 512], F32, "bOs")
    bOw = kb.ps([128, 512], F32, "bOw")
    qap = RPool(kb, 3, [128, 4, 128], BF16, "Q4")
    for qb_ in qap.bufs:
        kb.memset(qb_[64:128, :, :], 0.0, e='pool')
    gtp = RPool(kb, 2, [128, 12], F32, "GT")
    gsp = RPool(kb, 3, [128, 12], F32, "GS")
    scp = RPool(kb, 2, [128, 4, NB], F32, "sC")
    ecp = RPool(kb, 2, [128, 4, NB], F32, "eC")
    ecbp = RPool(kb, 2, [128, 4, NB], BF16, "eCb")
    ectp = RPool(kb, 2, [128, 4, 128], BF16, "eCT")
    stat = RPool(kb, 4, [128, 48], F32, "st")
    impp = RPool(kb, 2, [128, NB], F32, "imp")
    selp = RPool(kb, 2, [128, NB], F32, "sel")
    sel2p = RPool(kb, 2, [128, NB], F32, "sel2")
    selmp = RPool(kb, 2, [128, NB], BF16, "selm")
    negp = RPool(kb, 2, [128, 4, 128], BF16, "negm")
    tmpp = RPool(kb, 2, [128, 4, 128], F32, "tmpl")
    pp = RPool(kb, 4, [128, 4, 128], BF16, "P")
    attp = RPool(kb, 2, [128, 4, 64], F32, "att")
    attbp = RPool(kb, 2, [128, 256], BF16, "attb")
    attTp = RPool(kb, 2, [128, 2, 128], BF16, "attT")
    junkp = RPool(kb, 2, [128, 256], F32, "junk")
    ocp = RPool(kb, 3, [128, 4, 64], F32, "Oc")
    osp = RPool(kb, 4, [65, 512], F32, "OsT")
    ossp = RPool(kb, 4, [128, 260], F32, "OsS")
    identf = kb.sb([128, 128], F32, "identf2")
    kb.dma('sp', identf.v, g.ident.v)

    kb.dma('sp', pef[:, 0:32], g.pe2k[l])
    kb.dma('sp', pef[:, 32:64], g.pe2v[l])
    kb.copy(pek.v, pef[:, 0:32], e='pool')
    kb.copy(pev.v, pef[:, 32:64], e='pool')
    st = c.wstage.get()
    kb.dma('sp', st.v.rr("p (c e) -> p c e", e=64), g.cmp_w_k[l].rr("(c p) e -> p c e", p=128))
    kb.copy(Wck[:, :, 0:64], st.v.rr("p (c e) -> p c e", e=64), e='dve')
    kb.copy(Wck[:, :, 64:128], st.v.rr("p (c e) -> p c e", e=64), e='pool')
    st = c.wstage.get()
    kb.dma('act', st.v.rr("p (c e) -> p c e", e=64), g.cmp_w_v[l].rr("(c p) e -> p c e", p=128))
    kb.copy(Wcv.v, st.v.rr("p (c e) -> p c e", e=64), e='dve')

    for g_ in range(2):
        kb.dma('sp', KsT[0:64, :], g.zTb[768 + g_ * 64:768 + g_ * 64 + 64, :])
        kb.dma('act', KwT[0:64, :], g.zTb[896 + g_ * 64:896 + g_ * 64 + 64, :])
        kb.dma('sp', KC2[0:64, :], g.zTb[512 + g_ * 64:512 + g_ * 64 + 64, :])
        kb.dma('sp', KC2[64:128, 0:T - 1], g.zTb[512 + g_ * 64:512 + g_ * 64 + 64, 1:T])
        kb.dma('act', VC2[0:64, :], g.zTb[640 + g_ * 64:640 + g_ * 64 + 64, :])
        kb.dma('act', VC2[64:128, 0:T - 1], g.zTb[640 + g_ * 64:640 + g_ * 64 + 64, 1:T])
        for (VA, coff) in ((VsA, g_ * 64), (VwA, 128 + g_ * 64)):
            kb.memset(VA[:, :, 64:65], 1.0, e='pool')
            for j0 in range(0, NT, 16):
                nj = min(16, NT - j0)
                vs = vstage.get()
                kb.dma('sp', vs[:, :nj, :], g.zV[j0 * 128:(j0 + nj) * 128, coff:coff + 64].rr("(j p) d -> p j d", p=128))
                kb.copy(VA[:, j0:j0 + nj, 0:64], vs[:, :nj, :], e='dve')
        if LV <= 2:
            continue
        KC2v = KC2.v.rr("p (n r) -> p n r", r=64)
        VC2v = VC2.v.rr("p (n r) -> p n r", r=64)
        for cc in range(32):
            kb.mm(bankA[:, 0:1], Wck[:, cc, :], pek[:, cc:cc + 1], start=(cc == 0), stop=(cc == 31))
        kb.copy(ckc.v, bankA[:, 0:1], e='dve')
        for cc in range(32):
            kb.mm(bankOc[:, 0:NB], Wck[:, cc, :], KC2v[:, :, 2 * cc], start=(cc == 0), stop=(cc == 31))
        kb.ts(KcT.v, bankOc[:, 0:NB], ckc.v, None, op0=ALU.add)
        for cc in range(32):
            kb.mm(bankA[0:1, 0:64], pev[:, cc:cc + 1], Wcv[:, cc, :], start=(cc == 0), stop=(cc == 31))
        kb.copy(cvr.v, bankA[0:1, 0:64], e='dve')
        for cc in range(32):
            kb.mm(bankOc[:NB, 0:64], VC2v[:, :, 2 * cc], Wcv[:, cc, :], start=(cc == 0), stop=False)
        kb.mm(bankOc[:NB, 0:64], onesb[0:1, :NB], cvr[0:1, :], start=False, stop=True)
        kb.copy(Vc[:NB, :], bankOc[:NB, 0:64], e='dve')

        if LV <= 3:
            continue
        hs0 = slice(0, 64)

        def prologue(i, g_=g_):
            S = Ctx()
            S.i = i

            def chunkA():
                Q4 = qap.get()
                r0 = g_ * 256
                kb.dma('sp', Q4[0:64, :, :], g.zTb[r0:r0 + 256, i * 128:(i + 1) * 128].rr("(h d) t -> d h t", d=64))
                S.Qh = [Q4[0:64, h, :] for h in range(4)]
                S.Q4 = Q4
                GT = gtp.get()
                kb.dma('act', GT.v, g.zV[i * 128:(i + 1) * 128, 256 + g_ * 12:256 + g_ * 12 + 12])
                GS = gsp.get()
                kb.act(GS.v, GT.v, AF.Exp, scale=-1.0)
                kb.ts(GS.v, GS.v, 1.0, None, op0=ALU.add)
                kb.recip(GS.v, GS.v)
                S.G3 = GS.v.rr("p (h k) -> p h k", k=3)
                s_ = stat.get()
                S.s_ = s_
                mx, nmx, sumC, rC = s_[:, 0:4], s_[:, 4:8], s_[:, 8:12], s_[:, 12:16]
                S.rC = rC
                m1, m2, thr = s_[:, 36:44], s_[:, 36:44], s_[:, 44:45]
                psC = bankA.v.rr("p (h n) -> p h n", h=4)[:, :, 0:NB]
                for h in range(4):
                    kb.mm(psC[:, h, :], S.Q4[:, h, :], KcT.v, start=(h == 0), stop=(h == 3), sgc=True)
                sC = scp.get()
                kb.tt(sC.v, psC, TCt[:, g_ * 4:(g_ + 1) * 4, 128 - 2 * i:128 - 2 * i + NB], ALU.add)
                sv, mv = A(sC.v), A(mx)
                kb.op('dve', lambda eng, sv=sv, mv=mv: eng.tensor_reduce(mv, sv, AX.X, ALU.max), [sC], [s_])
                kb.ts(nmx, mx, -1.0, None, op0=ALU.mult)
                eC = ecp.get()
                for h in range(4):
                    kb.act(eC[:, h, :], sC[:, h, :], AF.Exp, bias=nmx[:, h:h + 1], accum_out=sumC[:, h:h + 1])
                kb.recip(rC, sumC)
                if i == 0:
                    kb.ts(rC, rC, RV0.v, None, op0=ALU.mult)
                imp = impp.get()
                kb.ts(imp.v, eC[:, 0, :], rC[:, 0:1], None, op0=ALU.mult)
                for h in range(1, 4):
                    kb.stt(imp.v, eC[:, h, :], rC[:, h:h + 1], imp.v, ALU.mult, ALU.add)
                S.eCb = ecbp.get()
                kb.copy(S.eCb.v, eC.v, e='pool')
                sel = selp.get()
                kb.tt(sel.v, imp.v, ADDT[:, 128 - 2 * i:128 - 2 * i + NB], ALU.add)
                kb.memset(sel[:, 0:1], 1e4, e='dve')
                sa, m1a = A(sel.v), A(m1)
                kb.op('dve', lambda eng, sa=sa, m1a=m1a: eng.max(m1a, sa), [sel], [s_])
                if NB > 16:
                    sel2 = sel2p.get()
                    s2a = A(sel2.v)
                    kb.op('dve', lambda eng, sa=sa, m1a=m1a, s2a=s2a: eng.match_replace(s2a, m1a, sa, -1e30), [sel, s_], [sel2])
                    kb.op('dve', lambda eng, s2a=s2a, m1a=m1a: eng.max(m1a, s2a), [sel2], [s_])
                    kb.ts(thr, m2[:, 7:8], -5e29, None, op0=ALU.max)
                else:
                    kb.memset(thr, -5e29, e='dve')
                S.selm = selmp.get()
                kb.ts(S.selm.v, sel.v, thr, None, op0=ALU.is_ge)

            def chunkB():
                for h in range(4):
                    kb.tr(bankT[:NB, h * 128:(h + 1) * 128], S.eCb[:, h, :], ident.v)
                kb.tr(bankT[:NB, 512:640], S.selm.v, ident.v)
                S.eCT = ectp.get()
                kb.copy(S.eCT[:NB].rr("p h q -> p (h q)"), bankT[:NB, 0:512], e='dve')
                S.negm = negp.get()
                for h in range(4):
                    kb.ts(S.negm[:NB, h, :], bankT[:NB, 512:640], -1.0, 30000.0, op0=ALU.add, op1=ALU.mult,
                          e='dve')

            def chunkC():
                psOc = bankOc.v.rr("p (h d) -> p h d", h=4)[:, :, 0:64]
                for h in range(4):
                    kb.mm(psOc[:, h, :], S.eCT[:NB, h, :], Vc[:NB, :], start=(h == 0), stop=(h == 3), sgc=True)
                S.Oc = ocp.get()
                kb.copy(S.Oc.v, psOc, e='dve')

            return S, [chunkA, chunkB, chunkC]

        def dense_and_epilogue(S, hooks, g_=g_):
            i = S.i
            Os = bOs.v[:, 0:260].rr("p (h d) -> p h d", h=4)
            Ow = bOw.v[:, 0:260].rr("p (h d) -> p h d", h=4)
            tasks = [(0, j) for j in range(0, i + 1)] + [(1, j) for j in range(max(0, i - 4), i + 1)]
            firstj = {0: 0, 1: max(0, i - 4)}
            Pbuf = {}
            DEP = 2

            def emit_S(t):
                br, j = tasks[t]
                KT = KsT if br == 0 else KwT
                STb = stp.get()
                ST = STb.v.rr("p (h q) -> p h q", h=4)
                if br == 0:
                    kb.mm(STb.v, EX[:NB, j * 128:(j + 1) * 128], S.negm[:NB].rr("p h q -> p (h q)"), start=True, stop=False, sgc=True)
                kb.mm(STb.v, KT[:, j * 128:(j + 1) * 128], S.Q4.v.rr("d h q -> d (h q)"),
                      start=(br == 1), stop=True, sgc=True)
                P = pp.get()
                tm = None
                if j == i:
                    tm = DGs[:, g_ * 4:(g_ + 1) * 4, :]
                elif j == i - 1:
                    tm = SDGs[:, g_ * 4:(g_ + 1) * 4, :]
                elif br == 1 and j == i - 4:
                    tm = WEt.v
                if tm is not None:
                    tp = tmpp.get()
                    kb.tt(tp.v, ST, tm, ALU.add)
                    kb.act(P.v, tp.v, AF.Exp)
                else:
                    kb.act(P.v, ST, AF.Exp)
                Pbuf[t] = P

            def emit_PV(t):
                br, j = tasks[t]
                VA, O = (VsA, bOs) if br == 0 else (VwA, bOw)
                P = Pbuf.pop(t)
                kb.mm(O[0:65, :], VA[:, j, :], P.v.rr("p h q -> p (h q)"), start=(j == firstj[br]), stop=(j == i))

            nt = len(tasks)
            for t in range(nt + DEP):
                if t < nt:
                    emit_S(t)
                if t >= DEP:
                    emit_PV(t - DEP)
                for f in hooks.pop(t, []):
                    f()
            for t in sorted(hooks):
                for f in hooks[t]:
                    f()
            s_ = S.s_
            rC = S.rC
            rS, rW, aC, aS, aW = s_[:, 16:20], s_[:, 20:24], s_[:, 24:28], s_[:, 28:32], s_[:, 32:36]
            OsT = osp.get()
            OwT = osp.get()
            kb.copy(OsT.v, bOs[0:65, :], e='dve')
            kb.copy(OwT.v, bOw[0:65, :], e='dve')
            E = Ctx()

            def E1b():
                for h in range(4):
                    kb.tr(bankA[:, h * 65:(h + 1) * 65], OsT[0:65, h * 128:(h + 1) * 128], identf[0:65, 0:65])
                for h in range(4):
                    kb.tr(bankOc[:, h * 65:(h + 1) * 65], OwT[0:65, h * 128:(h + 1) * 128], identf[0:65, 0:65])
                OsSb = ossp.get()
                OwSb = ossp.get()
                kb.copy(OsSb.v, bankA[:, 0:260], e='dve')
                kb.copy(OwSb.v, bankOc[:, 0:260], e='dve')
                OsS = OsSb.v.rr("p (h d) -> p h d", h=4)
                OwS = OwSb.v.rr("p (h d) -> p h d", h=4)
                kb.recip(rS, OsS[:, :, 64])
                kb.recip(rW, OwS[:, :, 64])
                kb.tt(aC, S.G3[:, :, 0], rC, ALU.mult)
                kb.tt(aS, S.G3[:, :, 1], rS, ALU.mult)
                kb.tt(aW, S.G3[:, :, 2], rW, ALU.mult)
                att = attp.get()
                for h in range(4):
                    kb.ts(att[:, h, :], S.Oc[:, h, :], aC[:, h:h + 1], None, op0=ALU.mult, e='pool')
                    kb.stt(att[:, h, :], OsS[:, h, 0:64], aS[:, h:h + 1], att[:, h, :], ALU.mult, ALU.add)
                    kb.stt(att[:, h, :], OwS[:, h, 0:64], aW[:, h:h + 1], att[:, h, :], ALU.mult, ALU.add)
                attf = att.v.rr("p h d -> p (h d)")
                jk = junkp.get()
                ja, aa, sa_ = A(jk.v), A(attf), A(ssa[:, g_ * NT + i:g_ * NT + i + 1])
                kb.op('dve', lambda eng, ja=ja, aa=aa, sa_=sa_: eng.scalar_tensor_tensor(ja, aa, 1.0, aa, ALU.mult, ALU.mult, accum_out=sa_),
                      [att], [jk, ssa])
                E.attb = attbp.get()
                kb.copy(E.attb.v, attf, e='pool')

            def E2():
                for fc in range(2):
                    kb.tr(bankT[:, 640 + fc * 128:640 + (fc + 1) * 128], E.attb[:, fc * 128:(fc + 1) * 128], ident.v)
                attT = attTp.get()
                kb.copy(attT.v.rr("p c q -> p (c q)"), bankT[:, 640:896], e='dve')
                kb.dma('act', g.catT[g_ * 256:(g_ + 1) * 256, i * 128:(i + 1) * 128].rr("(c p) t -> p c t", p=128), attT.v)

            return E1b, E2

        S, hk = prologue(0)
        for f in hk:
            f()
        pend = None
        for i in range(NT):
            hooks = {}
            if i + 1 < NT:
                Sn, ch = prologue(i + 1)
                hooks.setdefault(1, []).append(ch[0])
                hooks.setdefault(8, []).append(ch[1])
                hooks.setdefault(12, []).append(ch[2])
            if pend is not None:
                hooks.setdefault(0, []).append(pend[0])
                hooks.setdefault(9, []).append(pend[1])
            if i + 1 >= NT:
                Sn = None
            pend = dense_and_epilogue(S, hooks)
            S = Sn
        pend[0]()
        pend[1]()
    kb.dma('sp', g.ssq[:, 0:2 * NT], ssa.v)
    phase_end(kb)


def bcast_row(view_1xn, p=128):
    ap = view_1xn.ap
    return View(view_1xn.buf, ap.broadcast_to([p, ap.shape[-1]]))


def phase_LRU(kb, g, l):
    T, NT = g.T, g.NT
    phase_begin(kb)
    TC = min(2048, T)
    nchunk = T // TC
    gc = kb.sb([128, NV], F32, "gc")
    kb.dma('sp', gc.v, g.gcols[l])
    onesf = kb.sb([128, 128], F32, "onesf")
    kb.dma('sp', onesf.v, g.ones_tab.v)
    WaBD = kb.sb([128, 4, 128], BF16, "WaBD")
    WxBD = kb.sb([128, 4, 128], BF16, "WxBD")
    kb.memset(WaBD.v, 0.0, e='pool')
    kb.memset(WxBD.v, 0.0, e='pool')
    wst = kb.sb([128, 2, 4, 64], F32, "wst")
    for wi, (src, dst) in enumerate(((g.lru_wa, WaBD), (g.lru_wx, WxBD))):
        for n_ in range(2):
            kb.dma('sp', wst[n_ * 64:(n_ + 1) * 64, wi, :, :], src[l].rr("(c n) d e -> n d c e", n=2)[n_])
        kb.copy(dst[0:64, :, 0:64], wst[0:64, wi, :, :], e='dve')
        kb.copy(dst[64:128, :, 64:128], wst[64:128, wi, :, :], e='dve')
    cA = kb.sb([128, 4], F32, "cA")
    kb.act(cA.v, gc[:, 68:72], AF.Exp, scale=-1.0)
    kb.act(cA.v, cA.v, AF.Ln, bias=1.0)
    kb.ts(cA.v, cA.v, -8.0, None, op0=ALU.mult)
    carry = kb.sb([128, 4], F32, "carry")
    ssl = kb.sb([128, NT], F32, "ssl")
    psS = kb.ps([128, 512], F32, "psS")
    psg = RPool(kb, 4, [128, 512], F32, "psg", ps=True)
    xgp = RPool(kb, 2, [128, TC], F32, "xg")
    xrp = RPool(kb, 2, [128, TC + 3], F32, "xr")
    xcp = RPool(kb, 2, [128, TC], F32, "xc")
    xcbp = RPool(kb, 2, [128, TC], BF16, "xcb")
    rp = RPool(kb, 2, [128, TC], F32, "r")
    igp = RPool(kb, 2, [128, TC], F32, "ig")
    t1p = RPool(kb, 2, [128, TC], F32, "t1")
    t2p = RPool(kb, 2, [128, TC], F32, "t2")
    hhp = RPool(kb, 2, [128, TC], F32, "hh")
    obp = RPool(kb, 2, [128, TC], BF16, "ob")
    for tc in range(nchunk):
        c0 = tc * TC
        for c in range(4):
            xg = xgp.get()
            kb.dma('sp', xg.v, g.zTf[c * 128:(c + 1) * 128, c0:c0 + TC])
            xr = xrp.get()
            if tc == 0:
                kb.memset(xr[:, 0:3], 0.0, e='pool')
                kb.dma('act', xr[:, 3:3 + TC], g.zTf[512 + c * 128:512 + (c + 1) * 128, c0:c0 + TC])
            else:
                kb.dma('act', xr.v, g.zTf[512 + c * 128:512 + (c + 1) * 128, c0 - 3:c0 + TC])
            xc = xcp.get()
            kb.ts(xc.v, xr[:, 3:3 + TC], gc[:, 40 + 3 * 4 + c:40 + 3 * 4 + c + 1], gc[:, 56 + c:57 + c], op0=ALU.mult, op1=ALU.add)
            for k in (2, 1, 0):
                kb.stt(xc.v, xr[:, k:k + TC], gc[:, 40 + k * 4 + c:40 + k * 4 + c + 1], xc.v, ALU.mult, ALU.add)
            xcb = xcbp.get()
            kb.copy(xcb.v, xc.v, e='act')
            r = rp.get()
            ig = igp.get()
            for blk in range(TC // 512):
                cs = slice(blk * 512, (blk + 1) * 512)
                pa = psg.get()
                kb.mm(pa.v, WaBD[:, c, :], xcb[:, cs], start=True, stop=True)
                kb.act(r[:, cs], pa.v, AF.Sigmoid, bias=gc[:, 60 + c:61 + c])
                px = psg.get()
                kb.mm(px.v, WxBD[:, c, :], xcb[:, cs], start=True, stop=True)
                kb.act(ig[:, cs], px.v, AF.Sigmoid, bias=gc[:, 64 + c:65 + c])
            kb.act(r.v, r.v, AF.Exp, scale=cA[:, c:c + 1])
            t1 = t1p.get()
            kb.act(t1.v, r.v, AF.Square)
            kb.act(t1.v, t1.v, AF.Sqrt, scale=-1.0, bias=1.0)
            kb.tt(ig.v, ig.v, xc.v, ALU.mult, e='pool')
            kb.tt(t1.v, t1.v, ig.v, ALU.mult)
            hh = hhp.get()
            init = 0.0 if tc == 0 else A(carry[:, c:c + 1])
            ha, ra, ba = A(hh.v), A(r.v), A(t1.v)
            kb.op('dve', lambda eng, ha=ha, ra=ra, ba=ba, init=init: eng.tensor_tensor_scan(ha, ra, ba, init, ALU.mult, ALU.add),
                  [r, t1, carry], [hh])
            kb.copy(carry[:, c:c + 1], hh[:, TC - 1:TC], e='dve')
            t2 = t2p.get()
            kb.tt(t2.v, xg.v, xg.v, ALU.mult, e='pool')
            kb.ts(t2.v, t2.v, 0.044715, 1.0, op0=ALU.mult, op1=ALU.add, e='pool')
            kb.tt(t2.v, t2.v, xg.v, ALU.mult, e='pool')
            kb.act(t2.v, t2.v, AF.Sigmoid, scale=1.5957691216057308)
            kb.tt(t2.v, t2.v, xg.v, ALU.mult, e='pool')
            kb.tt(hh.v, hh.v, t2.v, ALU.mult)
            ob = obp.get()
            kb.copy(ob.v, hh.v, e='act')
            kb.dma('sp', g.catT[512 + c * 128:512 + (c + 1) * 128, c0:c0 + TC], ob.v)
            kb.tt(t2.v, hh.v, hh.v, ALU.mult, e='pool')
            nb128 = TC // 128
            for b in range(nb128):
                kb.mm(psS[:, b:b + 1], t2[:, b * 128:(b + 1) * 128], onesf[:, 0:1],
                      start=(c == 0 and b == 0), stop=(c == 3 and b == nb128 - 1), sgc=True)
        kb.copy(ssl[:, tc * (TC // 128):(tc + 1) * (TC // 128)], psS[:, 0:TC // 128], e='dve')
    kb.dma('sp', g.ssq[:, 2 * NT:3 * NT], ssl.v)
    phase_end(kb)


def phase_XKV(kb, g, l):
    phase_begin(kb)
    c = Ctx()
    c.wstage = RPool(kb, 3, [128, 2048], F32, "wst")
    c.dq = ['sp', 'act']
    c.dqi = 0
    c.cei = 0
    gc = kb.sb([128, NV], F32, "gc")
    kb.dma('sp', gc.v, g.gcols[l])
    ident = load_const_bf16(kb, c, g.ident.v, [128, 128], "ident")
    Wkv = kb.sb([128, 8, 2048], BF16, "Wkv")
    for k in range(8):
        wload(kb, c, Wkv[:, k, :], g.xkv[l, k * 128:(k + 1) * 128, :], gc[:, 24 + k:25 + k])
    c.stat = RPool(kb, 2, [128, 4], F32, "stat")
    c.junk = RPool(kb, 1, [128, 1024], F32, "junk")
    c.hb = RPool(kb, 2, [128, 1024], BF16, "hb")
    c.pT = RPool(kb, 1, [128, 1024], BF16, "pT", ps=True)
    mnT = kb.sb([128, 8, 256], BF16, "mnT")
    mp = RPool(kb, 2, [128, 1024], F32, "memt")
    for s in range(2):
        mt = mp.get()
        kb.dma('sp', mt.v, g.mem[s * 128:(s + 1) * 128, :])
        norm_T(kb, c, mt.v, mnT[:, :, s * 128:(s + 1) * 128], ident)
    psp = RPool(kb, 3, [128, 512], F32, "psx", ps=True)
    ob = RPool(kb, 3, [128, 512], BF16, "ob")
    for fc in range(8):
        p = psp.get()
        for k in range(8):
            kb.mm(p[:, 0:256], Wkv[:, k, fc * 128:(fc + 1) * 128], mnT[:, k, :], start=(k == 0), stop=(k == 7))
        o = ob.get()
        kb.copy(o[:, 0:256], p[:, 0:256], e='dve')
        kb.dma('sp', g.ckT[fc * 128:(fc + 1) * 128, :], o[:, 0:256])
    for mc in range(2):
        for half in range(2):
            p = psp.get()
            for k in range(8):
                kb.mm(p.v, mnT[:, k, mc * 128:(mc + 1) * 128], Wkv[:, k, 1024 + half * 512:1024 + (half + 1) * 512],
                      start=(k == 0), stop=(k == 7))
            o = ob.get()
            kb.copy(o.v, p.v, e='act')
            kb.dma('sp', g.cv[mc * 128:(mc + 1) * 128, half * 512:(half + 1) * 512], o.v)
    phase_end(kb)


def phase_R1(kb, g, l, xsrc):
    T, NT = g.T, g.NT
    phase_begin(kb)
    c = Ctx()
    c.wstage = RPool(kb, 2, [128, 1024], F32, "wst")
    c.dq = ['sp', 'act']
    c.dqi = 0
    c.cei = 0
    gc = kb.sb([128, NV], F32, "gc")
    kb.dma('sp', gc.v, g.gcols[l])
    ident = load_const_bf16(kb, c, g.ident.v, [128, 128], "ident")
    onesb = load_const_bf16(kb, c, g.ones_tab.v, [128, 128], "onesb")
    WoA = kb.sb([128, 4, 1024], BF16, "WoA")
    WoL = kb.sb([128, 4, 1024], BF16, "WoL")
    Wq = kb.sb([128, 8, 1024], BF16, "Wq")
    Wo = kb.sb([128, 8, 1024], BF16, "Wo")
    for k in range(4):
        wload(kb, c, WoA[:, k, :], g.w_out[l, k * 128:(k + 1) * 128, :], gc[:, 32 + k:33 + k])
        wload(kb, c, WoL[:, k, :], g.w_out[l, 512 + k * 128:512 + (k + 1) * 128, :], gc[:, 36 + k:37 + k])
    for k in range(8):
        wload(kb, c, Wq[:, k, :], g.xq[l, k * 128:(k + 1) * 128, :], gc[:, 8 + k:9 + k], 1.0 / 16.0)
        wload(kb, c, Wo[:, k, :], g.xo[l, k * 128:(k + 1) * 128, :])
    ckT = kb.sb([128, 8, 256], BF16, "ckTs")
    kb.dma('sp', ckT.v, g.ckT.v.rr("(c p) m -> p c m", p=128))
    cv = kb.sb([128, 2, 1024], BF16, "cvs")
    kb.dma('act', cv.v, g.cv.v.rr("(c p) f -> p c f", p=128))
    GP1 = kb.sb([128, 1024], F32, "GP1")
    GP2 = kb.sb([128, 1024], F32, "GP2")
    kb.dma('sp', GP1.v, bcast_row(g.grows[l, 0:1, :]))
    kb.dma('act', GP2.v, bcast_row(g.grows[l, 1:2, :]))
    SS = kb.sb([128, 3 * NT], F32, "SS")
    kb.dma('sp', SS.v, g.ssq.v)
    rA = kb.sb([128, NT], F32, "rA")
    rL = kb.sb([128, NT], F32, "rL")
    kb.tt(rA.v, SS[:, 0:NT], SS[:, NT:2 * NT], ALU.add)
    rstd_from_ss(kb, rA.v, 512)
    kb.copy(rL.v, SS[:, 2 * NT:3 * NT], e='dve')
    rstd_from_ss(kb, rL.v, 512)
    c.stat = RPool(kb, 4, [128, 4], F32, "stat")
    c.junk = RPool(kb, 1, [128, 1024], F32, "junk")
    c.hb = RPool(kb, 2, [128, 1024], BF16, "hb")
    c.pT = RPool(kb, 1, [128, 1024], BF16, "pT", ps=True)
    MT = min(512, T)
    nsub = MT // 128
    P1 = kb.ps([128, 1024], F32, "P1")
    P2 = kb.ps([128, 1024], F32, "P2")
    psp = RPool(kb, 3, [128, 512], F32, "psr", ps=True)
    catp = RPool(kb, 2, [128, 8, MT], BF16, "cat")
    x4p = RPool(kb, 2, [128, nsub, 1024], F32, "x4")
    hTp = RPool(kb, 1, [128, 8, MT], BF16, "hT")
    cqp = RPool(kb, 1, [128, 8, MT], BF16, "cqT")
    cop = RPool(kb, 1, [128, 8, MT], BF16, "coT")
    ptp = RPool(kb, 4, [128, MT], BF16, "PT")
    rdp = RPool(kb, 2, [128, MT], F32, "rden")
    mxp = RPool(kb, 2, [128, 1024], F32, "mixed")
    xop = RPool(kb, 2, [128, 1024], F32, "xo")
    for m in range(T // MT):
        t0 = m * MT
        cat = catp.get()
        kb.dma('sp', cat.v, g.catT[:, t0:t0 + MT].rr("(c p) t -> p c t", p=128))
        x4 = x4p.get()
        kb.dma('act', x4.v, xsrc[t0:t0 + MT, :].rr("(s p) d -> p s d", p=128))
        hT = hTp.get()

        def Wout(s):
            ts_ = slice(s * 128, (s + 1) * 128)
            for half in range(2):
                hs = slice(half * 512, (half + 1) * 512)
                for k in range(4):
                    kb.mm(P1[:, hs], cat[:, k, ts_], WoA[:, k, hs], start=(k == 0), stop=(k == 3))
                for k in range(4):
                    kb.mm(P2[:, hs], cat[:, 4 + k, ts_], WoL[:, k, hs], start=(k == 0), stop=(k == 3))

        def chainA(s):
            ti = m * nsub + s
            mx = mxp.get()
            kb.act(mx.v, P1.v, AF.Copy, scale=rA[:, ti:ti + 1])
            kb.stt(mx.v, P2.v, rL[:, ti:ti + 1], mx.v, ALU.mult, ALU.add)
            st = c.stat.get()
            jk = c.junk.get()
            kb.act(jk.v, mx.v, AF.Square, accum_out=st[:, 0:1])
            rstd_from_ss(kb, st[:, 0:1], D)
            kb.stt(mx.v, mx.v, st[:, 0:1], GP1.v, ALU.mult, ALU.mult)
            kb.tt(x4[:, s, :], x4[:, s, :], mx.v, ALU.add, e='dve')
            return norm_T1(kb, c, x4[:, s, :])

        Wout(0)
        for s in range(nsub):
            hb_ = chainA(s)
            if s + 1 < nsub:
                Wout(s + 1)
            norm_T2(kb, c, hb_, hT[:, :, s * 128:(s + 1) * 128], ident)
        cqT = cqp.get()
        for fc in range(8):
            p = psp.get()
            for k in range(8):
                kb.mm(p[:, :MT], Wq[:, k, fc * 128:(fc + 1) * 128], hT[:, k, :], start=(k == 0), stop=(k == 7))
            kb.copy(cqT[:, fc, :], p[:, :MT], e=('dve' if fc % 2 else 'act'))
        coT = cop.get()
        for hx in range(4):
            PTs = []
            for mc in range(2):
                p = psp.get()
                for f2 in range(2):
                    kb.mm(p[:, :MT], ckT[:, 2 * hx + f2, mc * 128:(mc + 1) * 128], cqT[:, 2 * hx + f2, :],
                          start=(f2 == 0), stop=(f2 == 1))
                PT = ptp.get()
                kb.act(PT.v, p[:, :MT], AF.Exp)
                PTs.append(PT)
            p = psp.get()
            for mc in range(2):
                kb.mm(p[:, :MT], onesb.v, PTs[mc].v, start=(mc == 0), stop=(mc == 1))
            rden = rdp.get()
            kb.recip(rden.v, p[:, :MT])
            for dvc in range(2):
                p = psp.get()
                for mc in range(2):
                    kb.mm(p[:, :MT], cv[:, mc, hx * 256 + dvc * 128:hx * 256 + (dvc + 1) * 128], PTs[mc].v,
                          start=(mc == 0), stop=(mc == 1))
                kb.tt(coT[:, 2 * hx + dvc, :], p[:, :MT], rden.v, ALU.mult)
        for s in range(nsub):
            ts_ = slice(s * 128, (s + 1) * 128)
            for half in range(2):
                hs = slice(half * 512, (half + 1) * 512)
                for k in range(8):
                    kb.mm(P1[:, hs], coT[:, k, ts_], Wo[:, k, hs], start=(k == 0), stop=(k == 7))
            st = c.stat.get()
            jk = c.junk.get()
            kb.act(jk.v, P1.v, AF.Square, accum_out=st[:, 0:1])
            rstd_from_ss(kb, st[:, 0:1], D)
            xo = xop.get()
            kb.stt(xo.v, P1.v, st[:, 0:1], GP2.v, ALU.mult, ALU.mult)
            kb.tt(xo.v, xo.v, x4[:, s, :], ALU.add, e='dve')
            kb.dma('sp', g.xmid[t0 + s * 128:t0 + (s + 1) * 128, :], xo.v)
    phase_end(kb)


def phase_R2(kb, g, l, xdst):
    T, NT = g.T, g.NT
    phase_begin(kb)
    c = Ctx()
    c.wstage = RPool(kb, 2, [128, 1024], F32, "wst")
    c.dq = ['sp', 'act']
    c.dqi = 0
    c.cei = 0
    gc = kb.sb([128, NV], F32, "gc")
    kb.dma('sp', gc.v, g.gcols[l])
    ident = load_const_bf16(kb, c, g.ident.v, [128, 128], "ident")
    W1 = kb.sb([128, 8, 4096], BF16, "W1")
    W2 = kb.sb([128, 32, 1024], BF16, "W2")
    for k in range(8):
        for q in range(4):
            wload(kb, c, W1[:, k, q * 1024:(q + 1) * 1024], g.mlp_w1[l, k * 128:(k + 1) * 128, q * 1024:(q + 1) * 1024],
                  gc[:, 16 + k:17 + k])
    for k in range(32):
        wload(kb, c, W2[:, k, :], g.mlp_w2[l, k * 128:(k + 1) * 128, :])
    GP3 = kb.sb([128, 1024], F32, "GP3")
    kb.dma('sp', GP3.v, bcast_row(g.grows[l, 2:3, :]))
    c.stat = RPool(kb, 8, [128, 4], F32, "stat")
    c.junk = RPool(kb, 1, [128, 1024], F32, "junk")
    c.hb = RPool(kb, 4, [128, 1024], BF16, "hb")
    c.pT = RPool(kb, 1, [128, 1024], BF16, "pT", ps=True)
    MT = 256 if T % 256 == 0 else 128
    nsub = MT // 128
    Py = kb.ps([128, 1024], F32, "Py")
    psp = RPool(kb, 4, [128, 512], F32, "psm", ps=True)
    x2p = RPool(kb, 2, [128, nsub, 1024], F32, "x2")
    hTp = RPool(kb, 2, [128, 8, MT], BF16, "hT")
    utp = RPool(kb, 1, [128, 32, MT], BF16, "uT")
    rlp = RPool(kb, 3, [128, MT], F32, "rl")
    xop = RPool(kb, 2, [128, 1024], F32, "xo")
    NM = T // MT

    def A1(m):
        x2 = x2p.get()
        kb.dma('sp', x2.v, g.xmid[m * MT:(m + 1) * MT, :].rr("(s p) d -> p s d", p=128))
        return x2, [norm_T1(kb, c, x2[:, s_, :]) for s_ in range(nsub)]

    def A2(hbs):
        hT = hTp.get()
        for s_ in range(nsub):
            norm_T2(kb, c, hbs[s_], hT[:, :, s_ * 128:(s_ + 1) * 128], ident)
        return hT

    x2, hbs = A1(0)
    hT = A2(hbs)
    for m in range(NM):
        t0 = m * MT
        uT = utp.get()
        nxt = None
        for fc in range(32):
            p = psp.get()
            for k in range(8):
                kb.mm(p[:, :MT], W1[:, k, fc * 128:(fc + 1) * 128], hT[:, k, :], start=(k == 0), stop=(k == 7))
            rl = rlp.get()
            kb.act(rl.v, p[:, :MT], AF.Relu)
            kb.tt(uT[:, fc, :], rl.v, rl.v, ALU.mult, e='dve')
            if fc == 6 and m + 1 < NM:
                nxt = A1(m + 1)
        if nxt is not None:
            hTn = A2(nxt[1])
        for s_ in range(nsub):
            for half in range(2):
                hs = slice(half * 512, (half + 1) * 512)
                for k in range(32):
                    kb.mm(Py[:, hs], uT[:, k, s_ * 128:(s_ + 1) * 128], W2[:, k, hs], start=(k == 0), stop=(k == 31))
            st = c.stat.get()
            jk = c.junk.get()
            kb.act(jk.v, Py.v, AF.Square, accum_out=st[:, 0:1])
            rstd_from_ss(kb, st[:, 0:1], D)
            xo = xop.get()
            kb.stt(xo.v, Py.v, st[:, 0:1], GP3.v, ALU.mult, ALU.mult)
            kb.tt(xo.v, xo.v, x2[:, s_, :], ALU.add, e='dve')
            kb.dma('act', xdst[t0 + s_ * 128:t0 + (s_ + 1) * 128, :], xo.v)
        if nxt is not None:
            x2, hT = nxt[0], hTn
    phase_end(kb)


def t5_bucket_np(d):
    d = np.maximum(d, 0)
    df = np.maximum(d, 1).astype(np.float32)
    large = 16 + (np.log(df / np.float32(16)) / np.float32(math.log(8.0)) * np.float32(16)).astype(np.int32)
    large = np.minimum(large, 31)
    return np.where(d < 16, d, large)


def host_tables(rel_bias):
    rb = np.asarray(rel_bias, np.float32)
    q = np.arange(128)
    tabs = {}
    jp = np.arange(256)
    dist = q[:, None] + 64 * (128 - jp[None, :]) - 63
    bk = t5_bucket_np(dist)
    tc = rb[bk]
    tc = np.where((dist >= 0)[:, :, None], tc, np.float32(NEGM)).transpose(0, 2, 1)
    tabs["tc_tab"] = np.ascontiguousarray(tc, np.float32).reshape(128, 8 * 256)
    k = np.arange(128)
    dist = q[None, :] - k[:, None]
    dg = rb[t5_bucket_np(dist)]
    dg = np.where((dist >= 0)[:, :, None], dg, np.float32(NEGM)).transpose(0, 2, 1)
    tabs["dg_tab"] = np.ascontiguousarray(dg, np.float32).reshape(128, 8 * 128)
    dist = 128 + q[None, :] - k[:, None]
    sdg = rb[t5_bucket_np(dist)].transpose(0, 2, 1)
    tabs["sdg_tab"] = np.ascontiguousarray(sdg, np.float32).reshape(128, 8 * 128)
    c31 = np.broadcast_to(rb[31][None, :, None], (128, 8, 128))
    tabs["c31_tab"] = np.ascontiguousarray(c31, np.float32).reshape(128, 8 * 128)
    we = np.where(k[:, None] > q[None, :], np.float32(0), np.float32(NEGM))
    tabs["we_tab"] = np.ascontiguousarray(np.broadcast_to(we[:, None, :], (128, 4, 128)), np.float32).reshape(128, 512)
    rel = (jp[None, :] - 128) - (q[:, None] >= 64)
    addt = np.where(rel > 0, np.float32(-1e30), np.where(rel >= -1, np.float32(1e4), np.float32(0)))
    tabs["addt_tab"] = np.ascontiguousarray(addt, np.float32)
    tabs["rv0_tab"] = (q >= 63).astype(np.float32).reshape(128, 1)
    tabs["ident"] = np.eye(128, dtype=np.float32)
    bm = np.zeros((4, 4, 128), np.float32)
    for h in range(4):
        bm[h, h, :] = 1
    tabs["ones_tab"] = np.ones((128, 128), np.float32)
    return tabs


def host_layer_tables(inp, L):
    def col(v, nch):
        return np.asarray(v, np.float32)[:L].reshape(L, nch, 128).transpose(0, 2, 1)
    cols = [col(inp["ln_mix_pre"], 8), col(inp["ln_x_pre"], 8), col(inp["ln_mlp_pre"], 8), col(inp["ln_mem"], 8),
            col(inp["gn_attn"], 4), col(inp["gn_lru"], 4)]
    cw = np.asarray(inp["conv_w"], np.float32)[:L]
    cols.append(cw.reshape(L, 4, 4, 128).transpose(0, 3, 1, 2).reshape(L, 128, 16))
    for nm in ["conv_b", "lru_ba", "lru_bx", "lru_lambda"]:
        cols.append(col(inp[nm], 4))
    gcols = np.ascontiguousarray(np.concatenate(cols, axis=2), np.float32)
    assert gcols.shape == (L, 128, NV)
    grows = np.ascontiguousarray(np.stack([inp["ln_mix_post"][:L], inp["ln_x_post"][:L], inp["ln_mlp_post"][:L]], axis=1), np.float32)
    out = {"gcols": gcols, "grows": grows}
    for nm, key in [("pe2k", "cmp_pe_k"), ("pe2v", "cmp_pe_v")]:
        pe = np.asarray(inp[key], np.float32)[:L]
        out[nm] = np.ascontiguousarray(pe.reshape(L, 32, 2, 64).transpose(0, 2, 3, 1).reshape(L, 128, 32))
    return out


WNAMES = [("w_in", [1024, 2328]), ("cmp_w_k", [4096, 64]), ("cmp_w_v", [4096, 64]), ("lru_wa", [8, 64, 64]),
          ("lru_wx", [8, 64, 64]), ("w_out", [1024, 1024]), ("xq", [1024, 1024]), ("xkv", [1024, 2048]),
          ("xo", [1024, 1024]), ("mlp_w1", [1024, 4096]), ("mlp_w2", [4096, 1024])]
TABS = [("tc_tab", [128, 2048]), ("dg_tab", [128, 1024]), ("sdg_tab", [128, 1024]), ("c31_tab", [128, 1024]),
        ("we_tab", [128, 512]), ("addt_tab", [128, 256]), ("rv0_tab", [128, 1]), ("ident", [128, 128]),
        ("ones_tab", [128, 128])]


def build(T, L, debug=False, stop_after=None):
    nc = bass.Bass("TRN2", target_bir_lowering=False)
    es = ExitStack()
    with es:
        kb = KB(nc, es)
        g = Ctx()
        g.T, g.NT, g.NB, g.L = T, T // 128, T // 64, L
        g.x = kb.dram("x", [T, D], F32, kind="ExternalInput")
        g.mem = kb.dram("mem", [256, D], F32, kind="ExternalInput")
        for nm, shp in WNAMES:
            setattr(g, nm, kb.dram(nm, [L] + shp, F32, kind="ExternalInput"))
        for nm, shp in TABS:
            setattr(g, nm, kb.dram(nm, shp, F32, kind="ExternalInput"))
        g.gcols = kb.dram("gcols", [L, 128, NV], F32, kind="ExternalInput")
        g.grows = kb.dram("grows", [L, 3, D], F32, kind="ExternalInput")
        g.pe2k = kb.dram("pe2k", [L, 128, 32], F32, kind="ExternalInput")
        g.pe2v = kb.dram("pe2v", [L, 128, 32], F32, kind="ExternalInput")
        g.out = kb.dram("out", [T, D], F32, kind="ExternalOutput")
        sk = "ExternalOutput" if debug else "Internal"
        g.zTb = kb.dram("zTb", [1024, T], BF16, kind=sk)
        g.zTf = kb.dram("zTf", [1024, T], F32, kind=sk)
        g.zV = kb.dram("zV", [T, 280], F32, kind=sk)
        g.catT = kb.dram("catT", [1024, T], BF16, kind=sk)
        g.ssq = kb.dram("ssq", [128, 3 * g.NT], F32, kind=sk)
        g.ckT = kb.dram("ckT", [1024, 256], BF16, kind=sk)
        g.cv = kb.dram("cv", [256, 1024], BF16, kind=sk)
        g.xmid = kb.dram("xmid", [T, D], F32, kind=sk)
        g.xres = kb.dram("xres", [T, D], F32, kind=sk)
        kb.cur_phase = 0
        phases = []
        for l in range(L):
            xsrc = g.x if l == 0 else g.xres
            xdst = g.out if l == L - 1 else g.xres
            phases += [("M1", lambda l=l, xsrc=xsrc: phase_M1(kb, g, l, xsrc)),
                       ("ATT", lambda l=l: phase_ATT(kb, g, l)),
                       ("LRU", lambda l=l: phase_LRU(kb, g, l)),
                       ("XKV", lambda l=l: phase_XKV(kb, g, l)),
                       ("R1", lambda l=l, xsrc=xsrc: phase_R1(kb, g, l, xsrc)),
                       ("R2", lambda l=l, xdst=xdst: phase_R2(kb, g, l, xdst))]
        for i, (nm, fn) in enumerate(phases):
            kb.cur_phase = i + 1
            fn()
            if stop_after is not None and i + 1 >= stop_after:
                break
    return nc


def make_inmaps(inputs, T, L, ncores):
    tabs = host_tables(inputs["rel_bias"])
    lt = host_layer_tables(inputs, L)
    shared = {}
    for nm, shp in WNAMES:
        shared[nm] = np.ascontiguousarray(np.asarray(inputs[nm], np.float32)[:L].reshape([L] + shp))
    shared.update(tabs)
    shared.update(lt)
    maps = []
    B = inputs["x"].shape[0]
    for c in range(ncores):
        b = c % B
        m = dict(shared)
        m["x"] = np.ascontiguousarray(np.asarray(inputs["x"], np.float32)[b, :T])
        m["mem"] = np.ascontiguousarray(np.asarray(inputs["mem"], np.float32)[b])
        maps.append(m)
    return maps


_NC_CACHE = {}


def kernel(**inputs):
    T, L = 8192, 2
    B = inputs["x"].shape[0]
    key = (T, L)
    if key not in _NC_CACHE:
        _NC_CACHE[key] = build(T, L)
    nc = _NC_CACHE[key]
    ncores = 8
    maps = make_inmaps(inputs, T, L, ncores)
    res = run_bass_kernel_spmd(nc, maps, core_ids=list(range(ncores)))
    out = np.stack([np.asarray(res.results[b]["out"], np.float32) for b in range(B)], axis=0)
    return out
```
